# Optimizing a Trainium2 kernel written in Bass

```python
import jax, jax.numpy as jnp
from jax import lax
import numpy as np

D_MODEL = 1024
BATCH = 2
SEQ = 8192
DEPTH = 1

CHUNK = 64
MEM_LEN = 256
EPS = 1e-6
CONV_W = 4

GDN_HEADS = 4
GDN_DK = 128
GDN_DV = 128
GDN_QK = GDN_HEADS * GDN_DK
GDN_VD = GDN_HEADS * GDN_DV
LRU_DIM = D_MODEL // 2
LRU_BLOCKS = 8
LRU_BW = LRU_DIM // LRU_BLOCKS
LRU_C = 8.0
MIX_DIM = GDN_VD + LRU_DIM
IN_SPLITS = (3 * GDN_QK, GDN_VD, GDN_HEADS, GDN_HEADS, LRU_DIM, LRU_DIM)
IN_COLS = 3 * GDN_QK + GDN_VD + 2 * GDN_HEADS + 2 * LRU_DIM

XA_HEADS = 4
XA_DH = D_MODEL // XA_HEADS

PEER_HEADS = 8
PEER_NKEYS = 128
PEER_N = PEER_NKEYS * PEER_NKEYS
PEER_DQ = 256
PEER_DH = PEER_DQ // 2
PEER_TOPK = 16
PEER_BLOCK = 128

kernel_name = 'hybrid_gdn_rglru_peer_block'


def rmsnorm(x, w):
    xf = x.astype(jnp.float32)
    y = xf * lax.rsqrt(jnp.mean(xf * xf, axis=-1, keepdims=True) + EPS)
    return (y * w.astype(jnp.float32)).astype(x.dtype)


def l2norm(x):
    return x * lax.rsqrt(jnp.sum(x * x, axis=-1, keepdims=True) + EPS)


def causal_dwconv(x, w, b=None):
    s = x.shape[1]
    xp = jnp.pad(x, ((0, 0), (CONV_W - 1, 0), (0, 0)))
    y = sum(xp[:, j:j + s] * w[j] for j in range(CONV_W))
    return y if b is None else y + b


def gated_delta_rule(q, k, v, g, beta):
    bsz, s, h, dk = q.shape
    dv = v.shape[-1]
    n = s // CHUNK
    f32 = jnp.float32
    q = l2norm(q.astype(f32)) * (dk ** -0.5)
    k = l2norm(k.astype(f32))
    v = v.astype(f32)

    def chunks(t):
        return t.reshape(bsz, n, CHUNK, h, -1).transpose(0, 3, 1, 2, 4)

    qc, kc, vc = chunks(q), chunks(k), chunks(v)
    gc = g.astype(f32).reshape(bsz, n, CHUNK, h).transpose(0, 3, 1, 2)
    bc = beta.astype(f32).reshape(bsz, n, CHUNK, h).transpose(0, 3, 1, 2)
    gcum = jnp.cumsum(gc, axis=-1)
    pos = jnp.arange(CHUNK)
    causal = pos[:, None] >= pos[None, :]
    strict = pos[:, None] > pos[None, :]
    decay = jnp.exp(jnp.where(causal, gcum[..., :, None] - gcum[..., None, :], -jnp.inf))
    kb = kc * bc[..., None]
    vb = vc * bc[..., None]
    a_mat = jnp.where(strict, jnp.einsum('bhncd,bhnsd->bhncs', kb, kc) * decay, 0.0)
    t_mat = a_mat + jnp.eye(CHUNK, dtype=f32)
    rhs = jnp.concatenate([vb, kb * jnp.exp(gcum)[..., None]], axis=-1)
    sol = lax.linalg.triangular_solve(t_mat, rhs, left_side=True, lower=True, unit_diagonal=True)
    u, w = sol[..., :dv], sol[..., dv:]
    attn = jnp.where(causal, jnp.einsum('bhncd,bhnsd->bhncs', qc, kc) * decay, 0.0)
    q_dec = qc * jnp.exp(gcum)[..., None]
    k_dec = kc * jnp.exp(gcum[..., -1:] - gcum)[..., None]
    chunk_decay = jnp.exp(gcum[..., -1])

    def step(state, xs):
        q_i, w_i, u_i, a_i, k_i, d_i = xs
        v_new = u_i - jnp.einsum('bhcd,bhdv->bhcv', w_i, state)
        o = jnp.einsum('bhcd,bhdv->bhcv', q_i, state) + jnp.einsum('bhcs,bhsv->bhcv', a_i, v_new)
        state = state * d_i[..., None, None] + jnp.einsum('bhcd,bhcv->bhdv', k_i, v_new)
        return state, o

    xs = tuple(jnp.moveaxis(t, 2, 0) for t in (q_dec, w, u, attn, k_dec, chunk_decay))
    state0 = jnp.zeros((bsz, h, dk, dv), f32)
    _, o = lax.scan(step, state0, xs)
    return o.transpose(1, 0, 3, 2, 4).reshape(bsz, s, h, dv)


def rg_lru(xr, w_a, b_a, w_x, b_x, lam):
    bsz, s, c = xr.shape
    f32 = jnp.float32
    xf = xr.astype(f32)
    xb = xf.reshape(bsz, s, LRU_BLOCKS, LRU_BW)
    r = jax.nn.sigmoid(jnp.einsum('bsgi,gij->bsgj', xb, w_a.astype(f32)).reshape(bsz, s, c) + b_a.astype(f32))
    i = jax.nn.sigmoid(jnp.einsum('bsgi,gij->bsgj', xb, w_x.astype(f32)).reshape(bsz, s, c) + b_x.astype(f32))
    log_a = -LRU_C * r * jax.nn.softplus(-lam.astype(f32))
    a = jnp.exp(log_a)
    b_term = jnp.sqrt(-jnp.expm1(2.0 * log_a)) * (i * xf)

    def combine(c1, c2):
        a1, b1 = c1
        a2, b2 = c2
        return a1 * a2, a2 * b1 + b2

    _, hs = lax.associative_scan(combine, (a, b_term), axis=1)
    return hs


def hybrid_mixer(h, w_in, conv_qkv_w, gdn_a_log, gdn_dt_bias, gdn_norm_w,
                 lru_conv_w, lru_conv_b, lru_wa, lru_ba, lru_wx, lru_bx, lru_lambda, w_out):
    bsz, s, _ = h.shape
    f32 = jnp.float32
    proj = h @ w_in
    offs = np.cumsum(IN_SPLITS)[:-1].tolist()
    qkv, z, a_in, b_in, xr, gate = jnp.split(proj, offs, axis=-1)
    qkv = jax.nn.silu(causal_dwconv(qkv, conv_qkv_w))
    q, k, v = jnp.split(qkv, [GDN_QK, 2 * GDN_QK], axis=-1)
    g = -jnp.exp(gdn_a_log.astype(f32)) * jax.nn.softplus(a_in.astype(f32) + gdn_dt_bias.astype(f32))
    beta = jax.nn.sigmoid(b_in.astype(f32))
    o = gated_delta_rule(q.reshape(bsz, s, GDN_HEADS, GDN_DK), k.reshape(bsz, s, GDN_HEADS, GDN_DK),
                         v.reshape(bsz, s, GDN_HEADS, GDN_DV), g, beta)
    o = rmsnorm(o, gdn_norm_w) * jax.nn.silu(z.reshape(bsz, s, GDN_HEADS, GDN_DV).astype(f32))
    y_gdn = o.reshape(bsz, s, GDN_VD).astype(h.dtype)
    xr = causal_dwconv(xr, lru_conv_w, lru_conv_b)
    y_lru = (rg_lru(xr, lru_wa, lru_ba, lru_wx, lru_bx, lru_lambda) * jax.nn.gelu(gate.astype(f32))).astype(h.dtype)
    return jnp.concatenate([y_gdn, y_lru], axis=-1) @ w_out


def memory_cross_attention(h, mem_n, wq, wkv, wo):
    bsz, s, _ = h.shape
    m = mem_n.shape[1]
    f32 = jnp.float32
    q = (h @ wq).reshape(bsz, s, XA_HEADS, XA_DH)
    kv = (mem_n @ wkv).reshape(bsz, m, 2, XA_HEADS, XA_DH)
    k, v = kv[:, :, 0], kv[:, :, 1]
    scores = jnp.einsum('bshd,bmhd->bhsm', q.astype(f32), k.astype(f32)) * (XA_DH ** -0.5)
    p = jax.nn.softmax(scores, axis=-1)
    o = jnp.einsum('bhsm,bmhd->bshd', p, v.astype(f32)).reshape(bsz, s, XA_HEADS * XA_DH).astype(h.dtype)
    return o @ wo


def peer_ffn(h, wq, subkeys, u_tab, v_tab):
    bsz, s, d = h.shape
    t = h.reshape(bsz * s, d)
    ntok = t.shape[0]
    f32 = jnp.float32
    q = (t @ wq).reshape(ntok, PEER_HEADS, 2, PEER_DH).astype(f32)
    sc = jnp.einsum('thpd,hpkd->thpk', q, subkeys.astype(f32))
    s1, i1 = lax.top_k(sc[:, :, 0], PEER_TOPK)
    s2, i2 = lax.top_k(sc[:, :, 1], PEER_TOPK)
    cand = (s1[..., :, None] + s2[..., None, :]).reshape(ntok, PEER_HEADS, PEER_TOPK * PEER_TOPK)
    best, pos = lax.top_k(cand, PEER_TOPK)
    e1 = jnp.take_along_axis(i1, pos // PEER_TOPK, axis=-1)
    e2 = jnp.take_along_axis(i2, pos % PEER_TOPK, axis=-1)
    idx = e1 * PEER_NKEYS + e2
    gate = jax.nn.softmax(best, axis=-1).astype(h.dtype)
    nb = ntok // PEER_BLOCK

    def block(args):
        tb, ib, gb = args
        act = jax.nn.gelu(jnp.einsum('phkd,pd->phk', u_tab[ib], tb)) * gb
        return jnp.einsum('phk,phkd->pd', act, v_tab[ib])

    out = lax.map(block, (t.reshape(nb, PEER_BLOCK, d),
                          idx.reshape(nb, PEER_BLOCK, PEER_HEADS, PEER_TOPK),
                          gate.reshape(nb, PEER_BLOCK, PEER_HEADS, PEER_TOPK)))
    return out.reshape(bsz, s, d)


def setup_inputs(seed: int = 0) -> dict:
    key = jax.random.key(seed)
    ks = jax.random.split(key, 32)
    f32 = jnp.float32
    L = DEPTH

    def nrm(k, shape, scale):
        return jax.random.normal(k, shape, f32) * scale

    def gain(k, shape):
        return 1.0 + 0.05 * jax.random.normal(k, shape, f32)

    x = nrm(ks[0], (BATCH, SEQ, D_MODEL), 1.0)
    mem = nrm(ks[1], (BATCH, MEM_LEN, D_MODEL), 1.0)
    norm_mix_w = gain(ks[2], (L, D_MODEL))
    w_in = nrm(ks[3], (L, D_MODEL, IN_COLS), D_MODEL ** -0.5)
    conv_qkv_w = nrm(ks[4], (L, CONV_W, 3 * GDN_QK), 0.5)
    gdn_a_log = jnp.log(jax.random.uniform(ks[5], (L, GDN_HEADS), f32, 1.0, 16.0))
    dt = jnp.exp(jax.random.uniform(ks[6], (L, GDN_HEADS), f32, np.log(1e-3), np.log(1e-1)))
    gdn_dt_bias = dt + jnp.log(-jnp.expm1(-dt))
    gdn_norm_w = gain(ks[7], (L, GDN_DV))
    lru_conv_w = nrm(ks[8], (L, CONV_W, LRU_DIM), 0.5)
    lru_conv_b = nrm(ks[9], (L, LRU_DIM), 0.01)
    lru_wa = nrm(ks[10], (L, LRU_BLOCKS, LRU_BW, LRU_BW), LRU_BW ** -0.5)
    lru_ba = nrm(ks[11], (L, LRU_DIM), 0.01)
    lru_wx = nrm(ks[12], (L, LRU_BLOCKS, LRU_BW, LRU_BW), LRU_BW ** -0.5)
    lru_bx = nrm(ks[13], (L, LRU_DIM), 0.01)
    a0 = jax.random.uniform(ks[14], (L, LRU_DIM), f32, 0.9, 0.999)
    a_base = a0 ** (1.0 / LRU_C)
    lru_lambda = jnp.log(a_base) - jnp.log1p(-a_base)
    w_out = nrm(ks[15], (L, MIX_DIM, D_MODEL), MIX_DIM ** -0.5)
    norm_xattn_w = gain(ks[16], (L, D_MODEL))
    norm_mem_w = gain(ks[17], (L, D_MODEL))
    xattn_wq = nrm(ks[18], (L, D_MODEL, XA_HEADS * XA_DH), D_MODEL ** -0.5)
    xattn_wkv = nrm(ks[19], (L, D_MODEL, 2 * XA_HEADS * XA_DH), D_MODEL ** -0.5)
    xattn_wo = nrm(ks[20], (L, XA_HEADS * XA_DH, D_MODEL), (XA_HEADS * XA_DH) ** -0.5)
    norm_ffn_w = gain(ks[21], (L, D_MODEL))
    peer_wq = nrm(ks[22], (L, D_MODEL, PEER_HEADS * PEER_DQ), D_MODEL ** -0.5)
    peer_subkeys = nrm(ks[23], (L, PEER_HEADS, 2, PEER_NKEYS, PEER_DH), PEER_DH ** -0.5)
    peer_u = nrm(ks[24], (L, PEER_N, D_MODEL), D_MODEL ** -0.5)
    peer_v = nrm(ks[25], (L, PEER_N, D_MODEL), PEER_HEADS ** -0.5)
    norm_final_w = gain(ks[26], (D_MODEL,))
    return {'x': x, 'mem': mem, 'norm_mix_w': norm_mix_w, 'w_in': w_in, 'conv_qkv_w': conv_qkv_w,
            'gdn_a_log': gdn_a_log, 'gdn_dt_bias': gdn_dt_bias, 'gdn_norm_w': gdn_norm_w,
            'lru_conv_w': lru_conv_w, 'lru_conv_b': lru_conv_b, 'lru_wa': lru_wa, 'lru_ba': lru_ba,
            'lru_wx': lru_wx, 'lru_bx': lru_bx, 'lru_lambda': lru_lambda, 'w_out': w_out,
            'norm_xattn_w': norm_xattn_w, 'norm_mem_w': norm_mem_w, 'xattn_wq': xattn_wq,
            'xattn_wkv': xattn_wkv, 'xattn_wo': xattn_wo, 'norm_ffn_w': norm_ffn_w,
            'peer_wq': peer_wq, 'peer_subkeys': peer_subkeys, 'peer_u': peer_u, 'peer_v': peer_v,
            'norm_final_w': norm_final_w}


def reference(x, mem, norm_mix_w, w_in, conv_qkv_w, gdn_a_log, gdn_dt_bias, gdn_norm_w,
              lru_conv_w, lru_conv_b, lru_wa, lru_ba, lru_wx, lru_bx, lru_lambda, w_out,
              norm_xattn_w, norm_mem_w, xattn_wq, xattn_wkv, xattn_wo, norm_ffn_w,
              peer_wq, peer_subkeys, peer_u, peer_v, norm_final_w):
    for l in range(DEPTH):
        x = x + hybrid_mixer(rmsnorm(x, norm_mix_w[l]), w_in[l], conv_qkv_w[l], gdn_a_log[l],
                             gdn_dt_bias[l], gdn_norm_w[l], lru_conv_w[l], lru_conv_b[l],
                             lru_wa[l], lru_ba[l], lru_wx[l], lru_bx[l], lru_lambda[l], w_out[l])
        x = x + memory_cross_attention(rmsnorm(x, norm_xattn_w[l]), rmsnorm(mem, norm_mem_w[l]),
                                       xattn_wq[l], xattn_wkv[l], xattn_wo[l])
        x = x + peer_ffn(rmsnorm(x, norm_ffn_w[l]), peer_wq[l], peer_subkeys[l], peer_u[l], peer_v[l])
    return rmsnorm(x, norm_final_w)
```

```python
import contextlib
import numpy as np
import concourse.bass as bass
import concourse.mybir as mybir
from concourse.bass_utils import run_bass_kernel_spmd

F32 = mybir.dt.float32
BF16 = mybir.dt.bfloat16
U32 = mybir.dt.uint32
AF = mybir.ActivationFunctionType
OP = mybir.AluOpType
AX = mybir.AxisListType

NCORES = 8
D = 1024
SEQ = 8192
TOK = 2048
EPS = 1e-6
ENG = ("pe", "act", "dve", "pool", "sp")


class Prog:
    def __init__(self, nc):
        self.nc = nc
        self.ops = {e: [] for e in ENG}
        self.cnt = {}
        self.epoch = {e: 0 for e in ENG}
        self.waited = {}
        self.lastw = {}
        self.reads = {}
        self.semkeys = []
        self.sems = {}

    def _semkey_engine(self, e):
        k = ("E", e, self.epoch[e])
        if self.cnt.get(k, 0) >= 30000:
            self.epoch[e] += 1
            k = ("E", e, self.epoch[e])
        return k

    def _deps(self, eng, r, w, skip_same_pe):
        deps = {}
        def add(sv):
            if sv is None:
                return
            k, v = sv
            if skip_same_pe and k[0] == "E" and k[1] == "pe":
                return
            if deps.get(k, 0) < v:
                deps[k] = v
        for b in r:
            add(self.lastw.get(b))
        for b in w:
            add(self.lastw.get(b))
            for sv in self.reads.get(b, ()):
                add(sv)
        out = []
        for k, v in deps.items():
            if self.waited.get((eng, k), 0) < v:
                self.waited[(eng, k)] = v
                out.append((k, v))
        return out

    def _commit(self, r, w, sv):
        for b in r:
            self.reads.setdefault(b, []).append(sv)
        for b in w:
            self.lastw[b] = sv
            self.reads[b] = []

    def _newsem(self, k):
        if k not in self.cnt:
            self.cnt[k] = 0
            self.semkeys.append(k)

    def I(self, eng, fn, r=(), w=(), ro=()):
        bk = [b for b in r if isinstance(b, tuple) and b[0] == "bank"]
        r = list(r) + list(ro)
        if bk:
            r = [b for b in r if b not in bk]
            w = list(w) + bk
        waits = self._deps(eng, r, w, skip_same_pe=(eng == "pe"))
        k = self._semkey_engine(eng)
        self._newsem(k)
        self.cnt[k] += 1
        sv = (k, self.cnt[k])
        self._commit(r, w, sv)
        self.ops[eng].append((waits, fn, k, 1))

    def DMA(self, eng, fn, r=(), w=(), ch=None):
        waits = self._deps(eng, r, w, skip_same_pe=False)
        k = ("D", ch, 0)
        n = 0
        while self.cnt.get(("D", ch, n), 0) >= 30000:
            n += 1
        k = ("D", ch, n)
        self._newsem(k)
        self.cnt[k] += 16
        sv = (k, self.cnt[k])
        self._commit(r, w, sv)
        self.ops[eng].append((waits, fn, k, 16))

    def CC(self, eng, fn, r=(), w=()):
        waits = self._deps(eng, r, w, skip_same_pe=False)
        k = ("C", "cc", 0)
        self._newsem(k)
        self.cnt[k] += 1
        sv = (k, self.cnt[k])
        self._commit(r, w, sv)
        self.ops[eng].append((waits, fn, k, 1))

    def barrier(self):
        for e in ENG:
            waits = []
            for k, v in self.cnt.items():
                if v > 0 and self.waited.get((e, k), 0) < v:
                    self.waited[(e, k)] = v
                    waits.append((k, v))
            if waits:
                self.ops[e].append((waits, None, None, 0))

    def emit(self):
        nc = self.nc
        with contextlib.ExitStack() as st:
            for i, k in enumerate(self.semkeys):
                self.sems[k] = st.enter_context(nc.semaphore("s%d" % i))
            block = st.enter_context(nc.Block())
            engobj = {"pe": "tensor", "act": "scalar", "dve": "vector", "pool": "gpsimd", "sp": "sync"}

            def run(e, ename):
                for waits, fn, k, inc in self.ops[ename]:
                    for wk, wv in waits:
                        e.wait_ge(self.sems[wk], wv)
                    if fn is not None:
                        fn(e).then_inc(self.sems[k], inc)

            @block.tensor
            def _(e):
                run(e, "pe")

            @block.scalar
            def _(e):
                run(e, "act")

            @block.vector
            def _(e):
                run(e, "dve")

            @block.gpsimd
            def _(e):
                run(e, "pool")
            @block.sync
            def _(e):
                run(e, "sp")


class Arena:
    def __init__(self, t, n):
        self.t = t
        self.n = n
        self.off = 0
        self.mark = 0

    def f32(self, cols):
        o = self.off
        self.off += cols
        assert self.off <= self.n, ("arena overflow", self.off, self.n)
        return self.t[:, o:o + cols]

    def bf16(self, cols):
        c = (cols + 1) // 2
        return self.f32(c).bitcast(BF16)

    def u32(self, cols):
        return self.f32(cols).bitcast(U32)


def build(debug=False, p2=True, nblk=16, cc=True, stop=0, ntile=16):
    nc = bass.Bass("TRN2", target_bir_lowering=False)
    P = Prog(nc)

    def din(name, shape, dt=F32):
        return nc.dram_tensor(name, list(shape), dt, kind="ExternalInput").ap()

    x_b = din("x_b", [SEQ, D])
    w1 = din("w1", [D, 1024])
    cw = din("cw", [128, 16])
    pv = din("pv", [128, 40])
    lw = din("lw", [2, 2, 64, 64])
    if not p2:
        din = lambda *a, **k: None
    mem_b = din("mem_b", [256, D])
    w_out = din("w_out", [D, D])
    xwq = din("xwq", [D, D])
    xwkv = din("xwkv", [D, 2 * D])
    xwo = din("xwo", [D, D])
    pwq = din("pwq", [D, 2 * D])
    skT = din("skT", [128, 16 * 128])
    peer_u = din("peer_u", [16384, D])
    peer_v = din("peer_v", [16384, D])
    rows = din("rows", [2, D])
    out = nc.dram_tensor("out", [TOK, D], F32, kind="ExternalOutput").ap()
    cin_t = [nc.dram_tensor("cin%d" % q, [256, 2048], BF16) for q in range(4)]
    coutall = nc.dram_tensor("coutall", [4, 1024, 2048], BF16)
    cin = [t.ap().rearrange("r (x t) -> (r x) t", t=128) for t in cin_t]
    coutflat = coutall.ap().rearrange("q r (x t) -> (q r x) t", t=128)
    uvb = nc.dram_tensor("uvb", [16384, 2 * D], BF16) if p2 else None
    ub = uvb
    idxy = din("idxy", [128, 8], U32)
    x_tok = din("x_tok", [TOK, D])
    if debug:
        dbg = nc.dram_tensor("dbg", [3, TOK, D], F32, kind="ExternalOutput").ap()

    st = contextlib.ExitStack()
    NA = 49000
    arena_t = st.enter_context(nc.sbuf_tensor("arena", [128, NA], F32))
    ps_all = st.enter_context(nc.psum_tensor("ps_all", [128, 4096], F32))
    psb = [ps_all[:, i * 512:(i + 1) * 512] for i in range(8)]

    A = Arena(arena_t, NA)
    ident = A.f32(128)
    identb = A.bf16(128)
    ones = A.f32(512)
    onesb = A.bf16(128)
    mU = A.f32(128)
    mUs = A.f32(128)
    cwt = A.f32(16)
    pvt = A.f32(40)
    A.mark = A.off

    P.I("pool", lambda e: e.memset(ones, 1.0), w=["ones"])
    P.I("pool", lambda e: e.memset(onesb, 1.0), w=["onesb"])
    P.I("pool", lambda e: e.affine_select(out=ident, in_=ones[:, 0:128], pattern=[[-1, 128]],
                                          compare_op=OP.is_equal, fill=0.0, base=0, channel_multiplier=1),
        r=["ones"], w=["ident"])
    P.I("pool", lambda e: e.tensor_copy(out=identb, in_=ident), r=["ident"], w=["identb"])
    P.I("pool", lambda e: e.affine_select(out=mU, in_=ones[:, 0:128], pattern=[[1, 128]],
                                          compare_op=OP.is_ge, fill=0.0, base=0, channel_multiplier=-1),
        r=["ones"], w=["mU"])
    P.I("pool", lambda e: e.affine_select(out=mUs, in_=ones[:, 0:128], pattern=[[1, 128]],
                                          compare_op=OP.is_gt, fill=0.0, base=0, channel_multiplier=-1),
        r=["ones"], w=["mUs"])
    P.DMA("sp", lambda e: e.dma_start(out=cwt, in_=cw[:, :]), w=["cwt"], ch="c_cwt")
    P.DMA("sp", lambda e: e.dma_start(out=pvt, in_=pv[:, :]), w=["pvt"], ch="c_pvt")

    PV_CB, PV_BA, PV_BX, PV_LAM, PV_ALOG, PV_DTB, PV_GNW = 0, 1, 2, 3, 4, 5, 6
    PV_NMIX, PV_NXA, PV_NMEM = 8, 16, 24

    def col(i):
        return pvt[:, i:i + 1]

    phase1(nc, P, A, locals())
    P.barrier()
    if cc:
      for q in range(4):
        P.CC("pool", lambda e, q=q: e.collective_compute("AllGather", OP.bypass,
                                                        replica_groups=[[0, 1, 2, 3], [4, 5, 6, 7]],
                                                        ins=[cin_t[q].ap()], outs=[coutall.ap()[q]]),
             r=["cin"], w=["cout"])
    P.barrier()
    A.off = A.mark
    if p2:
        phase2(nc, P, A, locals())
    P.barrier()
    P.emit()
    st.close()
    return nc


def phase1(nc, P, A, G):
    psb = G["psb"]; ident = G["ident"]; identb = G["identb"]; ones = G["ones"]
    mU = G["mU"]; mUs = G["mUs"]; cwt = G["cwt"]; pvt = G["pvt"]; col = G["col"]
    x_b = G["x_b"]; w1 = G["w1"]; lw = G["lw"]; cin = G["cin"]
    PV_CB, PV_BA, PV_BX, PV_LAM, PV_ALOG, PV_DTB, PV_GNW, PV_NMIX = 0, 1, 2, 3, 4, 5, 6, 8

    w1b = A.bf16(8 * 1024).rearrange("p (k c) -> p k c", k=8)
    wabd = A.f32(128)
    wxbd = A.f32(128)
    sc1 = A.f32(16)
    negsp8, negsp16, nA, dtb = sc1[:, 0:1], sc1[:, 1:2], sc1[:, 2:3], sc1[:, 3:4]
    tmpc = sc1[:, 4:6]
    xin = [A.f32(4 * 1024).rearrange("p (s d) -> p s d", s=4) for _ in range(2)]
    junkb = A.bf16(1024)
    ssq = A.f32(4); msq = A.f32(4); rstd = A.f32(4)
    xn = A.bf16(4 * 1024).rearrange("p (s d) -> p s d", s=4)
    hT = A.bf16(8 * 512).rearrange("p (k t) -> p k t", k=8)
    cb = [A.f32(515) for _ in range(4)]
    cy = [A.f32(512) for _ in range(4)]
    qs = A.f32(512); ks = A.f32(512); vs = A.f32(512)
    gg = A.f32(512); zs = A.f32(512); grow = A.f32(512); brow = A.f32(512)
    t512 = [A.f32(512) for _ in range(8)]
    qn = A.f32(512); kn = A.f32(512)
    hprev = A.f32(1)
    ystage = [A.bf16(2 * 512).rearrange("p (g t) -> p g t", g=2) for _ in range(2)]
    S = [A.f32(128) for _ in range(2)]
    NCH = 4
    HB = {nm: [buf, A.f32(512)] for nm, buf in (("qn", qn), ("kn", kn), ("vs", vs), ("grow", grow), ("brow", brow), ("zs", zs))}
    def cbufs():
        d = {}
        for nm in ["gcum", "arg", "DT", "eg", "ekd", "t1", "t2", "t3", "kbgT", "vbT", "qdT", "kdT",
                   "B", "Am", "M", "B2", "A2", "kbg", "vb", "kd", "wT", "u", "attnT", "vnew", "on"]:
            d[nm] = A.f32(128)
        d["small"] = A.f32(8)
        return d
    CB = [cbufs() for _ in range(NCH)]
    pTs = [psb[6][:, :].bitcast(BF16), psb[7][:, :].bitcast(BF16)]

    P.DMA("pool", lambda e: e.dma_start(out=w1b, in_=w1.rearrange("(k p) c -> p k c", p=128)), w=["w1b"], ch="w1")
    P.I("pool", lambda e: e.memset(wabd, 0.0), w=["wabd"])
    P.I("pool", lambda e: e.memset(wxbd, 0.0), w=["wxbd"])
    for i in range(2):
        P.DMA("sp", lambda e, i=i: e.dma_start(out=wabd[i * 64:(i + 1) * 64, i * 64:(i + 1) * 64], in_=lw[0, i]), w=["wabd"], ch="c_wabd")
        P.DMA("sp", lambda e, i=i: e.dma_start(out=wxbd[i * 64:(i + 1) * 64, i * 64:(i + 1) * 64], in_=lw[1, i]), w=["wxbd"], ch="c_wxbd")
    P.I("act", lambda e: e.activation(out=tmpc[:, 0:1], in_=col(PV_LAM), func=AF.Exp, scale=-1.0), r=["pvt"], w=["tmpc"])
    P.I("act", lambda e: e.activation(out=tmpc[:, 1:2], in_=tmpc[:, 0:1], func=AF.Ln, bias=1.0), r=["tmpc"], w=["tmpc2"])
    P.I("dve", lambda e: e.tensor_scalar(out=negsp8, in0=tmpc[:, 1:2], scalar1=-8.0, scalar2=None, op0=OP.mult), r=["tmpc2"], w=["negsp8"])
    P.I("dve", lambda e: e.tensor_scalar(out=negsp16, in0=tmpc[:, 1:2], scalar1=-16.0, scalar2=None, op0=OP.mult), r=["tmpc2"], w=["negsp16"])
    P.I("act", lambda e: e.activation(out=nA, in_=col(PV_ALOG), func=AF.Exp), r=["pvt"], w=["nA0"])
    P.I("dve", lambda e: e.tensor_scalar(out=nA, in0=nA, scalar1=-1.0, scalar2=None, op0=OP.mult), r=["nA0"], w=["nA"])
    for c in range(4):
        P.I("pool", lambda e, c=c: e.memset(cb[c][:, 0:3], 0.0), w=[("cbh", c)])
    P.I("pool", lambda e: e.memset(S[0], 0.0), w=[("S", 0)])
    P.I("pool", lambda e: e.memset(hprev, 0.0), w=["hprev"])

    NBLK = G['nblk']
    stop = G['stop']
    ps_rot = [0]

    def load_x(blk, DMA):
        s = blk % 2
        DMA("sp", lambda e: e.dma_start(out=xin[s], in_=x_b[blk * 512:(blk + 1) * 512, :].rearrange("(s p) d -> p s d", p=128)),
              w=[("xin", s)], ch=("xin", s))

    import collections
    pending = collections.deque()

    def Idef(*a, **k):
        pending.append(("I", a, k))

    def DMAdef(*a, **k):
        pending.append(("DMA", a, k))

    cnt = [0]

    def CI(*a, **k):
        P.I(*a, **k)
        cnt[0] += 1
        if cnt[0] % 3 != 0 and pending:
            kind, a2, k2 = pending.popleft()
            getattr(P, kind)(*a2, **k2)

    def front(blk, I, DMA):
        sl = blk % 2
        par = blk % 2
        qn, kn, vs, grow, brow, zs = (HB[nm][par] for nm in ("qn", "kn", "vs", "grow", "brow", "zs"))
        if G["ub"] is not None:
            for ck in range(blk * 16 // NBLK, (blk + 1) * 16 // NBLK):
                for (src, c0) in ((G["peer_u"], 0), (G["peer_v"], 1024)):
                    DMA("pool", lambda e, ck=ck, src=src, c0=c0: e.dma_start(out=G["uvb"].ap()[ck * 1024:(ck + 1) * 1024, c0:c0 + 1024], in_=src[ck * 1024:(ck + 1) * 1024, :]),
                          w=["uvb"], ch="cvt")
        if blk + 1 < NBLK:
            load_x(blk + 1, DMA)
        X = xin[sl]
        for s in range(4):
            I("act", lambda e, s=s, X=X: e.activation(out=junkb, in_=X[:, s, :], func=AF.Square, accum_out=ssq[:, s:s + 1]),
                r=[("xin", sl)], w=["junkb", ("ssq", s)])
        I("dve", lambda e: e.tensor_scalar(out=msq, in0=ssq, scalar1=1.0 / D, scalar2=EPS, op0=OP.mult, op1=OP.add),
            r=[("ssq", s) for s in range(4)], w=["msq"])
        I("act", lambda e: e.activation(out=msq, in_=msq, func=AF.Sqrt), r=["msq"], w=["msq"])
        I("dve", lambda e: e.reciprocal(out=rstd, in_=msq), r=["msq"], w=["rstd"])
        for s in range(4):
            eng = "dve" if s % 2 == 0 else "pool"
            I(eng, lambda e, s=s, X=X: e.tensor_scalar(out=xn[:, s, :], in0=X[:, s, :], scalar1=rstd[:, s:s + 1], scalar2=None, op0=OP.mult),
                r=[("xin", sl), "rstd"], w=[("xn", s)])
        for k in range(8):
            half = k % 2
            for s in range(4):
                I("pe", lambda e, k=k, s=s, half=half: e.transpose(out=pTs[half][:, s * 128:(s + 1) * 128],
                                                                     in_=xn[:, s, k * 128:(k + 1) * 128], identity=identb),
                    r=[("xn", s), "identb"], w=[("bank", 6 + half)])
            if False:
                I("act", lambda e, k=k, half=half: e.activation(out=hT[:, k, :], in_=pTs[half][:, 0:512], func=AF.Copy,
                                                                  scale=col(PV_NMIX + k)),
                    r=[("bank", 6 + half), "pvt"], w=[("hT", k)])
            else:
                I("dve", lambda e, k=k, half=half: e.tensor_scalar(out=hT[:, k, :], in0=pTs[half][:, 0:512],
                                                                     scalar1=col(PV_NMIX + k), scalar2=None, op0=OP.mult),
                    r=[("bank", 6 + half), "pvt"], w=[("hT", k)])
        for c in range(8):
            pb = ps_rot[0] % 2
            ps_rot[0] += 1
            pst = psb[pb]
            for k in range(8):
                I("pe", lambda e, c=c, k=k, pst=pst: e.matmul(pst[:, :], lhsT=w1b[:, k, c * 128:(c + 1) * 128], rhs=hT[:, k, :],
                                                                start=(k == 0), stop=(k == 7)),
                    r=["w1b", ("hT", k)], w=[("bank", pb)])
            if c < 4:
                j = c
                if blk > 0:
                    I("dve", lambda e, j=j: e.tensor_copy(out=cb[j][:, 0:3], in_=cb[j][:, 512:515]), r=[("cb", j)], w=[("cbh", j)])
                I("act", lambda e, j=j, pst=pst: e.activation(out=cb[j][:, 3:515], in_=pst[:, :], func=AF.Copy),
                    r=[("bank", pb), ("cbh", j)], w=[("cb", j)])
            elif c == 4:
                I("act", lambda e, pst=pst: e.activation(out=gg, in_=pst[:, :], func=AF.Gelu_apprx_tanh), r=[("bank", pb)], w=["gg"])
            elif c == 5:
                I("act", lambda e, pst=pst: e.activation(out=zs, in_=pst[:, :], func=AF.Silu), r=[("bank", pb)], w=[("zs", par)])
            elif c == 6:
                I("act", lambda e, pst=pst: e.activation(out=grow, in_=pst[:, :], func=AF.Exp, bias=col(PV_DTB)), r=[("bank", pb), "pvt"], w=[("grow", par)])
                I("act", lambda e: e.activation(out=grow, in_=grow, func=AF.Ln, bias=1.0), r=[("grow", par)], w=[("grow", par)])
                I("dve", lambda e: e.tensor_scalar(out=grow, in0=grow, scalar1=nA, scalar2=None, op0=OP.mult), r=[("grow", par), "nA"], w=[("grow", par)])
            else:
                I("act", lambda e, pst=pst: e.activation(out=brow, in_=pst[:, :], func=AF.Sigmoid), r=[("bank", pb)], w=[("brow", par)])
        for j in range(4):
            eng = "dve" if j % 2 == 0 else "pool"
            if j == 3:
                I("dve", lambda e, j=j: e.tensor_scalar(out=cy[j], in0=cb[j][:, 0:512], scalar1=cwt[:, 4 * j:4 * j + 1], scalar2=col(PV_CB),
                                                          op0=OP.mult, op1=OP.add), r=[("cb", j), ("cbh", j), "cwt", "pvt"], w=[("cy", j)])
            else:
                I("dve", lambda e, j=j: e.tensor_scalar(out=cy[j], in0=cb[j][:, 0:512], scalar1=cwt[:, 4 * j:4 * j + 1], scalar2=None,
                                                          op0=OP.mult), r=[("cb", j), ("cbh", j), "cwt"], w=[("cy", j)])
            for tp in range(1, 4):
                I("dve", lambda e, j=j, tp=tp: e.scalar_tensor_tensor(out=cy[j], in0=cb[j][:, tp:tp + 512], scalar=cwt[:, 4 * j + tp:4 * j + tp + 1],
                                                                         in1=cy[j], op0=OP.mult, op1=OP.add),
                    r=[("cb", j), ("cbh", j), "cwt", ("cy", j)], w=[("cy", j)])
        I("act", lambda e: e.activation(out=qs, in_=cy[0], func=AF.Silu), r=[("cy", 0)], w=["qs"])
        I("act", lambda e: e.activation(out=ks, in_=cy[1], func=AF.Silu), r=[("cy", 1)], w=["ks"])
        I("act", lambda e: e.activation(out=vs, in_=cy[2], func=AF.Silu), r=[("cy", 2)], w=[("vs", par)])
        xrc = cy[3]
        r_, i_, a_, a2_, ix_, hs_ = t512[0], t512[1], t512[2], t512[3], t512[4], t512[5]
        I("pe", lambda e: e.matmul(psb[2][:, :], lhsT=wabd, rhs=xrc, start=True, stop=True), r=["wabd", ("cy", 3)], w=[("bank", 2)])
        I("act", lambda e: e.activation(out=r_, in_=psb[2][:, :], func=AF.Sigmoid, bias=col(PV_BA)), r=[("bank", 2), "pvt"], w=["r_"])
        I("pe", lambda e: e.matmul(psb[2][:, :], lhsT=wxbd, rhs=xrc, start=True, stop=True), r=["wxbd", ("cy", 3)], w=[("bank", 2)])
        I("act", lambda e: e.activation(out=i_, in_=psb[2][:, :], func=AF.Sigmoid, bias=col(PV_BX)), r=[("bank", 2), "pvt"], w=["i_"])
        I("act", lambda e: e.activation(out=a_, in_=r_, func=AF.Exp, scale=negsp8), r=["r_", "negsp8"], w=["a_"])
        I("act", lambda e: e.activation(out=a2_, in_=r_, func=AF.Exp, scale=negsp16), r=["r_", "negsp16"], w=["a2_"])
        I("dve", lambda e: e.tensor_scalar(out=a2_, in0=a2_, scalar1=-1.0, scalar2=1.0, op0=OP.mult, op1=OP.add), r=["a2_"], w=["a2_"])
        I("act", lambda e: e.activation(out=a2_, in_=a2_, func=AF.Sqrt), r=["a2_"], w=["a2_"])
        I("pool", lambda e: e.tensor_tensor(out=ix_, in0=i_, in1=xrc, op=OP.mult), r=["i_", ("cy", 3)], w=["ix_"])
        I("pool", lambda e: e.tensor_tensor(out=ix_, in0=ix_, in1=a2_, op=OP.mult), r=["ix_", "a2_"], w=["ix_"])
        I("dve", lambda e: e.tensor_tensor_scan(out=hs_, data0=a_, data1=ix_, initial=hprev[:, 0:1], op0=OP.mult, op1=OP.add),
            r=["a_", "ix_", "hprev"], w=["hs_"])
        I("dve", lambda e: e.tensor_copy(out=hprev, in_=hs_[:, 511:512]), r=["hs_"], w=["hprev"])
        I("pool", lambda e, sl=sl: e.tensor_tensor(out=ystage[sl][:, 1, :], in0=hs_, in1=gg, op=OP.mult), r=["hs_", "gg"], w=[("ystage", sl)])
        sq_, rq_ = t512[6], t512[7]
        for (src, dst, scl, nm) in ((qs, qn, 128.0 ** -0.5, ("qn", par)), (ks, kn, 1.0, ("kn", par))):
            I("pool", lambda e, src=src: e.tensor_tensor(out=sq_, in0=src, in1=src, op=OP.mult), r=["qs", "ks"], w=["sq_"])
            I("pe", lambda e: e.matmul(psb[2][:, :], lhsT=ones[:, 0:128], rhs=sq_, start=True, stop=True), r=["ones", "sq_"], w=[("bank", 2)])
            I("act", lambda e: e.activation(out=rq_, in_=psb[2][:, :], func=AF.Sqrt, bias=EPS), r=[("bank", 2)], w=["rq_"])
            I("dve", lambda e: e.reciprocal(out=rq_, in_=rq_), r=["rq_"], w=["rq_"])
            I("dve", lambda e, src=src, dst=dst, scl=scl: e.scalar_tensor_tensor(out=dst, in0=src, scalar=scl, in1=rq_, op0=OP.mult, op1=OP.mult),
                r=["qs", "ks", "rq_"], w=[nm])

    def chunks(blk):
        sl = blk % 2
        par = blk % 2
        qn, kn, vs, grow, brow, zs = (HB[nm][par] for nm in ("qn", "kn", "vs", "grow", "brow", "zs"))
        slots = [(3, 0), (4, 0), (3, 1), (4, 1), (3, 2), (4, 2), (3, 3), (4, 3)]
        sr = [0]

        def pslot():
            b, q = slots[sr[0] % len(slots)]
            sr[0] += 1
            return psb[b][:, q * 128:(q + 1) * 128], ("bank", b)

        def K(ch, nm):
            return (nm, ch)

        for ch in range(NCH):
            c = CB[ch]
            cs = slice(ch * 128, (ch + 1) * 128)
            sm = c["small"]
            gcol, ngcol, gl, dcol = sm[:, 0:1], sm[:, 1:2], sm[:, 2:3], sm[:, 3:4]
            CI("dve", lambda e, c=c, cs=cs: e.tensor_tensor_scan(out=c["gcum"], data0=ones[:, 0:128], data1=grow[:, cs], initial=0.0,
                                                                   op0=OP.mult, op1=OP.add), r=[("grow", par), "ones"], w=[K(ch, "gcum")])
            pt, pk = pslot()
            CI("pe", lambda e, c=c, pt=pt: e.matmul(pt, lhsT=c["gcum"], rhs=ident, start=True, stop=True), r=[K(ch, "gcum"), "ident"], w=[pk])
            CI("dve", lambda e, pt=pt, ngcol=ngcol: e.tensor_scalar(out=ngcol, in0=pt[:, 0:1], scalar1=-1.0, scalar2=None, op0=OP.mult),
                r=[pk], w=[K(ch, "ngcol")])
            CI("dve", lambda e, c=c, ngcol=ngcol: e.tensor_scalar(out=c["arg"], in0=c["gcum"], scalar1=ngcol, scalar2=0.0, op0=OP.add, op1=OP.min),
                r=[K(ch, "gcum"), K(ch, "ngcol")], w=[K(ch, "arg")])
            CI("act", lambda e, c=c: e.activation(out=c["DT"], in_=c["arg"], func=AF.Exp), r=[K(ch, "arg")], w=[K(ch, "DT")])
            CI("act", lambda e, c=c: e.activation(out=c["eg"], in_=c["gcum"], func=AF.Exp), r=[K(ch, "gcum")], w=[K(ch, "eg")])
            CI("act", lambda e, c=c: e.activation(out=c["ekd"], in_=c["gcum"], func=AF.Exp, scale=-1.0, bias=c["gcum"][:, 127:128]),
                r=[K(ch, "gcum")], w=[K(ch, "ekd")])
            CI("act", lambda e, c=c, dcol=dcol: e.activation(out=dcol, in_=c["gcum"][:, 127:128], func=AF.Exp), r=[K(ch, "gcum")], w=[K(ch, "dcol")])
            CI("pool", lambda e, c=c: e.tensor_tensor(out=c["t1"], in0=c["DT"], in1=mUs, op=OP.mult), r=[K(ch, "DT"), "mUs"], w=[K(ch, "t1")])
            CI("pool", lambda e, c=c, cs=cs: e.tensor_tensor(out=c["t2"], in0=c["t1"], in1=brow[:, cs], op=OP.mult), r=[K(ch, "t1"), ("brow", par)], w=[K(ch, "t2")])
            CI("pool", lambda e, c=c: e.tensor_tensor(out=c["t3"], in0=c["DT"], in1=mU, op=OP.mult), r=[K(ch, "DT"), "mU"], w=[K(ch, "t3")])
            CI("dve", lambda e, c=c, cs=cs: e.tensor_tensor(out=c["vbT"], in0=vs[:, cs], in1=brow[:, cs], op=OP.mult), r=[("vs", par), ("brow", par)], w=[K(ch, "vbT")])
            CI("dve", lambda e, c=c, cs=cs: e.tensor_tensor(out=c["kbgT"], in0=kn[:, cs], in1=brow[:, cs], op=OP.mult), r=[("kn", par), ("brow", par)], w=[K(ch, "kbgT")])
            CI("dve", lambda e, c=c: e.tensor_tensor(out=c["kbgT"], in0=c["kbgT"], in1=c["eg"], op=OP.mult), r=[K(ch, "kbgT"), K(ch, "eg")], w=[K(ch, "kbgT")])
            CI("pool", lambda e, c=c, cs=cs: e.tensor_tensor(out=c["qdT"], in0=qn[:, cs], in1=c["eg"], op=OP.mult), r=[("qn", par), K(ch, "eg")], w=[K(ch, "qdT")])
            CI("pool", lambda e, c=c, cs=cs: e.tensor_tensor(out=c["kdT"], in0=kn[:, cs], in1=c["ekd"], op=OP.mult), r=[("kn", par), K(ch, "ekd")], w=[K(ch, "kdT")])
            pt, pk = pslot()
            CI("pe", lambda e, cs=cs, pt=pt: e.matmul(pt, lhsT=kn[:, cs], rhs=kn[:, cs], start=True, stop=True), r=[("kn", par)], w=[pk])
            CI("dve", lambda e, c=c, pt=pt: e.tensor_tensor(out=c["B"], in0=pt, in1=c["t2"], op=OP.mult), r=[pk, K(ch, "t2")], w=[K(ch, "B")])
            pt, pk = pslot()
            CI("pe", lambda e, cs=cs, pt=pt: e.matmul(pt, lhsT=kn[:, cs], rhs=qn[:, cs], start=True, stop=True), r=[("kn", par), ("qn", par)], w=[pk])
            CI("dve", lambda e, c=c, pt=pt: e.tensor_tensor(out=c["attnT"], in0=pt, in1=c["t3"], op=OP.mult), r=[pk, K(ch, "t3")], w=[K(ch, "attnT")])
            pt, pk = pslot()
            CI("pe", lambda e, c=c, pt=pt: e.matmul(pt, lhsT=c["B"], rhs=ident, start=True, stop=True), r=[K(ch, "B"), "ident"], w=[pk])
            CI("act", lambda e, c=c, pt=pt: e.activation(out=c["Am"], in_=pt, func=AF.Copy), r=[pk], w=[K(ch, "Am")])
            CI("dve", lambda e, c=c: e.tensor_tensor(out=c["M"], in0=ident, in1=c["B"], op=OP.subtract), r=["ident", K(ch, "B")], w=[K(ch, "M")])
            for (srcn, dstn) in (("kbgT", "kbg"), ("vbT", "vb"), ("kdT", "kd")):
                pt, pk = pslot()
                CI("pe", lambda e, c=c, pt=pt, srcn=srcn: e.matmul(pt, lhsT=c[srcn], rhs=ident, start=True, stop=True), r=[K(ch, srcn), "ident"], w=[pk])
                CI("act", lambda e, c=c, pt=pt, dstn=dstn: e.activation(out=c[dstn], in_=pt, func=AF.Copy), r=[pk], w=[K(ch, dstn)])
        cur = [("Am", "B")] * NCH
        for lev in range(6):
            for ch in range(NCH):
                c = CB[ch]
                an, bn = cur[ch]
                na, nb = ("A2", "B2") if an == "Am" else ("Am", "B")
                pt, pk = pslot()
                CI("pe", lambda e, c=c, pt=pt, an=an, bn=bn: e.matmul(pt, lhsT=c[bn], rhs=c[an], start=True, stop=True),
                    r=[K(ch, an), K(ch, bn)], w=[pk])
                pt2, pk2 = (None, None)
                if lev < 5:
                    pt2, pk2 = pslot()
                    CI("pe", lambda e, c=c, pt2=pt2, an=an, bn=bn: e.matmul(pt2, lhsT=c[an], rhs=c[bn], start=True, stop=True),
                        r=[K(ch, an), K(ch, bn)], w=[pk2])
                CI("act", lambda e, c=c, pt=pt, na=na: e.activation(out=c[na], in_=pt, func=AF.Copy), r=[pk], w=[K(ch, na)])
                if lev < 5:
                    CI("dve", lambda e, c=c, pt2=pt2, nb=nb: e.tensor_copy(out=c[nb], in_=pt2), r=[pk2], w=[K(ch, nb)])
                pt3, pk3 = pslot()
                CI("pe", lambda e, c=c, pt3=pt3, na=na: e.matmul(pt3, lhsT=c[na], rhs=c["M"], start=True, stop=True),
                    r=[K(ch, na), K(ch, "M")], w=[pk3])
                CI("dve", lambda e, c=c, pt3=pt3: e.tensor_tensor(out=c["M"], in0=pt3, in1=c["M"], op=OP.add), r=[pk3, K(ch, "M")], w=[K(ch, "M")])
                cur[ch] = (na, nb)
        for ch in range(NCH):
            c = CB[ch]
            pt, pk = pslot()
            CI("pe", lambda e, c=c, pt=pt: e.matmul(pt, lhsT=c["kbg"], rhs=c["M"], start=True, stop=True), r=[K(ch, "kbg"), K(ch, "M")], w=[pk])
            CI("act", lambda e, c=c, pt=pt: e.activation(out=c["wT"], in_=pt, func=AF.Copy), r=[pk], w=[K(ch, "wT")])
            pt, pk = pslot()
            CI("pe", lambda e, c=c, pt=pt: e.matmul(pt, lhsT=c["M"], rhs=c["vb"], start=True, stop=True), r=[K(ch, "vb"), K(ch, "M")], w=[pk])
            CI("act", lambda e, c=c, pt=pt: e.activation(out=c["u"], in_=pt, func=AF.Copy), r=[pk], w=[K(ch, "u")])
        for ch in range(NCH):
            c = CB[ch]
            cs = slice(ch * 128, (ch + 1) * 128)
            n = blk * NCH + ch
            Sc, Sn = S[n % 2], S[(n + 1) % 2]
            kSc, kSn = ("S", n % 2), ("S", (n + 1) % 2)
            sm = c["small"]
            dcol = sm[:, 3:4]; osq = sm[:, 4:5]; orstd = sm[:, 5:6]
            p_ws, p_o, p_ks, p_t = (psb[5][:, q * 128:(q + 1) * 128] for q in range(4))
            CI("pe", lambda e, c=c, Sc=Sc, p_ws=p_ws: e.matmul(p_ws, lhsT=c["wT"], rhs=Sc, start=True, stop=True), r=[K(ch, "wT"), kSc], w=[("bank", 5)])
            CI("dve", lambda e, c=c, p_ws=p_ws: e.tensor_tensor(out=c["vnew"], in0=c["u"], in1=p_ws, op=OP.subtract), r=[K(ch, "u"), ("bank", 5)], w=[K(ch, "vnew")])
            CI("pe", lambda e, c=c, Sc=Sc, p_o=p_o: e.matmul(p_o, lhsT=c["qdT"], rhs=Sc, start=True, stop=False), r=[K(ch, "qdT"), kSc, K(ch, "vnew"), K(ch, "attnT")], w=[("bank", 5)])
            CI("pe", lambda e, c=c, p_o=p_o: e.matmul(p_o, lhsT=c["attnT"], rhs=c["vnew"], start=False, stop=True), r=[K(ch, "attnT"), K(ch, "vnew")], w=[("bank", 5)])
            CI("pe", lambda e, c=c, p_ks=p_ks: e.matmul(p_ks, lhsT=c["kd"], rhs=c["vnew"], start=True, stop=True), r=[K(ch, "kd"), K(ch, "vnew")], w=[("bank", 5)])
            CI("dve", lambda e, Sc=Sc, Sn=Sn, dcol=dcol, p_ks=p_ks: e.scalar_tensor_tensor(out=Sn, in0=Sc, scalar=dcol, in1=p_ks, op0=OP.mult, op1=OP.add),
                r=[kSc, K(ch, "dcol"), ("bank", 5)], w=[kSn])
            CI("act", lambda e, c=c, p_o=p_o, osq=osq: e.activation(out=c["on"], in_=p_o, func=AF.Copy), r=[("bank", 5)], w=[K(ch, "on")])
            CI("act", lambda e, c=c, osq=osq: e.activation(out=c["t1"], in_=c["on"], func=AF.Square, accum_out=osq), r=[K(ch, "on")], w=[K(ch, "t1"), K(ch, "osq")])
            CI("dve", lambda e, osq=osq, orstd=orstd: e.tensor_scalar(out=orstd, in0=osq, scalar1=1.0 / 128, scalar2=EPS, op0=OP.mult, op1=OP.add), r=[K(ch, "osq")], w=[K(ch, "orstd")])
            CI("act", lambda e, orstd=orstd: e.activation(out=orstd, in_=orstd, func=AF.Sqrt), r=[K(ch, "orstd")], w=[K(ch, "orstd")])
            CI("dve", lambda e, orstd=orstd: e.reciprocal(out=orstd, in_=orstd), r=[K(ch, "orstd")], w=[K(ch, "orstd")])
            CI("dve", lambda e, c=c, orstd=orstd: e.tensor_scalar(out=c["t2"], in0=c["on"], scalar1=orstd, scalar2=None, op0=OP.mult), r=[K(ch, "on"), K(ch, "orstd")], w=[K(ch, "t2")])
            pt, pk = pslot()
            CI("pe", lambda e, c=c, pt=pt: e.matmul(pt, lhsT=c["t2"], rhs=ident, start=True, stop=True), r=[K(ch, "t2"), "ident"], w=[pk])
            CI("dve", lambda e, pt=pt, cs=cs, sl=sl: e.scalar_tensor_tensor(out=ystage[sl][:, 0, cs], in0=pt, scalar=col(PV_GNW), in1=zs[:, cs], op0=OP.mult, op1=OP.mult),
                r=[pk, "pvt", ("zs", par)], w=[("ystage", sl)])
        for g in range(2):
            P.DMA("sp", lambda e, blk=blk, sl=sl, g=g: e.dma_start(
                out=cin[blk // 4].rearrange("(i g p) t -> p i g t", i=16, g=2)[:, (blk % 4) * 4:(blk % 4) * 4 + 4, g, :],
                in_=ystage[sl][:, g, :].rearrange("p (i t) -> p i t", i=4)),
                  r=[("ystage", sl)], w=["cin"], ch=("yst", sl))


    load_x(0, P.DMA)
    front(0, P.I, P.DMA)
    for blk in range(NBLK):
        if blk + 1 < NBLK:
            front(blk + 1, Idef, DMAdef)
        chunks(blk)
        while pending:
            kind, a2, k2 = pending.popleft()
            getattr(P, kind)(*a2, **k2)


def phase2(nc, P, A, G):
    psb = G["psb"]; ident = G["ident"]; identb = G["identb"]; ones = G["ones"]; onesb = G["onesb"]
    pvt = G["pvt"]; col = G["col"]; debug = G["debug"]
    PV_NXA, PV_NMEM = 16, 24
    x_tok = G["x_tok"]; mem_b = G["mem_b"]; coutflat = G["coutflat"]; idxy = G["idxy"]
    NT = G.get("ntile", 16)

    def wload(dram, ncols, key):
        t = A.bf16(8 * ncols).rearrange("p (k c) -> p k c", k=8)
        for k in range(8):
            P.DMA("pool", lambda e, k=k, t=t: e.dma_start(out=t[:, k, :], in_=dram[k * 128:(k + 1) * 128, :]), w=[key], ch=key)
        return t
    woutb = wload(G["w_out"], 1024, "woutb")
    wqb = wload(G["xwq"], 1024, "wqb")
    wob = wload(G["xwo"], 1024, "wob")
    big = A.bf16(8 * 2048).rearrange("p (k c) -> p k c", k=8)
    for k in range(8):
        P.DMA("pool", lambda e, k=k: e.dma_start(out=big[:, k, :], in_=G["xwkv"][k * 128:(k + 1) * 128, :]), w=["big"], ch="big")
    skt = A.f32(2048).rearrange("p (c k) -> p c k", c=16)
    P.DMA("sp", lambda e: e.dma_start(out=skt, in_=G["skT"].rearrange("p (c k) -> p c k", c=16)), w=["skt"], ch="skt")
    iy = A.u32(8)
    P.DMA("sp", lambda e: e.dma_start(out=iy, in_=idxy[:, :]), w=["iy"], ch="iy")
    h3acc = A.f32(2048)
    rowt = h3acc
    P.DMA("sp", lambda e: e.dma_start(out=rowt[0:1, :], in_=G["rows"].rearrange("a d -> (a d)").unsqueeze(0)), w=["rowt"], ch="rowt")
    wbc = A.f32(2048)
    for i in range(4):
        P.I("pe", lambda e, i=i: e.matmul(psb[0][:, :], lhsT=ones[0:1, 0:128], rhs=rowt[0:1, i * 512:(i + 1) * 512], start=True, stop=True),
            r=["ones", "rowt"], w=[("bank", 0)])
        P.I("act", lambda e, i=i: e.activation(out=wbc[:, i * 512:(i + 1) * 512], in_=psb[0][:, :], func=AF.Copy), r=[("bank", 0)], w=["wbc"])
    iota16 = A.f32(256)
    P.I("pool", lambda e: e.iota(iota16, pattern=[[1, 256]], base=0, channel_multiplier=0, allow_small_or_imprecise_dtypes=True), w=["iota"])

    kT = A.bf16(8 * 256).rearrange("p (c m) -> p c m", c=8)
    vv = A.bf16(2 * 1024).rearrange("p (m c) -> p m c", m=2)
    xt = A.f32(1024); junk = A.f32(1024); h3 = h3acc[:, 0:1024]; acc = h3acc[:, 1024:2048]
    xnb = A.bf16(1024)
    hT = A.bf16(8 * 128).rearrange("p (k t) -> p k t", k=8)
    yT = A.bf16(8 * 128).rearrange("p (k t) -> p k t", k=8)
    qT = A.bf16(8 * 128).rearrange("p (k t) -> p k t", k=8)
    oT = A.bf16(8 * 128).rearrange("p (k t) -> p k t", k=8)
    expT = A.bf16(2 * 128).rearrange("p (m t) -> p m t", m=2)
    rden = A.f32(128)
    sm = A.f32(8)
    qsall = A.f32(4096)
    qpT = qsall[:, 0:2048].rearrange("p (c t) -> p c t", c=16)
    scs = qsall[:, 2048:4096].rearrange("p (c k) -> p c k", c=16)
    sct = A.f32(128)
    tv = A.f32(256).rearrange("p (c k) -> p c k", c=16)
    tiu = A.u32(256).rearrange("p (c k) -> p c k", c=16)
    tif = A.f32(256).rearrange("p (c k) -> p c k", c=16)
    cand = A.f32(256); cand2 = A.f32(256); cidx = A.f32(256)
    best = A.f32(16); posu = A.u32(16); posf = A.f32(16)
    oh = qsall.rearrange("p (k a) -> p k a", k=16)
    idxf = A.f32(128); idxu = A.u32(128); gate = A.f32(128); sval = A.f32(128); act = A.f32(128)
    NB = 5
    gb = [A.bf16(2048) for _ in range(NB)]
    junkb2 = A.bf16(1024)
    ps_all = G["ps_all"]
    h3p = ps_all[:, 0:1024]
    accp = [ps_all[:, 1024:2048], ps_all[:, 2048:3072]]
    accb = [[("bank", 2), ("bank", 3)], [("bank", 4), ("bank", 5)]]
    print("phase2 arena used", A.off, "of", A.n)
    pTb = psb[2][:, :].bitcast(BF16)

    def rms_to_hT(src, wcol0, key_src):
        P.I("act", lambda e: e.activation(out=junk, in_=src, func=AF.Square, accum_out=sm[:, 0:1]), r=[key_src], w=["junk", "sm0"])
        P.I("dve", lambda e: e.tensor_scalar(out=sm[:, 1:2], in0=sm[:, 0:1], scalar1=1.0 / D, scalar2=EPS, op0=OP.mult, op1=OP.add), r=["sm0"], w=["sm1"])
        P.I("act", lambda e: e.activation(out=sm[:, 1:2], in_=sm[:, 1:2], func=AF.Sqrt), r=["sm1"], w=["sm1"])
        P.I("dve", lambda e: e.reciprocal(out=sm[:, 2:3], in_=sm[:, 1:2]), r=["sm1"], w=["rstd"])
        if wcol0 is None:
            return
        P.I("dve", lambda e: e.tensor_scalar(out=xnb, in0=src, scalar1=sm[:, 2:3], scalar2=None, op0=OP.mult), r=[key_src, "rstd"], w=["xnb"])
        for k in range(8):
            P.I("pe", lambda e, k=k: e.transpose(out=pTb[:, (k % 4) * 128:(k % 4 + 1) * 128], in_=xnb[:, k * 128:(k + 1) * 128], identity=identb),
                r=["xnb", "identb"], w=[("bank", 2)])
            P.I("dve", lambda e, k=k: e.tensor_scalar(out=hT[:, k, :], in0=pTb[:, (k % 4) * 128:(k % 4 + 1) * 128], scalar1=col(wcol0 + k), scalar2=None, op0=OP.mult),
                r=[("bank", 2), "pvt"], w=["hT"])

    for mt in range(2):
        P.DMA("sp", lambda e, mt=mt: e.dma_start(out=xt, in_=mem_b[mt * 128:(mt + 1) * 128, :]), w=[("xt", 0)], ch="xtmem")
        rms_to_hT(xt, PV_NMEM, ("xt", 0))
        for half in range(2):
            for k in range(8):
                P.I("pe", lambda e, k=k, half=half: e.matmul(psb[half][:, :], lhsT=hT[:, k, :], rhs=big[:, k, 1024 + half * 512:1024 + (half + 1) * 512],
                                                             start=(k == 0), stop=(k == 7)), r=["hT", "big"], w=[("bank", half)])
            P.I("act", lambda e, half=half, mt=mt: e.activation(out=vv[:, mt, half * 512:(half + 1) * 512], in_=psb[half][:, :], func=AF.Copy), r=[("bank", half)], w=["vv"])
        for c in range(8):
            pb = 3 + c % 2
            for k in range(8):
                P.I("pe", lambda e, k=k, c=c, pb=pb: e.matmul(psb[pb][:, 0:128], lhsT=big[:, k, c * 128:(c + 1) * 128], rhs=hT[:, k, :], start=(k == 0), stop=(k == 7)),
                    r=["hT", "big"], w=[("bank", pb)])
            P.I("act", lambda e, c=c, pb=pb, mt=mt: e.activation(out=kT[:, c, mt * 128:(mt + 1) * 128], in_=psb[pb][:, 0:128], func=AF.Copy), r=[("bank", pb)], w=["kT"])
    for k in range(8):
        P.DMA("pool", lambda e, k=k: e.dma_start(out=big[:, k, :], in_=G["pwq"][k * 128:(k + 1) * 128, :]), r=[], w=["big"], ch="big2")

    cflat = coutflat
    import collections
    xts = [xt, A.f32(1024)]
    gates = [gate, A.f32(128)]
    idxus = [idxu, A.u32(128)]
    smA = sm
    smG = A.f32(8)
    accp1 = accp[0]
    accbk = accb[0]
    pT6 = psb[6][:, :].bitcast(BF16)
    print("phase2 arena used (pipelined)", A.off, "of", A.n)
    P.I("pe", lambda e: e.matmul(psb[4][:, 0:8], lhsT=ones[0:1, 0:128], rhs=ones[0:1, 0:8], start=True, stop=True),
        r=["woutb", "wqb", "wob", "big"], w=["wts", ("bank", 4)])

    def rms_stats(I, src, key_src, smx, tag, jout, jkeys):
        I("act", lambda e: e.activation(out=jout, in_=src, func=AF.Square, accum_out=smx[:, 0:1]), r=[key_src], w=list(jkeys) + [tag + "0"])
        I("dve", lambda e: e.tensor_scalar(out=smx[:, 1:2], in0=smx[:, 0:1], scalar1=1.0 / D, scalar2=EPS, op0=OP.mult, op1=OP.add), r=[tag + "0"], w=[tag + "1"])
        I("act", lambda e: e.activation(out=smx[:, 1:2], in_=smx[:, 1:2], func=AF.Sqrt), r=[tag + "1"], w=[tag + "1"])
        I("dve", lambda e: e.reciprocal(out=smx[:, 2:3], in_=smx[:, 1:2]), r=[tag + "1"], w=[tag + "rstd"])

    def to_hT(I, wcol0):
        for k in range(8):
            I("pe", lambda e, k=k: e.transpose(out=pT6[:, (k % 4) * 128:(k % 4 + 1) * 128], in_=xnb[:, k * 128:(k + 1) * 128], identity=identb),
              r=["xnb", "identb"], w=[("bank", 6)])
            if wcol0 is None:
                I("dve", lambda e, k=k: e.tensor_copy(out=hT[:, k, :], in_=pT6[:, (k % 4) * 128:(k % 4 + 1) * 128]), r=[("bank", 6)], w=["hT"])
            else:
                I("dve", lambda e, k=k: e.tensor_scalar(out=hT[:, k, :], in0=pT6[:, (k % 4) * 128:(k % 4 + 1) * 128], scalar1=col(wcol0 + k), scalar2=None, op0=OP.mult),
                  r=[("bank", 6), "pvt"], w=["hT"])

    def resid_add(I, lhs, wmat, key_l, chunk_of, xt_, kx):
        for half in range(2):
            bk = 4 + half
            for k in range(8):
                I("pe", lambda e, k=k, half=half, bk=bk: e.matmul(psb[bk][:, :], lhsT=lhs[:, k, :], rhs=wmat[:, chunk_of(k), half * 512:(half + 1) * 512],
                                                                 start=(k == 0), stop=(k == 7)), r=[key_l, "wts"], w=[("bank", bk)])
            I("dve", lambda e, half=half, bk=bk: e.tensor_tensor(out=xt_[:, half * 512:(half + 1) * 512], in0=psb[bk][:, :], in1=xt_[:, half * 512:(half + 1) * 512], op=OP.add),
              r=[("bank", bk), kx], w=[kx])

    def stageA(it, par, I, DMA):
        xt_ = xts[par]; kx = ("xt", par); gate_ = gates[par]; idxu_ = idxus[par]; kg = ("gate", par); ki = ("idxu", par)
        DMA("sp", lambda e: e.dma_start(out=xt_, in_=x_tok[it * 128:(it + 1) * 128, :]), w=[kx], ch=("xt", par))
        for ag in range(8):
            DMA("pool", lambda e, ag=ag: e.indirect_dma_start(out=yT[:, ag, :], out_offset=None, in_=cflat,
                                                             in_offset=bass.IndirectOffsetOnAxis(ap=iy[:, ag:ag + 1], axis=0),
                                                             element_offset=it * 256 * 128),
                r=["cout", "iy"], w=["yT"], ch="yT")
        resid_add(I, yT, woutb, "yT", lambda k: (k // 2) + 4 * (k % 2), xt_, kx)
        if debug:
            DMA("sp", lambda e: e.dma_start(out=G["dbg"][0, it * 128:(it + 1) * 128, :], in_=xt_), r=[kx], w=["dbg0"], ch="dbg0")
        rms_stats(I, xt_, kx, smA, "smA", junk, ["junk"])
        I("dve", lambda e: e.tensor_scalar(out=xnb, in0=xt_, scalar1=smA[:, 2:3], scalar2=None, op0=OP.mult), r=[kx, "smArstd"], w=["xnb"])
        to_hT(I, PV_NXA)
        for c in range(8):
            pb = 6 + c % 2
            for k in range(8):
                I("pe", lambda e, k=k, c=c, pb=pb: e.matmul(psb[pb][:, 0:128], lhsT=wqb[:, k, c * 128:(c + 1) * 128], rhs=hT[:, k, :], start=(k == 0), stop=(k == 7)),
                  r=["hT", "wts"], w=[("bank", pb)])
            I("act", lambda e, c=c, pb=pb: e.activation(out=qT[:, c, :], in_=psb[pb][:, 0:128], func=AF.Copy), r=[("bank", pb)], w=["qT"])
        for h in range(4):
            for mc in range(2):
                for dc in range(2):
                    I("pe", lambda e, h=h, mc=mc, dc=dc: e.matmul(psb[6][:, mc * 128:(mc + 1) * 128], lhsT=kT[:, 2 * h + dc, mc * 128:(mc + 1) * 128], rhs=qT[:, 2 * h + dc, :],
                                                                  start=(dc == 0), stop=(dc == 1)), r=["kT", "qT"], w=[("bank", 6)])
            I("act", lambda e: e.activation(out=expT.rearrange("p m t -> p (m t)"), in_=psb[6][:, 0:256], func=AF.Exp, scale=1.0 / 16.0), r=[("bank", 6)], w=["expT"])
            for mc in range(2):
                I("pe", lambda e, mc=mc: e.matmul(psb[7][:, 0:128], lhsT=onesb, rhs=expT[:, mc, :], start=(mc == 0), stop=(mc == 1)), r=["expT", "onesb"], w=[("bank", 7)])
            I("dve", lambda e: e.reciprocal(out=rden, in_=psb[7][:, 0:128]), r=[("bank", 7)], w=["rden"])
            for dc in range(2):
                for mc in range(2):
                    I("pe", lambda e, h=h, mc=mc, dc=dc: e.matmul(psb[4][:, 0:128], lhsT=vv[:, mc, (2 * h + dc) * 128:(2 * h + dc + 1) * 128], rhs=expT[:, mc, :],
                                                                  start=(mc == 0), stop=(mc == 1)), r=["expT", "vv"], w=[("bank", 4)])
                I("dve", lambda e, h=h, dc=dc: e.tensor_tensor(out=oT[:, 2 * h + dc, :], in0=psb[4][:, 0:128], in1=rden, op=OP.mult), r=[("bank", 4), "rden"], w=["oT"])
        resid_add(I, oT, wob, "oT", lambda k: k, xt_, kx)
        if debug:
            DMA("sp", lambda e: e.dma_start(out=G["dbg"][1, it * 128:(it + 1) * 128, :], in_=xt_), r=[kx], w=["dbg1"], ch="dbg1")
        rms_stats(I, xt_, kx, smA, "smA", junk, ["junk"])
        I("dve", lambda e: e.scalar_tensor_tensor(out=h3, in0=xt_, scalar=smA[:, 2:3], in1=wbc[:, 0:1024], op0=OP.mult, op1=OP.mult), r=[kx, "smArstd", "wbc"], w=["h3"])
        I("act", lambda e: e.activation(out=xnb, in_=h3, func=AF.Copy), r=["h3"], w=["xnb"])
        to_hT(I, None)
        for c in range(16):
            pb = 6 + c % 2
            for k in range(8):
                I("pe", lambda e, k=k, c=c, pb=pb: e.matmul(psb[pb][:, 0:128], lhsT=big[:, k, c * 128:(c + 1) * 128], rhs=hT[:, k, :], start=(k == 0), stop=(k == 7)),
                  r=["hT", "wts"], w=[("bank", pb)])
            I("act", lambda e, c=c, pb=pb: e.activation(out=qpT[:, c, :], in_=psb[pb][:, 0:128], func=AF.Copy), r=[("bank", pb)], w=["qpT"])
        for c in range(16):
            pb = 4 + c % 2
            I("pe", lambda e, c=c, pb=pb: e.matmul(psb[pb][:, 0:128], lhsT=qpT[:, c, :], rhs=skt[:, c, :], start=True, stop=True), r=["qpT", "skt"], w=[("bank", pb)])
            I("act", lambda e, c=c, pb=pb: e.activation(out=scs[:, c, :], in_=psb[pb][:, 0:128], func=AF.Copy), r=[("bank", pb)], w=[("scs", c)])
            I("dve", lambda e, c=c: e.max(out=tv[:, c, 0:8], in_=scs[:, c, :]), r=[("scs", c)], w=[("tv", c)])
            I("dve", lambda e, c=c: e.max_index(out=tiu[:, c, 0:8], in_max=tv[:, c, 0:8], in_values=scs[:, c, :]), r=[("scs", c), ("tv", c)], w=[("tiu", c)])
            I("dve", lambda e, c=c: e.match_replace(out=sct, in_to_replace=tv[:, c, 0:8], in_values=scs[:, c, :], imm_value=-1e30), r=[("scs", c), ("tv", c)], w=["sct"])
            I("dve", lambda e, c=c: e.max(out=tv[:, c, 8:16], in_=sct), r=["sct"], w=[("tv", c)])
            I("dve", lambda e, c=c: e.max_index(out=tiu[:, c, 8:16], in_max=tv[:, c, 8:16], in_values=sct), r=["sct", ("tv", c)], w=[("tiu", c)])
            I("dve", lambda e, c=c: e.tensor_copy(out=tif[:, c, :], in_=tiu[:, c, :]), r=[("tiu", c)], w=[("tif", c)])
        for h in range(8):
            c1, c2 = 2 * h, 2 * h + 1
            c3 = cand.rearrange("p (a b) -> p a b", a=16)
            I("dve", lambda e, c1=c1, c2=c2, c3=c3: e.tensor_tensor(out=c3, in0=tv[:, c1, :].unsqueeze(2).broadcast_to([128, 16, 16]),
                                                                    in1=tv[:, c2, :].unsqueeze(1).broadcast_to([128, 16, 16]), op=OP.add),
              r=[("tv", c1), ("tv", c2)] + [("scs", c) for c in range(16)], w=["cand"])
            I("dve", lambda e, c1=c1, c2=c2: e.scalar_tensor_tensor(out=cidx.rearrange("p (a b) -> p a b", a=16), in0=tif[:, c1, :].unsqueeze(2).broadcast_to([128, 16, 16]), scalar=128.0,
                                                                    in1=tif[:, c2, :].unsqueeze(1).broadcast_to([128, 16, 16]), op0=OP.mult, op1=OP.add),
              r=[("tif", c1), ("tif", c2)], w=["cidx"])
            I("dve", lambda e: e.max(out=best[:, 0:8], in_=cand), r=["cand"], w=["best"])
            I("dve", lambda e: e.max_index(out=posu[:, 0:8], in_max=best[:, 0:8], in_values=cand), r=["cand", "best"], w=["posu"])
            I("dve", lambda e: e.match_replace(out=cand2, in_to_replace=best[:, 0:8], in_values=cand, imm_value=-1e30), r=["cand", "best"], w=["cand2"])
            I("dve", lambda e: e.max(out=best[:, 8:16], in_=cand2), r=["cand2"], w=["best"])
            I("dve", lambda e: e.max_index(out=posu[:, 8:16], in_max=best[:, 8:16], in_values=cand2), r=["cand2", "best"], w=["posu"])
            I("dve", lambda e: e.tensor_copy(out=posf, in_=posu), r=["posu"], w=["posf"])
            I("dve", lambda e: e.tensor_tensor(out=oh, in0=iota16.unsqueeze(1).broadcast_to([128, 16, 256]), in1=posf.unsqueeze(2).broadcast_to([128, 16, 256]), op=OP.is_equal),
              r=["iota", "posf", "qpT"] + [("scs", c) for c in range(16)], w=["oh"])
            I("dve", lambda e: e.tensor_tensor(out=oh, in0=oh, in1=cidx.unsqueeze(1).broadcast_to([128, 16, 256]), op=OP.mult), r=["oh", "cidx"], w=["oh"])
            I("dve", lambda e, h=h: e.tensor_reduce(out=idxf[:, h * 16:(h + 1) * 16], in_=oh, axis=AX.X, op=OP.add), r=["oh"], w=["idxf"])
            I("dve", lambda e: e.tensor_scalar(out=smA[:, 3:4], in0=best[:, 0:1], scalar1=-1.0, scalar2=None, op0=OP.mult), r=["best"], w=["nmax"])
            I("act", lambda e, h=h: e.activation(out=gate_[:, h * 16:(h + 1) * 16], in_=best, func=AF.Exp, bias=smA[:, 3:4], accum_out=smA[:, 4:5]), r=["best", "nmax"], w=[kg, "gsum"])
            I("dve", lambda e: e.reciprocal(out=smA[:, 5:6], in_=smA[:, 4:5]), r=["gsum"], w=["grs"])
            I("dve", lambda e, h=h: e.tensor_scalar(out=gate_[:, h * 16:(h + 1) * 16], in0=gate_[:, h * 16:(h + 1) * 16], scalar1=smA[:, 5:6], scalar2=None, op0=OP.mult), r=[kg, "grs"], w=[kg])
        I("dve", lambda e: e.tensor_copy(out=idxu_, in_=idxf), r=["idxf"], w=[ki])

    def stageG(it, par, pump):
        xt_ = xts[par]; kx = ("xt", par); gate_ = gates[par]; idxu_ = idxus[par]; kg = ("gate", par); ki = ("idxu", par)
        P.I("dve", lambda e: e.tensor_copy(out=h3p, in_=h3), r=["h3"], w=[("bank", 0), ("bank", 1)])
        P.I("dve", lambda e: e.tensor_copy(out=accp1, in_=xt_), r=[kx], w=accbk)
        for j in range(128 + 2):
            if j < 128:
                sb = j % NB
                b = gb[sb]
                P.DMA("pool", lambda e, j=j, b=b: e.indirect_dma_start(out=b, out_offset=None, in_=G["uvb"].ap()[:, :],
                                                                       in_offset=bass.IndirectOffsetOnAxis(ap=idxu_[:, j:j + 1], axis=0)),
                      r=[ki, "uvb"], w=[("gb", sb)], ch=("gb", sb))
                P.I("dve", lambda e, j=j, b=b: e.scalar_tensor_tensor(out=junkb2, in0=b[:, 0:1024], scalar=1.0, in1=h3p, op0=OP.mult, op1=OP.mult, accum_out=sval[:, j:j + 1]),
                    r=[("gb", sb)], w=[("sval", j)], ro=[("bank", 0), ("bank", 1)])
            if 1 <= j <= 128:
                jj = j - 1
                P.I("act", lambda e, jj=jj: e.activation(out=act[:, jj:jj + 1], in_=sval[:, jj:jj + 1], func=AF.Gelu_apprx_tanh), r=[("sval", jj)], w=[("act", jj)])
            if j >= 2:
                jj = j - 2
                sb = jj % NB
                b = gb[sb]
                P.I("dve", lambda e, jj=jj: e.tensor_tensor(out=act[:, jj:jj + 1], in0=act[:, jj:jj + 1], in1=gate_[:, jj:jj + 1], op=OP.mult), r=[("act", jj), kg], w=[("actg", jj)])
                P.I("dve", lambda e, jj=jj, b=b: e.scalar_tensor_tensor(out=accp1, in0=b[:, 1024:2048], scalar=act[:, jj:jj + 1], in1=accp1, op0=OP.mult, op1=OP.add),
                    r=[("gb", sb), ("actg", jj)], w=accbk)
            pump()
        P.I("dve", lambda e: e.tensor_copy(out=xt_, in_=accp1), r=accbk, w=[kx])
        if debug:
            P.DMA("sp", lambda e: e.dma_start(out=G["dbg"][2, it * 128:(it + 1) * 128, :], in_=xt_), r=[kx], w=["dbg2"], ch="dbg2")
        rms_stats(P.I, xt_, kx, smG, "smG", h3p, [("bank", 0), ("bank", 1)])
        P.I("dve", lambda e: e.scalar_tensor_tensor(out=acc, in0=xt_, scalar=smG[:, 2:3], in1=wbc[:, 1024:2048], op0=OP.mult, op1=OP.mult), r=[kx, "smGrstd", "wbc"], w=["acc"])
        P.DMA("sp", lambda e: e.dma_start(out=G["out"][it * 128:(it + 1) * 128, :], in_=acc), r=["acc"], w=["outd"], ch="outd")

    pending = collections.deque()

    def Idef(*a, **k):
        pending.append(("I", a, k))

    def DMAdef(*a, **k):
        pending.append(("DMA", a, k))

    npump = [3]

    def pump(n=None):
        for _ in range(npump[0] if n is None else n):
            if not pending:
                return
            kind, a, k = pending.popleft()
            getattr(P, kind)(*a, **k)

    stageA(0, 0, P.I, P.DMA)
    for it in range(NT):
        if it + 1 < NT:
            stageA(it + 1, (it + 1) % 2, Idef, DMAdef)
            npump[0] = len(pending) // 125 + 1
        stageG(it, it % 2, pump)
        while pending:
            pump(1000)


def prep_inputs(inp):
    f = lambda a: np.ascontiguousarray(np.asarray(a, dtype=np.float32))
    x = f(inp["x"]); mem = f(inp["mem"])
    w_in = f(inp["w_in"])[0]
    cq = f(inp["conv_qkv_w"])[0]; lcw = f(inp["lru_conv_w"])[0]
    maps = []
    for c in range(NCORES):
        b, j = c // 4, c % 4
        cols = []
        for base in (0, 512, 1024, 2056, 2568, 1536):
            cols.append(w_in[:, base + j * 128: base + (j + 1) * 128])
        cols.append(np.repeat(w_in[:, 2048 + j:2049 + j], 128, axis=1))
        cols.append(np.repeat(w_in[:, 2052 + j:2053 + j], 128, axis=1))
        w1 = np.ascontiguousarray(np.concatenate(cols, axis=1))
        cwm = np.zeros((128, 16), np.float32)
        for s_, base in enumerate((0, 512, 1024)):
            cwm[:, 4 * s_:4 * s_ + 4] = cq[:, base + j * 128: base + (j + 1) * 128].T
        cwm[:, 12:16] = lcw[:, j * 128:(j + 1) * 128].T
        pvm = np.zeros((128, 40), np.float32)
        sl = slice(j * 128, (j + 1) * 128)
        pvm[:, 0] = f(inp["lru_conv_b"])[0, sl]
        pvm[:, 1] = f(inp["lru_ba"])[0, sl]
        pvm[:, 2] = f(inp["lru_bx"])[0, sl]
        pvm[:, 3] = f(inp["lru_lambda"])[0, sl]
        pvm[:, 4] = f(inp["gdn_a_log"])[0, j]
        pvm[:, 5] = f(inp["gdn_dt_bias"])[0, j]
        pvm[:, 6] = f(inp["gdn_norm_w"])[0]
        pvm[:, 8:16] = f(inp["norm_mix_w"])[0].reshape(8, 128).T
        pvm[:, 16:24] = f(inp["norm_xattn_w"])[0].reshape(8, 128).T
        pvm[:, 24:32] = f(inp["norm_mem_w"])[0].reshape(8, 128).T
        lwm = np.stack([f(inp["lru_wa"])[0, 2 * j:2 * j + 2], f(inp["lru_wx"])[0, 2 * j:2 * j + 2]])
        skT = np.ascontiguousarray(f(inp["peer_subkeys"])[0].reshape(16, 128, 128).transpose(2, 0, 1).reshape(128, 16 * 128))
        maps.append({
            "x_b": x[b], "w1": w1, "cw": cwm, "pv": pvm, "lw": np.ascontiguousarray(lwm),
            "mem_b": mem[b], "w_out": f(inp["w_out"])[0], "xwq": f(inp["xattn_wq"])[0],
            "xwkv": f(inp["xattn_wkv"])[0], "xwo": f(inp["xattn_wo"])[0], "pwq": f(inp["peer_wq"])[0],
            "skT": skT, "peer_u": f(inp["peer_u"])[0], "peer_v": f(inp["peer_v"])[0],
            "rows": np.stack([f(inp["norm_ffn_w"])[0], f(inp["norm_final_w"])]),
            "x_tok": np.ascontiguousarray(x[b, j * TOK:(j + 1) * TOK]),
            "idxy": np.ascontiguousarray((j * 16384 + np.arange(4)[None, :, None] * 4096 + np.arange(2)[None, None, :] * 128
                                          + np.arange(128)[:, None, None]).reshape(128, 8).astype(np.uint32)),
        })
    return maps


_NC = {}


def kernel(**inputs):
    if "nc" not in _NC:
        _NC["nc"] = build(False)
    maps = prep_inputs(inputs)
    res = run_bass_kernel_spmd(_NC["nc"], maps, core_ids=list(range(NCORES)))
    o = np.concatenate([res.results[c]["out"] for c in range(NCORES)], axis=0)
    return o.reshape(2, SEQ, D).astype(np.float32)
```

```python
import contextlib
import numpy as np
import concourse.bass as bass
import concourse.mybir as mybir
from concourse.bass_utils import run_bass_kernel_spmd

F32 = mybir.dt.float32
BF16 = mybir.dt.bfloat16
U32 = mybir.dt.uint32
AF = mybir.ActivationFunctionType
OP = mybir.AluOpType
AX = mybir.AxisListType

NCORES = 8
D = 1024
SEQ = 8192
TOK = 2048
EPS = 1e-6
ENG = ("pe", "act", "dve", "pool", "sp")


class Prog:
    def __init__(self, nc):
        self.nc = nc
        self.ops = {e: [] for e in ENG}
        self.cnt = {}
        self.epoch = {e: 0 for e in ENG}
        self.waited = {}
        self.lastw = {}
        self.reads = {}
        self.semkeys = []
        self.sems = {}

    def _semkey_engine(self, e):
        k = ("E", e, self.epoch[e])
        if self.cnt.get(k, 0) >= 30000:
            self.epoch[e] += 1
            k = ("E", e, self.epoch[e])
        return k

    def _deps(self, eng, r, w, skip_same_pe):
        deps = {}
        def add(sv):
            if sv is None:
                return
            k, v = sv
            if skip_same_pe and k[0] == "E" and k[1] == "pe":
                return
            if deps.get(k, 0) < v:
                deps[k] = v
        for b in r:
            add(self.lastw.get(b))
        for b in w:
            add(self.lastw.get(b))
            for sv in self.reads.get(b, ()):
                add(sv)
        out = []
        for k, v in deps.items():
            if self.waited.get((eng, k), 0) < v:
                self.waited[(eng, k)] = v
                out.append((k, v))
        return out

    def _commit(self, r, w, sv):
        for b in r:
            self.reads.setdefault(b, []).append(sv)
        for b in w:
            self.lastw[b] = sv
            self.reads[b] = []

    def _newsem(self, k):
        if k not in self.cnt:
            self.cnt[k] = 0
            self.semkeys.append(k)

    def I(self, eng, fn, r=(), w=(), ro=()):
        bk = [b for b in r if isinstance(b, tuple) and b[0] == "bank"]
        r = list(r) + list(ro)
        if bk:
            r = [b for b in r if b not in bk]
            w = list(w) + bk
        waits = self._deps(eng, r, w, skip_same_pe=(eng == "pe"))
        k = self._semkey_engine(eng)
        self._newsem(k)
        self.cnt[k] += 1
        sv = (k, self.cnt[k])
        self._commit(r, w, sv)
        self.ops[eng].append((waits, fn, k, 1))

    def DMA(self, eng, fn, r=(), w=(), ch=None):
        waits = self._deps(eng, r, w, skip_same_pe=False)
        k = ("D", ch, 0)
        n = 0
        while self.cnt.get(("D", ch, n), 0) >= 30000:
            n += 1
        k = ("D", ch, n)
        self._newsem(k)
        self.cnt[k] += 16
        sv = (k, self.cnt[k])
        self._commit(r, w, sv)
        self.ops[eng].append((waits, fn, k, 16))

    def CC(self, eng, fn, r=(), w=()):
        waits = self._deps(eng, r, w, skip_same_pe=False)
        k = ("C", "cc", 0)
        self._newsem(k)
        self.cnt[k] += 1
        sv = (k, self.cnt[k])
        self._commit(r, w, sv)
        self.ops[eng].append((waits, fn, k, 1))

    def barrier(self):
        for e in ENG:
            waits = []
            for k, v in self.cnt.items():
                if v > 0 and self.waited.get((e, k), 0) < v:
                    self.waited[(e, k)] = v
                    waits.append((k, v))
            if waits:
                self.ops[e].append((waits, None, None, 0))

    def emit(self):
        nc = self.nc
        with contextlib.ExitStack() as st:
            for i, k in enumerate(self.semkeys):
                self.sems[k] = st.enter_context(nc.semaphore("s%d" % i))
            block = st.enter_context(nc.Block())
            engobj = {"pe": "tensor", "act": "scalar", "dve": "vector", "pool": "gpsimd", "sp": "sync"}

            def run(e, ename):
                for waits, fn, k, inc in self.ops[ename]:
                    for wk, wv in waits:
                        e.wait_ge(self.sems[wk], wv)
                    if fn is not None:
                        fn(e).then_inc(self.sems[k], inc)

            @block.tensor
            def _(e):
                run(e, "pe")

            @block.scalar
            def _(e):
                run(e, "act")

            @block.vector
            def _(e):
                run(e, "dve")

            @block.gpsimd
            def _(e):
                run(e, "pool")
            @block.sync
            def _(e):
                run(e, "sp")


class Arena:
    def __init__(self, t, n):
        self.t = t
        self.n = n
        self.off = 0
        self.mark = 0

    def f32(self, cols):
        o = self.off
        self.off += cols
        assert self.off <= self.n, ("arena overflow", self.off, self.n)
        return self.t[:, o:o + cols]

    def bf16(self, cols):
        c = (cols + 1) // 2
        return self.f32(c).bitcast(BF16)

    def u32(self, cols):
        return self.f32(cols).bitcast(U32)


def build(debug=False, p2=True, nblk=16, cc=True, stop=0, ntile=16):
    nc = bass.Bass("TRN2", target_bir_lowering=False)
    P = Prog(nc)

    def din(name, shape, dt=F32):
        return nc.dram_tensor(name, list(shape), dt, kind="ExternalInput").ap()

    x_b = din("x_b", [SEQ, D])
    w1 = din("w1", [D, 1024])
    cw = din("cw", [128, 16])
    pv = din("pv", [128, 40])
    lw = din("lw", [2, 2, 64, 64])
    if not p2:
        din = lambda *a, **k: None
    mem_b = din("mem_b", [256, D])
    w_out = din("w_out", [D, D])
    xwq = din("xwq", [D, D])
    xwkv = din("xwkv", [D, 2 * D])
    xwo = din("xwo", [D, D])
    pwq = din("pwq", [D, 2 * D])
    skT = din("skT", [128, 16 * 128])
    peer_u = din("peer_u", [16384, D])
    peer_v = din("peer_v", [16384, D])
    rows = din("rows", [2, D])
    out = nc.dram_tensor("out", [TOK, D], F32, kind="ExternalOutput").ap()
    cin_t = [nc.dram_tensor("cin%d" % q, [256, 2048], BF16) for q in range(4)]
    coutall = nc.dram_tensor("coutall", [4, 1024, 2048], BF16)
    cin = [t.ap().rearrange("r (x t) -> (r x) t", t=128) for t in cin_t]
    coutflat = coutall.ap().rearrange("q r (x t) -> (q r x) t", t=128)
    ub = nc.dram_tensor("ub", [16384, D], BF16) if p2 else None
    vb = nc.dram_tensor("vb", [16384, D], BF16) if p2 else None
    idxy = din("idxy", [128, 8], U32)
    x_tok = din("x_tok", [TOK, D])
    if debug:
        dbg = nc.dram_tensor("dbg", [3, TOK, D], F32, kind="ExternalOutput").ap()

    st = contextlib.ExitStack()
    NA = 49000
    arena_t = st.enter_context(nc.sbuf_tensor("arena", [128, NA], F32))
    ps_all = st.enter_context(nc.psum_tensor("ps_all", [128, 4096], F32))
    psb = [ps_all[:, i * 512:(i + 1) * 512] for i in range(8)]

    A = Arena(arena_t, NA)
    ident = A.f32(128)
    identb = A.bf16(128)
    ones = A.f32(512)
    onesb = A.bf16(128)
    mU = A.f32(128)
    mUs = A.f32(128)
    cwt = A.f32(16)
    pvt = A.f32(40)
    A.mark = A.off

    P.I("pool", lambda e: e.memset(ones, 1.0), w=["ones"])
    P.I("pool", lambda e: e.memset(onesb, 1.0), w=["onesb"])
    P.I("pool", lambda e: e.affine_select(out=ident, in_=ones[:, 0:128], pattern=[[-1, 128]],
                                          compare_op=OP.is_equal, fill=0.0, base=0, channel_multiplier=1),
        r=["ones"], w=["ident"])
    P.I("pool", lambda e: e.tensor_copy(out=identb, in_=ident), r=["ident"], w=["identb"])
    P.I("pool", lambda e: e.affine_select(out=mU, in_=ones[:, 0:128], pattern=[[1, 128]],
                                          compare_op=OP.is_ge, fill=0.0, base=0, channel_multiplier=-1),
        r=["ones"], w=["mU"])
    P.I("pool", lambda e: e.affine_select(out=mUs, in_=ones[:, 0:128], pattern=[[1, 128]],
                                          compare_op=OP.is_gt, fill=0.0, base=0, channel_multiplier=-1),
        r=["ones"], w=["mUs"])
    P.DMA("sp", lambda e: e.dma_start(out=cwt, in_=cw[:, :]), w=["cwt"], ch="c_cwt")
    P.DMA("sp", lambda e: e.dma_start(out=pvt, in_=pv[:, :]), w=["pvt"], ch="c_pvt")

    PV_CB, PV_BA, PV_BX, PV_LAM, PV_ALOG, PV_DTB, PV_GNW = 0, 1, 2, 3, 4, 5, 6
    PV_NMIX, PV_NXA, PV_NMEM = 8, 16, 24

    def col(i):
        return pvt[:, i:i + 1]

    phase1(nc, P, A, locals())
    P.barrier()
    if cc:
      for q in range(4):
        P.CC("pool", lambda e, q=q: e.collective_compute("AllGather", OP.bypass,
                                                        replica_groups=[[0, 1, 2, 3], [4, 5, 6, 7]],
                                                        ins=[cin_t[q].ap()], outs=[coutall.ap()[q]]),
             r=["cin"], w=["cout"])
    P.barrier()
    A.off = A.mark
    if p2:
        phase2(nc, P, A, locals())
    P.barrier()
    P.emit()
    st.close()
    return nc


def phase1(nc, P, A, G):
    psb = G["psb"]; ident = G["ident"]; identb = G["identb"]; ones = G["ones"]
    mU = G["mU"]; mUs = G["mUs"]; cwt = G["cwt"]; pvt = G["pvt"]; col = G["col"]
    x_b = G["x_b"]; w1 = G["w1"]; lw = G["lw"]; cin = G["cin"]
    PV_CB, PV_BA, PV_BX, PV_LAM, PV_ALOG, PV_DTB, PV_GNW, PV_NMIX = 0, 1, 2, 3, 4, 5, 6, 8

    w1b = A.bf16(8 * 1024).rearrange("p (k c) -> p k c", k=8)
    wabd = A.f32(128)
    wxbd = A.f32(128)
    sc1 = A.f32(16)
    negsp8, negsp16, nA, dtb = sc1[:, 0:1], sc1[:, 1:2], sc1[:, 2:3], sc1[:, 3:4]
    tmpc = sc1[:, 4:6]
    xin = [A.f32(4 * 1024).rearrange("p (s d) -> p s d", s=4) for _ in range(2)]
    junkb = A.bf16(1024)
    ssq = A.f32(4); msq = A.f32(4); rstd = A.f32(4)
    xn = A.bf16(4 * 1024).rearrange("p (s d) -> p s d", s=4)
    hT = A.bf16(8 * 512).rearrange("p (k t) -> p k t", k=8)
    cb = [A.f32(515) for _ in range(4)]
    cy = [A.f32(512) for _ in range(4)]
    qs = A.f32(512); ks = A.f32(512); vs = A.f32(512)
    gg = A.f32(512); zs = A.f32(512); grow = A.f32(512); brow = A.f32(512)
    t512 = [A.f32(512) for _ in range(8)]
    qn = A.f32(512); kn = A.f32(512)
    hprev = A.f32(1)
    ystage = [A.bf16(2 * 512).rearrange("p (g t) -> p g t", g=2) for _ in range(2)]
    S = [A.f32(128) for _ in range(2)]
    NCH = 4
    HB = {nm: [buf, A.f32(512)] for nm, buf in (("qn", qn), ("kn", kn), ("vs", vs), ("grow", grow), ("brow", brow), ("zs", zs))}
    def cbufs():
        d = {}
        for nm in ["gcum", "arg", "DT", "eg", "ekd", "t1", "t2", "t3", "kbgT", "vbT", "qdT", "kdT",
                   "B", "Am", "M", "B2", "A2", "kbg", "vb", "kd", "wT", "u", "attnT", "vnew", "on"]:
            d[nm] = A.f32(128)
        d["small"] = A.f32(8)
        return d
    CB = [cbufs() for _ in range(NCH)]
    pTs = [psb[6][:, :].bitcast(BF16), psb[7][:, :].bitcast(BF16)]

    P.DMA("pool", lambda e: e.dma_start(out=w1b, in_=w1.rearrange("(k p) c -> p k c", p=128)), w=["w1b"], ch="w1")
    P.I("pool", lambda e: e.memset(wabd, 0.0), w=["wabd"])
    P.I("pool", lambda e: e.memset(wxbd, 0.0), w=["wxbd"])
    for i in range(2):
        P.DMA("sp", lambda e, i=i: e.dma_start(out=wabd[i * 64:(i + 1) * 64, i * 64:(i + 1) * 64], in_=lw[0, i]), w=["wabd"], ch="c_wabd")
        P.DMA("sp", lambda e, i=i: e.dma_start(out=wxbd[i * 64:(i + 1) * 64, i * 64:(i + 1) * 64], in_=lw[1, i]), w=["wxbd"], ch="c_wxbd")
    P.I("act", lambda e: e.activation(out=tmpc[:, 0:1], in_=col(PV_LAM), func=AF.Exp, scale=-1.0), r=["pvt"], w=["tmpc"])
    P.I("act", lambda e: e.activation(out=tmpc[:, 1:2], in_=tmpc[:, 0:1], func=AF.Ln, bias=1.0), r=["tmpc"], w=["tmpc2"])
    P.I("dve", lambda e: e.tensor_scalar(out=negsp8, in0=tmpc[:, 1:2], scalar1=-8.0, scalar2=None, op0=OP.mult), r=["tmpc2"], w=["negsp8"])
    P.I("dve", lambda e: e.tensor_scalar(out=negsp16, in0=tmpc[:, 1:2], scalar1=-16.0, scalar2=None, op0=OP.mult), r=["tmpc2"], w=["negsp16"])
    P.I("act", lambda e: e.activation(out=nA, in_=col(PV_ALOG), func=AF.Exp), r=["pvt"], w=["nA0"])
    P.I("dve", lambda e: e.tensor_scalar(out=nA, in0=nA, scalar1=-1.0, scalar2=None, op0=OP.mult), r=["nA0"], w=["nA"])
    for c in range(4):
        P.I("pool", lambda e, c=c: e.memset(cb[c][:, 0:3], 0.0), w=[("cbh", c)])
    P.I("pool", lambda e: e.memset(S[0], 0.0), w=[("S", 0)])
    P.I("pool", lambda e: e.memset(hprev, 0.0), w=["hprev"])

    NBLK = G['nblk']
    stop = G['stop']
    ps_rot = [0]

    def load_x(blk, DMA):
        s = blk % 2
        DMA("sp", lambda e: e.dma_start(out=xin[s], in_=x_b[blk * 512:(blk + 1) * 512, :].rearrange("(s p) d -> p s d", p=128)),
              w=[("xin", s)], ch=("xin", s))

    import collections
    pending = collections.deque()

    def Idef(*a, **k):
        pending.append(("I", a, k))

    def DMAdef(*a, **k):
        pending.append(("DMA", a, k))

    cnt = [0]

    def CI(*a, **k):
        P.I(*a, **k)
        cnt[0] += 1
        if cnt[0] % 3 != 0 and pending:
            kind, a2, k2 = pending.popleft()
            getattr(P, kind)(*a2, **k2)

    def front(blk, I, DMA):
        sl = blk % 2
        par = blk % 2
        qn, kn, vs, grow, brow, zs = (HB[nm][par] for nm in ("qn", "kn", "vs", "grow", "brow", "zs"))
        if G["ub"] is not None:
            for ck in range(blk * 16 // NBLK, (blk + 1) * 16 // NBLK):
                for (src, dst, nm) in ((G["peer_u"], G["ub"], "ub"), (G["peer_v"], G["vb"], "vb")):
                    DMA("pool", lambda e, ck=ck, src=src, dst=dst: e.dma_start(out=dst.ap()[ck * 1024:(ck + 1) * 1024, :], in_=src[ck * 1024:(ck + 1) * 1024, :]),
                          w=[nm], ch="cvt")
        if blk + 1 < NBLK:
            load_x(blk + 1, DMA)
        X = xin[sl]
        for s in range(4):
            I("act", lambda e, s=s, X=X: e.activation(out=junkb, in_=X[:, s, :], func=AF.Square, accum_out=ssq[:, s:s + 1]),
                r=[("xin", sl)], w=["junkb", ("ssq", s)])
        I("dve", lambda e: e.tensor_scalar(out=msq, in0=ssq, scalar1=1.0 / D, scalar2=EPS, op0=OP.mult, op1=OP.add),
            r=[("ssq", s) for s in range(4)], w=["msq"])
        I("act", lambda e: e.activation(out=msq, in_=msq, func=AF.Sqrt), r=["msq"], w=["msq"])
        I("dve", lambda e: e.reciprocal(out=rstd, in_=msq), r=["msq"], w=["rstd"])
        for s in range(4):
            eng = "dve"
            I(eng, lambda e, s=s, X=X: e.tensor_scalar(out=xn[:, s, :], in0=X[:, s, :], scalar1=rstd[:, s:s + 1], scalar2=None, op0=OP.mult),
                r=[("xin", sl), "rstd"], w=[("xn", s)])
        for k in range(8):
            half = k % 2
            for s in range(4):
                I("pe", lambda e, k=k, s=s, half=half: e.transpose(out=pTs[half][:, s * 128:(s + 1) * 128],
                                                                     in_=xn[:, s, k * 128:(k + 1) * 128], identity=identb),
                    r=[("xn", s), "identb"], w=[("bank", 6 + half)])
            if False:
                I("act", lambda e, k=k, half=half: e.activation(out=hT[:, k, :], in_=pTs[half][:, 0:512], func=AF.Copy,
                                                                  scale=col(PV_NMIX + k)),
                    r=[("bank", 6 + half), "pvt"], w=[("hT", k)])
            else:
                I("dve", lambda e, k=k, half=half: e.tensor_scalar(out=hT[:, k, :], in0=pTs[half][:, 0:512],
                                                                     scalar1=col(PV_NMIX + k), scalar2=None, op0=OP.mult),
                    r=[("bank", 6 + half), "pvt"], w=[("hT", k)])
        for c in range(8):
            pb = ps_rot[0] % 2
            ps_rot[0] += 1
            pst = psb[pb]
            for k in range(8):
                I("pe", lambda e, c=c, k=k, pst=pst: e.matmul(pst[:, :], lhsT=w1b[:, k, c * 128:(c + 1) * 128], rhs=hT[:, k, :],
                                                                start=(k == 0), stop=(k == 7)),
                    r=["w1b", ("hT", k)], w=[("bank", pb)])
            if c < 4:
                j = c
                if blk > 0:
                    I("dve", lambda e, j=j: e.tensor_copy(out=cb[j][:, 0:3], in_=cb[j][:, 512:515]), r=[("cb", j)], w=[("cbh", j)])
                I("act", lambda e, j=j, pst=pst: e.activation(out=cb[j][:, 3:515], in_=pst[:, :], func=AF.Copy),
                    r=[("bank", pb), ("cbh", j)], w=[("cb", j)])
            elif c == 4:
                I("act", lambda e, pst=pst: e.activation(out=gg, in_=pst[:, :], func=AF.Gelu_apprx_tanh), r=[("bank", pb)], w=["gg"])
            elif c == 5:
                I("act", lambda e, pst=pst: e.activation(out=zs, in_=pst[:, :], func=AF.Silu), r=[("bank", pb)], w=[("zs", par)])
            elif c == 6:
                I("act", lambda e, pst=pst: e.activation(out=grow, in_=pst[:, :], func=AF.Exp, bias=col(PV_DTB)), r=[("bank", pb), "pvt"], w=[("grow", par)])
                I("act", lambda e: e.activation(out=grow, in_=grow, func=AF.Ln, bias=1.0), r=[("grow", par)], w=[("grow", par)])
                I("dve", lambda e: e.tensor_scalar(out=grow, in0=grow, scalar1=nA, scalar2=None, op0=OP.mult), r=[("grow", par), "nA"], w=[("grow", par)])
            else:
                I("act", lambda e, pst=pst: e.activation(out=brow, in_=pst[:, :], func=AF.Sigmoid), r=[("bank", pb)], w=[("brow", par)])
        for j in range(4):
            eng = "dve" if j % 2 == 0 else "pool"
            if j == 3:
                I("dve", lambda e, j=j: e.tensor_scalar(out=cy[j], in0=cb[j][:, 0:512], scalar1=cwt[:, 4 * j:4 * j + 1], scalar2=col(PV_CB),
                                                          op0=OP.mult, op1=OP.add), r=[("cb", j), ("cbh", j), "cwt", "pvt"], w=[("cy", j)])
            else:
                I("dve", lambda e, j=j: e.tensor_scalar(out=cy[j], in0=cb[j][:, 0:512], scalar1=cwt[:, 4 * j:4 * j + 1], scalar2=None,
                                                          op0=OP.mult), r=[("cb", j), ("cbh", j), "cwt"], w=[("cy", j)])
            for tp in range(1, 4):
                I("dve", lambda e, j=j, tp=tp: e.scalar_tensor_tensor(out=cy[j], in0=cb[j][:, tp:tp + 512], scalar=cwt[:, 4 * j + tp:4 * j + tp + 1],
                                                                         in1=cy[j], op0=OP.mult, op1=OP.add),
                    r=[("cb", j), ("cbh", j), "cwt", ("cy", j)], w=[("cy", j)])
        I("act", lambda e: e.activation(out=qs, in_=cy[0], func=AF.Silu), r=[("cy", 0)], w=["qs"])
        I("act", lambda e: e.activation(out=ks, in_=cy[1], func=AF.Silu), r=[("cy", 1)], w=["ks"])
        I("act", lambda e: e.activation(out=vs, in_=cy[2], func=AF.Silu), r=[("cy", 2)], w=[("vs", par)])
        xrc = cy[3]
        r_, i_, a_, a2_, ix_, hs_ = t512[0], t512[1], t512[2], t512[3], t512[4], t512[5]
        I("pe", lambda e: e.matmul(psb[2][:, :], lhsT=wabd, rhs=xrc, start=True, stop=True), r=["wabd", ("cy", 3)], w=[("bank", 2)])
        I("act", lambda e: e.activation(out=r_, in_=psb[2][:, :], func=AF.Sigmoid, bias=col(PV_BA)), r=[("bank", 2), "pvt"], w=["r_"])
        I("pe", lambda e: e.matmul(psb[2][:, :], lhsT=wxbd, rhs=xrc, start=True, stop=True), r=["wxbd", ("cy", 3)], w=[("bank", 2)])
        I("act", lambda e: e.activation(out=i_, in_=psb[2][:, :], func=AF.Sigmoid, bias=col(PV_BX)), r=[("bank", 2), "pvt"], w=["i_"])
        I("act", lambda e: e.activation(out=a_, in_=r_, func=AF.Exp, scale=negsp8), r=["r_", "negsp8"], w=["a_"])
        I("act", lambda e: e.activation(out=a2_, in_=r_, func=AF.Exp, scale=negsp16), r=["r_", "negsp16"], w=["a2_"])
        I("dve", lambda e: e.tensor_scalar(out=a2_, in0=a2_, scalar1=-1.0, scalar2=1.0, op0=OP.mult, op1=OP.add), r=["a2_"], w=["a2_"])
        I("act", lambda e: e.activation(out=a2_, in_=a2_, func=AF.Sqrt), r=["a2_"], w=["a2_"])
        I("pool", lambda e: e.tensor_tensor(out=ix_, in0=i_, in1=xrc, op=OP.mult), r=["i_", ("cy", 3)], w=["ix_"])
        I("pool", lambda e: e.tensor_tensor(out=ix_, in0=ix_, in1=a2_, op=OP.mult), r=["ix_", "a2_"], w=["ix_"])
        I("dve", lambda e: e.tensor_tensor_scan(out=hs_, data0=a_, data1=ix_, initial=hprev[:, 0:1], op0=OP.mult, op1=OP.add),
            r=["a_", "ix_", "hprev"], w=["hs_"])
        I("dve", lambda e: e.tensor_copy(out=hprev, in_=hs_[:, 511:512]), r=["hs_"], w=["hprev"])
        I("pool", lambda e, sl=sl: e.tensor_tensor(out=ystage[sl][:, 1, :], in0=hs_, in1=gg, op=OP.mult), r=["hs_", "gg"], w=[("ystage", sl)])
        sq_, rq_ = t512[6], t512[7]
        for (src, dst, scl, nm) in ((qs, qn, 128.0 ** -0.5, ("qn", par)), (ks, kn, 1.0, ("kn", par))):
            I("pool", lambda e, src=src: e.tensor_tensor(out=sq_, in0=src, in1=src, op=OP.mult), r=["qs", "ks"], w=["sq_"])
            I("pe", lambda e: e.matmul(psb[2][:, :], lhsT=ones[:, 0:128], rhs=sq_, start=True, stop=True), r=["ones", "sq_"], w=[("bank", 2)])
            I("act", lambda e: e.activation(out=rq_, in_=psb[2][:, :], func=AF.Sqrt, bias=EPS), r=[("bank", 2)], w=["rq_"])
            I("dve", lambda e: e.reciprocal(out=rq_, in_=rq_), r=["rq_"], w=["rq_"])
            I("dve", lambda e, src=src, dst=dst, scl=scl: e.scalar_tensor_tensor(out=dst, in0=src, scalar=scl, in1=rq_, op0=OP.mult, op1=OP.mult),
                r=["qs", "ks", "rq_"], w=[nm])

    def chunks(blk):
        sl = blk % 2
        par = blk % 2
        qn, kn, vs, grow, brow, zs = (HB[nm][par] for nm in ("qn", "kn", "vs", "grow", "brow", "zs"))
        slots = [(3, 0), (4, 0), (3, 1), (4, 1), (3, 2), (4, 2), (3, 3), (4, 3)]
        sr = [0]

        def pslot():
            b, q = slots[sr[0] % len(slots)]
            sr[0] += 1
            return psb[b][:, q * 128:(q + 1) * 128], ("bank", b)

        def K(ch, nm):
            return (nm, ch)

        for ch in range(NCH):
            c = CB[ch]
            cs = slice(ch * 128, (ch + 1) * 128)
            sm = c["small"]
            gcol, ngcol, gl, dcol = sm[:, 0:1], sm[:, 1:2], sm[:, 2:3], sm[:, 3:4]
            CI("dve", lambda e, c=c, cs=cs: e.tensor_tensor_scan(out=c["gcum"], data0=ones[:, 0:128], data1=grow[:, cs], initial=0.0,
                                                                   op0=OP.mult, op1=OP.add), r=[("grow", par), "ones"], w=[K(ch, "gcum")])
            pt, pk = pslot()
            CI("pe", lambda e, c=c, pt=pt: e.matmul(pt, lhsT=c["gcum"], rhs=ident, start=True, stop=True), r=[K(ch, "gcum"), "ident"], w=[pk])
            CI("dve", lambda e, pt=pt, ngcol=ngcol: e.tensor_scalar(out=ngcol, in0=pt[:, 0:1], scalar1=-1.0, scalar2=None, op0=OP.mult),
                r=[pk], w=[K(ch, "ngcol")])
            CI("dve", lambda e, c=c, ngcol=ngcol: e.tensor_scalar(out=c["arg"], in0=c["gcum"], scalar1=ngcol, scalar2=0.0, op0=OP.add, op1=OP.min),
                r=[K(ch, "gcum"), K(ch, "ngcol")], w=[K(ch, "arg")])
            CI("act", lambda e, c=c: e.activation(out=c["DT"], in_=c["arg"], func=AF.Exp), r=[K(ch, "arg")], w=[K(ch, "DT")])
            CI("act", lambda e, c=c: e.activation(out=c["eg"], in_=c["gcum"], func=AF.Exp), r=[K(ch, "gcum")], w=[K(ch, "eg")])
            CI("act", lambda e, c=c: e.activation(out=c["ekd"], in_=c["gcum"], func=AF.Exp, scale=-1.0, bias=c["gcum"][:, 127:128]),
                r=[K(ch, "gcum")], w=[K(ch, "ekd")])
            CI("act", lambda e, c=c, dcol=dcol: e.activation(out=dcol, in_=c["gcum"][:, 127:128], func=AF.Exp), r=[K(ch, "gcum")], w=[K(ch, "dcol")])
            CI("pool", lambda e, c=c: e.tensor_tensor(out=c["t1"], in0=c["DT"], in1=mUs, op=OP.mult), r=[K(ch, "DT"), "mUs"], w=[K(ch, "t1")])
            CI("pool", lambda e, c=c, cs=cs: e.tensor_tensor(out=c["t2"], in0=c["t1"], in1=brow[:, cs], op=OP.mult), r=[K(ch, "t1"), ("brow", par)], w=[K(ch, "t2")])
            CI("pool", lambda e, c=c: e.tensor_tensor(out=c["t3"], in0=c["DT"], in1=mU, op=OP.mult), r=[K(ch, "DT"), "mU"], w=[K(ch, "t3")])
            CI("dve", lambda e, c=c, cs=cs: e.tensor_tensor(out=c["vbT"], in0=vs[:, cs], in1=brow[:, cs], op=OP.mult), r=[("vs", par), ("brow", par)], w=[K(ch, "vbT")])
            CI("dve", lambda e, c=c, cs=cs: e.tensor_tensor(out=c["kbgT"], in0=kn[:, cs], in1=brow[:, cs], op=OP.mult), r=[("kn", par), ("brow", par)], w=[K(ch, "kbgT")])
            CI("dve", lambda e, c=c: e.tensor_tensor(out=c["kbgT"], in0=c["kbgT"], in1=c["eg"], op=OP.mult), r=[K(ch, "kbgT"), K(ch, "eg")], w=[K(ch, "kbgT")])
            CI("pool", lambda e, c=c, cs=cs: e.tensor_tensor(out=c["qdT"], in0=qn[:, cs], in1=c["eg"], op=OP.mult), r=[("qn", par), K(ch, "eg")], w=[K(ch, "qdT")])
            CI("pool", lambda e, c=c, cs=cs: e.tensor_tensor(out=c["kdT"], in0=kn[:, cs], in1=c["ekd"], op=OP.mult), r=[("kn", par), K(ch, "ekd")], w=[K(ch, "kdT")])
            pt, pk = pslot()
            CI("pe", lambda e, cs=cs, pt=pt: e.matmul(pt, lhsT=kn[:, cs], rhs=kn[:, cs], start=True, stop=True), r=[("kn", par)], w=[pk])
            CI("dve", lambda e, c=c, pt=pt: e.tensor_tensor(out=c["B"], in0=pt, in1=c["t2"], op=OP.mult), r=[pk, K(ch, "t2")], w=[K(ch, "B")])
            pt, pk = pslot()
            CI("pe", lambda e, cs=cs, pt=pt: e.matmul(pt, lhsT=kn[:, cs], rhs=qn[:, cs], start=True, stop=True), r=[("kn", par), ("qn", par)], w=[pk])
            CI("dve", lambda e, c=c, pt=pt: e.tensor_tensor(out=c["attnT"], in0=pt, in1=c["t3"], op=OP.mult), r=[pk, K(ch, "t3")], w=[K(ch, "attnT")])
            pt, pk = pslot()
            CI("pe", lambda e, c=c, pt=pt: e.matmul(pt, lhsT=c["B"], rhs=ident, start=True, stop=True), r=[K(ch, "B"), "ident"], w=[pk])
            CI("act", lambda e, c=c, pt=pt: e.activation(out=c["Am"], in_=pt, func=AF.Copy), r=[pk], w=[K(ch, "Am")])
            CI("dve", lambda e, c=c: e.tensor_tensor(out=c["M"], in0=ident, in1=c["B"], op=OP.subtract), r=["ident", K(ch, "B")], w=[K(ch, "M")])
            for (srcn, dstn) in (("kbgT", "kbg"), ("vbT", "vb"), ("kdT", "kd")):
                pt, pk = pslot()
                CI("pe", lambda e, c=c, pt=pt, srcn=srcn: e.matmul(pt, lhsT=c[srcn], rhs=ident, start=True, stop=True), r=[K(ch, srcn), "ident"], w=[pk])
                CI("act", lambda e, c=c, pt=pt, dstn=dstn: e.activation(out=c[dstn], in_=pt, func=AF.Copy), r=[pk], w=[K(ch, dstn)])
        cur = [("Am", "B")] * NCH
        for lev in range(6):
            for ch in range(NCH):
                c = CB[ch]
                an, bn = cur[ch]
                na, nb = ("A2", "B2") if an == "Am" else ("Am", "B")
                pt, pk = pslot()
                CI("pe", lambda e, c=c, pt=pt, an=an, bn=bn: e.matmul(pt, lhsT=c[bn], rhs=c[an], start=True, stop=True),
                    r=[K(ch, an), K(ch, bn)], w=[pk])
                pt2, pk2 = (None, None)
                if lev < 5:
                    pt2, pk2 = pslot()
                    CI("pe", lambda e, c=c, pt2=pt2, an=an, bn=bn: e.matmul(pt2, lhsT=c[an], rhs=c[bn], start=True, stop=True),
                        r=[K(ch, an), K(ch, bn)], w=[pk2])
                CI("act", lambda e, c=c, pt=pt, na=na: e.activation(out=c[na], in_=pt, func=AF.Copy), r=[pk], w=[K(ch, na)])
                if lev < 5:
                    CI("dve", lambda e, c=c, pt2=pt2, nb=nb: e.tensor_copy(out=c[nb], in_=pt2), r=[pk2], w=[K(ch, nb)])
                pt3, pk3 = pslot()
                CI("pe", lambda e, c=c, pt3=pt3, na=na: e.matmul(pt3, lhsT=c[na], rhs=c["M"], start=True, stop=True),
                    r=[K(ch, na), K(ch, "M")], w=[pk3])
                CI("dve", lambda e, c=c, pt3=pt3: e.tensor_tensor(out=c["M"], in0=pt3, in1=c["M"], op=OP.add), r=[pk3, K(ch, "M")], w=[K(ch, "M")])
                cur[ch] = (na, nb)
        for ch in range(NCH):
            c = CB[ch]
            pt, pk = pslot()
            CI("pe", lambda e, c=c, pt=pt: e.matmul(pt, lhsT=c["kbg"], rhs=c["M"], start=True, stop=True), r=[K(ch, "kbg"), K(ch, "M")], w=[pk])
            CI("act", lambda e, c=c, pt=pt: e.activation(out=c["wT"], in_=pt, func=AF.Copy), r=[pk], w=[K(ch, "wT")])
            pt, pk = pslot()
            CI("pe", lambda e, c=c, pt=pt: e.matmul(pt, lhsT=c["M"], rhs=c["vb"], start=True, stop=True), r=[K(ch, "vb"), K(ch, "M")], w=[pk])
            CI("act", lambda e, c=c, pt=pt: e.activation(out=c["u"], in_=pt, func=AF.Copy), r=[pk], w=[K(ch, "u")])
        for ch in range(NCH):
            c = CB[ch]
            cs = slice(ch * 128, (ch + 1) * 128)
            n = blk * NCH + ch
            Sc, Sn = S[n % 2], S[(n + 1) % 2]
            kSc, kSn = ("S", n % 2), ("S", (n + 1) % 2)
            sm = c["small"]
            dcol = sm[:, 3:4]; osq = sm[:, 4:5]; orstd = sm[:, 5:6]
            p_ws, p_o, p_ks, p_t = (psb[5][:, q * 128:(q + 1) * 128] for q in range(4))
            CI("pe", lambda e, c=c, Sc=Sc, p_ws=p_ws: e.matmul(p_ws, lhsT=c["wT"], rhs=Sc, start=True, stop=True), r=[K(ch, "wT"), kSc], w=[("bank", 5)])
            CI("dve", lambda e, c=c, p_ws=p_ws: e.tensor_tensor(out=c["vnew"], in0=c["u"], in1=p_ws, op=OP.subtract), r=[K(ch, "u"), ("bank", 5)], w=[K(ch, "vnew")])
            CI("pe", lambda e, c=c, Sc=Sc, p_o=p_o: e.matmul(p_o, lhsT=c["qdT"], rhs=Sc, start=True, stop=False), r=[K(ch, "qdT"), kSc, K(ch, "vnew"), K(ch, "attnT")], w=[("bank", 5)])
            CI("pe", lambda e, c=c, p_o=p_o: e.matmul(p_o, lhsT=c["attnT"], rhs=c["vnew"], start=False, stop=True), r=[K(ch, "attnT"), K(ch, "vnew")], w=[("bank", 5)])
            CI("pe", lambda e, c=c, p_ks=p_ks: e.matmul(p_ks, lhsT=c["kd"], rhs=c["vnew"], start=True, stop=True), r=[K(ch, "kd"), K(ch, "vnew")], w=[("bank", 5)])
            CI("dve", lambda e, Sc=Sc, Sn=Sn, dcol=dcol, p_ks=p_ks: e.scalar_tensor_tensor(out=Sn, in0=Sc, scalar=dcol, in1=p_ks, op0=OP.mult, op1=OP.add),
                r=[kSc, K(ch, "dcol"), ("bank", 5)], w=[kSn])
            CI("act", lambda e, c=c, p_o=p_o, osq=osq: e.activation(out=c["on"], in_=p_o, func=AF.Copy), r=[("bank", 5)], w=[K(ch, "on")])
            CI("act", lambda e, c=c, osq=osq: e.activation(out=c["t1"], in_=c["on"], func=AF.Square, accum_out=osq), r=[K(ch, "on")], w=[K(ch, "t1"), K(ch, "osq")])
            CI("dve", lambda e, osq=osq, orstd=orstd: e.tensor_scalar(out=orstd, in0=osq, scalar1=1.0 / 128, scalar2=EPS, op0=OP.mult, op1=OP.add), r=[K(ch, "osq")], w=[K(ch, "orstd")])
            CI("act", lambda e, orstd=orstd: e.activation(out=orstd, in_=orstd, func=AF.Sqrt), r=[K(ch, "orstd")], w=[K(ch, "orstd")])
            CI("dve", lambda e, orstd=orstd: e.reciprocal(out=orstd, in_=orstd), r=[K(ch, "orstd")], w=[K(ch, "orstd")])
            CI("dve", lambda e, c=c, orstd=orstd: e.tensor_scalar(out=c["t2"], in0=c["on"], scalar1=orstd, scalar2=None, op0=OP.mult), r=[K(ch, "on"), K(ch, "orstd")], w=[K(ch, "t2")])
            pt, pk = pslot()
            CI("pe", lambda e, c=c, pt=pt: e.matmul(pt, lhsT=c["t2"], rhs=ident, start=True, stop=True), r=[K(ch, "t2"), "ident"], w=[pk])
            CI("dve", lambda e, pt=pt, cs=cs, sl=sl: e.scalar_tensor_tensor(out=ystage[sl][:, 0, cs], in0=pt, scalar=col(PV_GNW), in1=zs[:, cs], op0=OP.mult, op1=OP.mult),
                r=[pk, "pvt", ("zs", par)], w=[("ystage", sl)])
        for g in range(2):
            P.DMA("sp", lambda e, blk=blk, sl=sl, g=g: e.dma_start(
                out=cin[blk // 4].rearrange("(i g p) t -> p i g t", i=16, g=2)[:, (blk % 4) * 4:(blk % 4) * 4 + 4, g, :],
                in_=ystage[sl][:, g, :].rearrange("p (i t) -> p i t", i=4)),
                  r=[("ystage", sl)], w=["cin"], ch=("yst", sl))


    load_x(0, P.DMA)
    front(0, P.I, P.DMA)
    for blk in range(NBLK):
        if blk + 1 < NBLK:
            front(blk + 1, Idef, DMAdef)
        chunks(blk)
        while pending:
            kind, a2, k2 = pending.popleft()
            getattr(P, kind)(*a2, **k2)


def phase2(nc, P, A, G):
    psb = G["psb"]; ident = G["ident"]; identb = G["identb"]; ones = G["ones"]; onesb = G["onesb"]
    pvt = G["pvt"]; col = G["col"]; debug = G["debug"]
    PV_NXA, PV_NMEM = 16, 24
    x_tok = G["x_tok"]; mem_b = G["mem_b"]; coutflat = G["coutflat"]; idxy = G["idxy"]
    NT = G.get("ntile", 16)

    def wload(dram, ncols, key):
        t = A.bf16(8 * ncols).rearrange("p (k c) -> p k c", k=8)
        for k in range(8):
            P.DMA("pool", lambda e, k=k, t=t: e.dma_start(out=t[:, k, :], in_=dram[k * 128:(k + 1) * 128, :]), w=[key], ch=key)
        return t
    woutb = wload(G["w_out"], 1024, "woutb")
    wqb = wload(G["xwq"], 1024, "wqb")
    wob = wload(G["xwo"], 1024, "wob")
    big = A.bf16(8 * 2048).rearrange("p (k c) -> p k c", k=8)
    for k in range(8):
        P.DMA("pool", lambda e, k=k: e.dma_start(out=big[:, k, :], in_=G["xwkv"][k * 128:(k + 1) * 128, :]), w=["big"], ch="big")
    skt = A.f32(2048).rearrange("p (c k) -> p c k", c=16)
    P.DMA("sp", lambda e: e.dma_start(out=skt, in_=G["skT"].rearrange("p (c k) -> p c k", c=16)), w=["skt"], ch="skt")
    iy = A.u32(8)
    P.DMA("sp", lambda e: e.dma_start(out=iy, in_=idxy[:, :]), w=["iy"], ch="iy")
    h3acc = A.f32(2048)
    rowt = h3acc
    P.DMA("sp", lambda e: e.dma_start(out=rowt[0:1, :], in_=G["rows"].rearrange("a d -> (a d)").unsqueeze(0)), w=["rowt"], ch="rowt")
    wbc = A.f32(2048)
    for i in range(4):
        P.I("pe", lambda e, i=i: e.matmul(psb[0][:, :], lhsT=ones[0:1, 0:128], rhs=rowt[0:1, i * 512:(i + 1) * 512], start=True, stop=True),
            r=["ones", "rowt"], w=[("bank", 0)])
        P.I("act", lambda e, i=i: e.activation(out=wbc[:, i * 512:(i + 1) * 512], in_=psb[0][:, :], func=AF.Copy), r=[("bank", 0)], w=["wbc"])
    iota16 = A.f32(256)
    P.I("pool", lambda e: e.iota(iota16, pattern=[[1, 256]], base=0, channel_multiplier=0, allow_small_or_imprecise_dtypes=True), w=["iota"])

    kT = A.bf16(8 * 256).rearrange("p (c m) -> p c m", c=8)
    vv = A.bf16(2 * 1024).rearrange("p (m c) -> p m c", m=2)
    xt = A.f32(1024); junk = A.f32(1024); h3 = h3acc[:, 0:1024]; acc = h3acc[:, 1024:2048]
    xnb = A.bf16(1024)
    hT = A.bf16(8 * 128).rearrange("p (k t) -> p k t", k=8)
    yT = A.bf16(8 * 128).rearrange("p (k t) -> p k t", k=8)
    qT = A.bf16(8 * 128).rearrange("p (k t) -> p k t", k=8)
    oT = A.bf16(8 * 128).rearrange("p (k t) -> p k t", k=8)
    expT = A.bf16(2 * 128).rearrange("p (m t) -> p m t", m=2)
    rden = A.f32(128)
    sm = A.f32(8)
    qsall = A.f32(4096)
    qpT = qsall[:, 0:2048].rearrange("p (c t) -> p c t", c=16)
    scs = qsall[:, 2048:4096].rearrange("p (c k) -> p c k", c=16)
    sct = A.f32(128)
    tv = A.f32(256).rearrange("p (c k) -> p c k", c=16)
    tiu = A.u32(256).rearrange("p (c k) -> p c k", c=16)
    tif = A.f32(256).rearrange("p (c k) -> p c k", c=16)
    cand = A.f32(256); cand2 = A.f32(256); cidx = A.f32(256)
    best = A.f32(16); posu = A.u32(16); posf = A.f32(16)
    oh = qsall.rearrange("p (k a) -> p k a", k=16)
    idxf = A.f32(128); idxu = A.u32(128); gate = A.f32(128); sval = A.f32(128); act = A.f32(128)
    NB = 6
    gb = [A.bf16(1024) for _ in range(NB)]
    junkb2 = A.bf16(1024)
    ps_all = G["ps_all"]
    h3p = ps_all[:, 0:1024]
    accp = [ps_all[:, 1024:2048], ps_all[:, 2048:3072]]
    accb = [[("bank", 2), ("bank", 3)], [("bank", 4), ("bank", 5)]]
    print("phase2 arena used", A.off, "of", A.n)
    pTb = psb[2][:, :].bitcast(BF16)

    def rms_to_hT(src, wcol0, key_src):
        P.I("act", lambda e: e.activation(out=junk, in_=src, func=AF.Square, accum_out=sm[:, 0:1]), r=[key_src], w=["junk", "sm0"])
        P.I("dve", lambda e: e.tensor_scalar(out=sm[:, 1:2], in0=sm[:, 0:1], scalar1=1.0 / D, scalar2=EPS, op0=OP.mult, op1=OP.add), r=["sm0"], w=["sm1"])
        P.I("act", lambda e: e.activation(out=sm[:, 1:2], in_=sm[:, 1:2], func=AF.Sqrt), r=["sm1"], w=["sm1"])
        P.I("dve", lambda e: e.reciprocal(out=sm[:, 2:3], in_=sm[:, 1:2]), r=["sm1"], w=["rstd"])
        if wcol0 is None:
            return
        P.I("dve", lambda e: e.tensor_scalar(out=xnb, in0=src, scalar1=sm[:, 2:3], scalar2=None, op0=OP.mult), r=[key_src, "rstd"], w=["xnb"])
        for k in range(8):
            P.I("pe", lambda e, k=k: e.transpose(out=pTb[:, (k % 4) * 128:(k % 4 + 1) * 128], in_=xnb[:, k * 128:(k + 1) * 128], identity=identb),
                r=["xnb", "identb"], w=[("bank", 2)])
            P.I("dve", lambda e, k=k: e.tensor_scalar(out=hT[:, k, :], in0=pTb[:, (k % 4) * 128:(k % 4 + 1) * 128], scalar1=col(wcol0 + k), scalar2=None, op0=OP.mult),
                r=[("bank", 2), "pvt"], w=["hT"])

    for mt in range(2):
        P.DMA("sp", lambda e, mt=mt: e.dma_start(out=xt, in_=mem_b[mt * 128:(mt + 1) * 128, :]), w=[("xt", 0)], ch="xtmem")
        rms_to_hT(xt, PV_NMEM, ("xt", 0))
        for half in range(2):
            for k in range(8):
                P.I("pe", lambda e, k=k, half=half: e.matmul(psb[half][:, :], lhsT=hT[:, k, :], rhs=big[:, k, 1024 + half * 512:1024 + (half + 1) * 512],
                                                             start=(k == 0), stop=(k == 7)), r=["hT", "big"], w=[("bank", half)])
            P.I("act", lambda e, half=half, mt=mt: e.activation(out=vv[:, mt, half * 512:(half + 1) * 512], in_=psb[half][:, :], func=AF.Copy), r=[("bank", half)], w=["vv"])
        for c in range(8):
            pb = 3 + c % 2
            for k in range(8):
                P.I("pe", lambda e, k=k, c=c, pb=pb: e.matmul(psb[pb][:, 0:128], lhsT=big[:, k, c * 128:(c + 1) * 128], rhs=hT[:, k, :], start=(k == 0), stop=(k == 7)),
                    r=["hT", "big"], w=[("bank", pb)])
            P.I("act", lambda e, c=c, pb=pb, mt=mt: e.activation(out=kT[:, c, mt * 128:(mt + 1) * 128], in_=psb[pb][:, 0:128], func=AF.Copy), r=[("bank", pb)], w=["kT"])
    for k in range(8):
        P.DMA("pool", lambda e, k=k: e.dma_start(out=big[:, k, :], in_=G["pwq"][k * 128:(k + 1) * 128, :]), r=[], w=["big"], ch="big2")

    cflat = coutflat
    import collections
    xts = [xt, A.f32(1024)]
    gates = [gate, A.f32(128)]
    idxus = [idxu, A.u32(128)]
    smA = sm
    smG = A.f32(8)
    idxTs = [A.u32(128), A.u32(128)]
    actT = A.bf16(128)
    accTs = A.f32(1024)
    accT = ps_all[:, 1024:2048]
    accp1 = accp[0]
    accbk = accb[0]
    pT6 = psb[6][:, :].bitcast(BF16)
    print("phase2 arena used (pipelined)", A.off, "of", A.n)
    P.I("pe", lambda e: e.matmul(psb[4][:, 0:8], lhsT=ones[0:1, 0:128], rhs=ones[0:1, 0:8], start=True, stop=True),
        r=["woutb", "wqb", "wob", "big"], w=["wts", ("bank", 4)])

    def rms_stats(I, src, key_src, smx, tag, jout, jkeys):
        I("act", lambda e: e.activation(out=jout, in_=src, func=AF.Square, accum_out=smx[:, 0:1]), r=[key_src], w=list(jkeys) + [tag + "0"])
        I("dve", lambda e: e.tensor_scalar(out=smx[:, 1:2], in0=smx[:, 0:1], scalar1=1.0 / D, scalar2=EPS, op0=OP.mult, op1=OP.add), r=[tag + "0"], w=[tag + "1"])
        I("act", lambda e: e.activation(out=smx[:, 1:2], in_=smx[:, 1:2], func=AF.Sqrt), r=[tag + "1"], w=[tag + "1"])
        I("dve", lambda e: e.reciprocal(out=smx[:, 2:3], in_=smx[:, 1:2]), r=[tag + "1"], w=[tag + "rstd"])

    def to_hT(I, wcol0):
        for k in range(8):
            I("pe", lambda e, k=k: e.transpose(out=pT6[:, (k % 4) * 128:(k % 4 + 1) * 128], in_=xnb[:, k * 128:(k + 1) * 128], identity=identb),
              r=["xnb", "identb"], w=[("bank", 6)])
            if wcol0 is None:
                I("dve", lambda e, k=k: e.tensor_copy(out=hT[:, k, :], in_=pT6[:, (k % 4) * 128:(k % 4 + 1) * 128]), r=[("bank", 6)], w=["hT"])
            else:
                I("dve", lambda e, k=k: e.tensor_scalar(out=hT[:, k, :], in0=pT6[:, (k % 4) * 128:(k % 4 + 1) * 128], scalar1=col(wcol0 + k), scalar2=None, op0=OP.mult),
                  r=[("bank", 6), "pvt"], w=["hT"])

    def resid_add(I, lhs, wmat, key_l, chunk_of, xt_, kx):
        for half in range(2):
            bk = 4 + half
            for k in range(8):
                I("pe", lambda e, k=k, half=half, bk=bk: e.matmul(psb[bk][:, :], lhsT=lhs[:, k, :], rhs=wmat[:, chunk_of(k), half * 512:(half + 1) * 512],
                                                                 start=(k == 0), stop=(k == 7)), r=[key_l, "wts"], w=[("bank", bk)])
            I("dve", lambda e, half=half, bk=bk: e.tensor_tensor(out=xt_[:, half * 512:(half + 1) * 512], in0=psb[bk][:, :], in1=xt_[:, half * 512:(half + 1) * 512], op=OP.add),
              r=[("bank", bk), kx], w=[kx])

    def stageA(it, par, I, DMA):
        xt_ = xts[par]; kx = ("xt", par); gate_ = gates[par]; idxu_ = idxus[par]; kg = ("gate", par); ki = ("idxu", par)
        DMA("sp", lambda e: e.dma_start(out=xt_, in_=x_tok[it * 128:(it + 1) * 128, :]), w=[kx], ch=("xt", par))
        for ag in range(8):
            DMA("pool", lambda e, ag=ag: e.indirect_dma_start(out=yT[:, ag, :], out_offset=None, in_=cflat,
                                                             in_offset=bass.IndirectOffsetOnAxis(ap=iy[:, ag:ag + 1], axis=0),
                                                             element_offset=it * 256 * 128),
                r=["cout", "iy"], w=["yT"], ch="yT")
        resid_add(I, yT, woutb, "yT", lambda k: (k // 2) + 4 * (k % 2), xt_, kx)
        if debug:
            DMA("sp", lambda e: e.dma_start(out=G["dbg"][0, it * 128:(it + 1) * 128, :], in_=xt_), r=[kx], w=["dbg0"], ch="dbg0")
        rms_stats(I, xt_, kx, smA, "smA", junk, ["junk"])
        I("dve", lambda e: e.tensor_scalar(out=xnb, in0=xt_, scalar1=smA[:, 2:3], scalar2=None, op0=OP.mult), r=[kx, "smArstd"], w=["xnb"])
        to_hT(I, PV_NXA)
        for c in range(8):
            pb = 6 + c % 2
            for k in range(8):
                I("pe", lambda e, k=k, c=c, pb=pb: e.matmul(psb[pb][:, 0:128], lhsT=wqb[:, k, c * 128:(c + 1) * 128], rhs=hT[:, k, :], start=(k == 0), stop=(k == 7)),
                  r=["hT", "wts"], w=[("bank", pb)])
            I("act", lambda e, c=c, pb=pb: e.activation(out=qT[:, c, :], in_=psb[pb][:, 0:128], func=AF.Copy), r=[("bank", pb)], w=["qT"])
        for h in range(4):
            for mc in range(2):
                for dc in range(2):
                    I("pe", lambda e, h=h, mc=mc, dc=dc: e.matmul(psb[6][:, mc * 128:(mc + 1) * 128], lhsT=kT[:, 2 * h + dc, mc * 128:(mc + 1) * 128], rhs=qT[:, 2 * h + dc, :],
                                                                  start=(dc == 0), stop=(dc == 1)), r=["kT", "qT"], w=[("bank", 6)])
            I("act", lambda e: e.activation(out=expT.rearrange("p m t -> p (m t)"), in_=psb[6][:, 0:256], func=AF.Exp, scale=1.0 / 16.0), r=[("bank", 6)], w=["expT"])
            for mc in range(2):
                I("pe", lambda e, mc=mc: e.matmul(psb[7][:, 0:128], lhsT=onesb, rhs=expT[:, mc, :], start=(mc == 0), stop=(mc == 1)), r=["expT", "onesb"], w=[("bank", 7)])
            I("dve", lambda e: e.reciprocal(out=rden, in_=psb[7][:, 0:128]), r=[("bank", 7)], w=["rden"])
            for dc in range(2):
                for mc in range(2):
                    I("pe", lambda e, h=h, mc=mc, dc=dc: e.matmul(psb[4][:, 0:128], lhsT=vv[:, mc, (2 * h + dc) * 128:(2 * h + dc + 1) * 128], rhs=expT[:, mc, :],
                                                                  start=(mc == 0), stop=(mc == 1)), r=["expT", "vv"], w=[("bank", 4)])
                I("dve", lambda e, h=h, dc=dc: e.tensor_tensor(out=oT[:, 2 * h + dc, :], in0=psb[4][:, 0:128], in1=rden, op=OP.mult), r=[("bank", 4), "rden"], w=["oT"])
        resid_add(I, oT, wob, "oT", lambda k: k, xt_, kx)
        if debug:
            DMA("sp", lambda e: e.dma_start(out=G["dbg"][1, it * 128:(it + 1) * 128, :], in_=xt_), r=[kx], w=["dbg1"], ch="dbg1")
        rms_stats(I, xt_, kx, smA, "smA", junk, ["junk"])
        I("dve", lambda e: e.scalar_tensor_tensor(out=h3, in0=xt_, scalar=smA[:, 2:3], in1=wbc[:, 0:1024], op0=OP.mult, op1=OP.mult), r=[kx, "smArstd", "wbc"], w=["h3"])
        I("act", lambda e: e.activation(out=xnb, in_=h3, func=AF.Copy), r=["h3"], w=["xnb"])
        to_hT(I, None)
        for c in range(16):
            pb = 6 + c % 2
            for k in range(8):
                I("pe", lambda e, k=k, c=c, pb=pb: e.matmul(psb[pb][:, 0:128], lhsT=big[:, k, c * 128:(c + 1) * 128], rhs=hT[:, k, :], start=(k == 0), stop=(k == 7)),
                  r=["hT", "wts"], w=[("bank", pb)])
            I("act", lambda e, c=c, pb=pb: e.activation(out=qpT[:, c, :], in_=psb[pb][:, 0:128], func=AF.Copy), r=[("bank", pb)], w=["qpT"])
        for c in range(16):
            pb = 4 + c % 2
            I("pe", lambda e, c=c, pb=pb: e.matmul(psb[pb][:, 0:128], lhsT=qpT[:, c, :], rhs=skt[:, c, :], start=True, stop=True), r=["qpT", "skt"], w=[("bank", pb)])
            I("act", lambda e, c=c, pb=pb: e.activation(out=scs[:, c, :], in_=psb[pb][:, 0:128], func=AF.Copy), r=[("bank", pb)], w=[("scs", c)])
            I("dve", lambda e, c=c: e.max(out=tv[:, c, 0:8], in_=scs[:, c, :]), r=[("scs", c)], w=[("tv", c)])
            I("dve", lambda e, c=c: e.max_index(out=tiu[:, c, 0:8], in_max=tv[:, c, 0:8], in_values=scs[:, c, :]), r=[("scs", c), ("tv", c)], w=[("tiu", c)])
            I("dve", lambda e, c=c: e.match_replace(out=sct, in_to_replace=tv[:, c, 0:8], in_values=scs[:, c, :], imm_value=-1e30), r=[("scs", c), ("tv", c)], w=["sct"])
            I("dve", lambda e, c=c: e.max(out=tv[:, c, 8:16], in_=sct), r=["sct"], w=[("tv", c)])
            I("dve", lambda e, c=c: e.max_index(out=tiu[:, c, 8:16], in_max=tv[:, c, 8:16], in_values=sct), r=["sct", ("tv", c)], w=[("tiu", c)])
            I("dve", lambda e, c=c: e.tensor_copy(out=tif[:, c, :], in_=tiu[:, c, :]), r=[("tiu", c)], w=[("tif", c)])
        for h in range(8):
            c1, c2 = 2 * h, 2 * h + 1
            c3 = cand.rearrange("p (a b) -> p a b", a=16)
            I("dve", lambda e, c1=c1, c2=c2, c3=c3: e.tensor_tensor(out=c3, in0=tv[:, c1, :].unsqueeze(2).broadcast_to([128, 16, 16]),
                                                                    in1=tv[:, c2, :].unsqueeze(1).broadcast_to([128, 16, 16]), op=OP.add),
              r=[("tv", c1), ("tv", c2)] + [("scs", c) for c in range(16)], w=["cand"])
            I("dve", lambda e, c1=c1, c2=c2: e.scalar_tensor_tensor(out=cidx.rearrange("p (a b) -> p a b", a=16), in0=tif[:, c1, :].unsqueeze(2).broadcast_to([128, 16, 16]), scalar=128.0,
                                                                    in1=tif[:, c2, :].unsqueeze(1).broadcast_to([128, 16, 16]), op0=OP.mult, op1=OP.add),
              r=[("tif", c1), ("tif", c2)], w=["cidx"])
            I("dve", lambda e: e.max(out=best[:, 0:8], in_=cand), r=["cand"], w=["best"])
            I("dve", lambda e: e.max_index(out=posu[:, 0:8], in_max=best[:, 0:8], in_values=cand), r=["cand", "best"], w=["posu"])
            I("dve", lambda e: e.match_replace(out=cand2, in_to_replace=best[:, 0:8], in_values=cand, imm_value=-1e30), r=["cand", "best"], w=["cand2"])
            I("dve", lambda e: e.max(out=best[:, 8:16], in_=cand2), r=["cand2"], w=["best"])
            I("dve", lambda e: e.max_index(out=posu[:, 8:16], in_max=best[:, 8:16], in_values=cand2), r=["cand2", "best"], w=["posu"])
            I("dve", lambda e: e.tensor_copy(out=posf, in_=posu), r=["posu"], w=["posf"])
            I("dve", lambda e: e.tensor_tensor(out=oh, in0=iota16.unsqueeze(1).broadcast_to([128, 16, 256]), in1=posf.unsqueeze(2).broadcast_to([128, 16, 256]), op=OP.is_equal),
              r=["iota", "posf", "qpT"] + [("scs", c) for c in range(16)], w=["oh"])
            I("dve", lambda e: e.tensor_tensor(out=oh, in0=oh, in1=cidx.unsqueeze(1).broadcast_to([128, 16, 256]), op=OP.mult), r=["oh", "cidx"], w=["oh"])
            I("dve", lambda e, h=h: e.tensor_reduce(out=idxf[:, h * 16:(h + 1) * 16], in_=oh, axis=AX.X, op=OP.add), r=["oh"], w=["idxf"])
            I("dve", lambda e: e.tensor_scalar(out=smA[:, 3:4], in0=best[:, 0:1], scalar1=-1.0, scalar2=None, op0=OP.mult), r=["best"], w=["nmax"])
            I("act", lambda e, h=h: e.activation(out=gate_[:, h * 16:(h + 1) * 16], in_=best, func=AF.Exp, bias=smA[:, 3:4], accum_out=smA[:, 4:5]), r=["best", "nmax"], w=[kg, "gsum"])
            I("dve", lambda e: e.reciprocal(out=smA[:, 5:6], in_=smA[:, 4:5]), r=["gsum"], w=["grs"])
            I("dve", lambda e, h=h: e.tensor_scalar(out=gate_[:, h * 16:(h + 1) * 16], in0=gate_[:, h * 16:(h + 1) * 16], scalar1=smA[:, 5:6], scalar2=None, op0=OP.mult), r=[kg, "grs"], w=[kg])
        I("dve", lambda e: e.tensor_copy(out=idxu_, in_=idxf), r=["idxf"], w=[ki])
        idxT_ = idxTs[par]
        I("pe", lambda e: e.matmul(psb[7][:, 0:128], lhsT=idxf, rhs=ident, start=True, stop=True), r=["idxf", "ident"], w=[("bank", 7)])
        I("dve", lambda e: e.tensor_copy(out=idxT_, in_=psb[7][:, 0:128]), r=[("bank", 7)], w=[("idxT", par)])

    def stageG(it, par, pump):
        xt_ = xts[par]; kx = ("xt", par); gate_ = gates[par]; idxu_ = idxus[par]; kg = ("gate", par); ki = ("idxu", par)
        P.I("dve", lambda e: e.tensor_copy(out=h3p, in_=h3), r=["h3"], w=[("bank", 0), ("bank", 1)])
        for j in range(128):
            sb = j % NB
            b = gb[sb]
            P.DMA("pool", lambda e, j=j, b=b: e.indirect_dma_start(out=b, out_offset=None, in_=G["ub"].ap()[:, :],
                                                                   in_offset=bass.IndirectOffsetOnAxis(ap=idxu_[:, j:j + 1], axis=0)),
                  r=[ki, "ub"], w=[("gb", sb)], ch=("gb", sb))
            P.I("dve", lambda e, j=j, b=b: e.scalar_tensor_tensor(out=junkb2, in0=b, scalar=1.0, in1=h3p, op0=OP.mult, op1=OP.mult, accum_out=sval[:, j:j + 1]),
                r=[("gb", sb)], w=[("sval", j)], ro=[("bank", 0), ("bank", 1)])
            pump()
        P.I("act", lambda e: e.activation(out=act, in_=sval, func=AF.Gelu_apprx_tanh), r=[("sval", j) for j in range(128)], w=["act"])
        P.I("dve", lambda e: e.tensor_tensor(out=act, in0=act, in1=gate_, op=OP.mult), r=["act", kg], w=["act"])
        idxT_ = idxTs[par]; kiT = ("idxT", par)
        P.I("pe", lambda e: e.matmul(psb[2][:, 0:128], lhsT=act, rhs=ident, start=True, stop=True), r=["act", "ident"], w=[("bank", 2)])
        P.I("act", lambda e: e.activation(out=actT, in_=psb[2][:, 0:128], func=AF.Copy), r=[("bank", 2)], w=["actT"])
        for t in range(128):
            sb = t % NB
            b = gb[sb]
            P.DMA("pool", lambda e, t=t, b=b: e.indirect_dma_start(out=b, out_offset=None, in_=G["vb"].ap()[:, :],
                                                                   in_offset=bass.IndirectOffsetOnAxis(ap=idxT_[:, t:t + 1], axis=0)),
                  r=[kiT, "vb"], w=[("gb", sb)], ch=("gb", sb))
            for c in range(8):
                P.I("pe", lambda e, t=t, b=b, c=c: e.matmul(accT[:, c * 128 + t:c * 128 + t + 1], lhsT=b[:, c * 128:(c + 1) * 128], rhs=actT[:, t:t + 1], start=True, stop=True),
                    r=[("gb", sb), "actT"], w=[("bank", 2 + c // 4)])
            pump()
        P.I("act", lambda e: e.activation(out=accTs, in_=accT, func=AF.Copy), r=[("bank", 2), ("bank", 3)], w=["accTs"])
        for c in range(8):
            P.I("pe", lambda e, c=c: e.matmul(psb[c // 4][:, (c % 4) * 128:(c % 4 + 1) * 128], lhsT=accTs[:, c * 128:(c + 1) * 128], rhs=ident, start=True, stop=True),
                r=["accTs", "ident"], w=[("bank", c // 4)])
        P.I("dve", lambda e: e.tensor_tensor(out=xt_, in0=h3p, in1=xt_, op=OP.add), r=[("bank", 0), ("bank", 1), kx], w=[kx])
        if debug:
            P.DMA("sp", lambda e: e.dma_start(out=G["dbg"][2, it * 128:(it + 1) * 128, :], in_=xt_), r=[kx], w=["dbg2"], ch="dbg2")
        rms_stats(P.I, xt_, kx, smG, "smG", h3p, [("bank", 0), ("bank", 1)])
        P.I("dve", lambda e: e.scalar_tensor_tensor(out=acc, in0=xt_, scalar=smG[:, 2:3], in1=wbc[:, 1024:2048], op0=OP.mult, op1=OP.mult), r=[kx, "smGrstd", "wbc"], w=["acc"])
        P.DMA("sp", lambda e: e.dma_start(out=G["out"][it * 128:(it + 1) * 128, :], in_=acc), r=["acc"], w=["outd"], ch="outd")

    pending = collections.deque()

    def Idef(*a, **k):
        pending.append(("I", a, k))

    def DMAdef(*a, **k):
        pending.append(("DMA", a, k))

    npump = [3]

    def pump(n=None):
        for _ in range(npump[0] if n is None else n):
            if not pending:
                return
            kind, a, k = pending.popleft()
            getattr(P, kind)(*a, **k)

    stageA(0, 0, P.I, P.DMA)
    for it in range(NT):
        if it + 1 < NT:
            stageA(it + 1, (it + 1) % 2, Idef, DMAdef)
            npump[0] = len(pending) // 250 + 1
        stageG(it, it % 2, pump)
        while pending:
            pump(1000)


def prep_inputs(inp):
    f = lambda a: np.ascontiguousarray(np.asarray(a, dtype=np.float32))
    x = f(inp["x"]); mem = f(inp["mem"])
    w_in = f(inp["w_in"])[0]
    cq = f(inp["conv_qkv_w"])[0]; lcw = f(inp["lru_conv_w"])[0]
    maps = []
    for c in range(NCORES):
        b, j = c // 4, c % 4
        cols = []
        for base in (0, 512, 1024, 2056, 2568, 1536):
            cols.append(w_in[:, base + j * 128: base + (j + 1) * 128])
        cols.append(np.repeat(w_in[:, 2048 + j:2049 + j], 128, axis=1))
        cols.append(np.repeat(w_in[:, 2052 + j:2053 + j], 128, axis=1))
        w1 = np.ascontiguousarray(np.concatenate(cols, axis=1))
        cwm = np.zeros((128, 16), np.float32)
        for s_, base in enumerate((0, 512, 1024)):
            cwm[:, 4 * s_:4 * s_ + 4] = cq[:, base + j * 128: base + (j + 1) * 128].T
        cwm[:, 12:16] = lcw[:, j * 128:(j + 1) * 128].T
        pvm = np.zeros((128, 40), np.float32)
        sl = slice(j * 128, (j + 1) * 128)
        pvm[:, 0] = f(inp["lru_conv_b"])[0, sl]
        pvm[:, 1] = f(inp["lru_ba"])[0, sl]
        pvm[:, 2] = f(inp["lru_bx"])[0, sl]
        pvm[:, 3] = f(inp["lru_lambda"])[0, sl]
        pvm[:, 4] = f(inp["gdn_a_log"])[0, j]
        pvm[:, 5] = f(inp["gdn_dt_bias"])[0, j]
        pvm[:, 6] = f(inp["gdn_norm_w"])[0]
        pvm[:, 8:16] = f(inp["norm_mix_w"])[0].reshape(8, 128).T
        pvm[:, 16:24] = f(inp["norm_xattn_w"])[0].reshape(8, 128).T
        pvm[:, 24:32] = f(inp["norm_mem_w"])[0].reshape(8, 128).T
        lwm = np.stack([f(inp["lru_wa"])[0, 2 * j:2 * j + 2], f(inp["lru_wx"])[0, 2 * j:2 * j + 2]])
        skT = np.ascontiguousarray(f(inp["peer_subkeys"])[0].reshape(16, 128, 128).transpose(2, 0, 1).reshape(128, 16 * 128))
        maps.append({
            "x_b": x[b], "w1": w1, "cw": cwm, "pv": pvm, "lw": np.ascontiguousarray(lwm),
            "mem_b": mem[b], "w_out": f(inp["w_out"])[0], "xwq": f(inp["xattn_wq"])[0],
            "xwkv": f(inp["xattn_wkv"])[0], "xwo": f(inp["xattn_wo"])[0], "pwq": f(inp["peer_wq"])[0],
            "skT": skT, "peer_u": f(inp["peer_u"])[0], "peer_v": f(inp["peer_v"])[0],
            "rows": np.stack([f(inp["norm_ffn_w"])[0], f(inp["norm_final_w"])]),
            "x_tok": np.ascontiguousarray(x[b, j * TOK:(j + 1) * TOK]),
            "idxy": np.ascontiguousarray((j * 16384 + np.arange(4)[None, :, None] * 4096 + np.arange(2)[None, None, :] * 128
                                          + np.arange(128)[:, None, None]).reshape(128, 8).astype(np.uint32)),
        })
    return maps


_NC = {}


def kernel(**inputs):
    if "nc" not in _NC:
        _NC["nc"] = build(False)
    maps = prep_inputs(inputs)
    res = run_bass_kernel_spmd(_NC["nc"], maps, core_ids=list(range(NCORES)))
    o = np.concatenate([res.results[c]["out"] for c in range(NCORES)], axis=0)
    return o.reshape(2, SEQ, D).astype(np.float32)
```

```python
import contextlib
import numpy as np
import concourse.bass as bass
import concourse.mybir as mybir
from concourse.bass_utils import run_bass_kernel_spmd

F32 = mybir.dt.float32
BF16 = mybir.dt.bfloat16
U32 = mybir.dt.uint32
AF = mybir.ActivationFunctionType
OP = mybir.AluOpType
AX = mybir.AxisListType

NCORES = 8
D = 1024
SEQ = 8192
TOK = 2048
EPS = 1e-6
ENG = ("pe", "act", "dve", "pool", "sp")


class Prog:
    def __init__(self, nc):
        self.nc = nc
        self.ops = {e: [] for e in ENG}
        self.cnt = {}
        self.epoch = {e: 0 for e in ENG}
        self.waited = {}
        self.lastw = {}
        self.reads = {}
        self.semkeys = []
        self.sems = {}

    def _semkey_engine(self, e):
        k = ("E", e, self.epoch[e])
        if self.cnt.get(k, 0) >= 30000:
            self.epoch[e] += 1
            k = ("E", e, self.epoch[e])
        return k

    def _deps(self, eng, r, w, skip_same_pe):
        deps = {}
        def add(sv):
            if sv is None:
                return
            k, v = sv
            if skip_same_pe and k[0] == "E" and k[1] == "pe":
                return
            if deps.get(k, 0) < v:
                deps[k] = v
        for b in r:
            add(self.lastw.get(b))
        for b in w:
            add(self.lastw.get(b))
            for sv in self.reads.get(b, ()):
                add(sv)
        out = []
        for k, v in deps.items():
            if self.waited.get((eng, k), 0) < v:
                self.waited[(eng, k)] = v
                out.append((k, v))
        return out

    def _commit(self, r, w, sv):
        for b in r:
            self.reads.setdefault(b, []).append(sv)
        for b in w:
            self.lastw[b] = sv
            self.reads[b] = []

    def _newsem(self, k):
        if k not in self.cnt:
            self.cnt[k] = 0
            self.semkeys.append(k)

    def I(self, eng, fn, r=(), w=(), ro=()):
        bk = [b for b in r if isinstance(b, tuple) and b[0] == "bank"]
        r = list(r) + list(ro)
        if bk:
            r = [b for b in r if b not in bk]
            w = list(w) + bk
        waits = self._deps(eng, r, w, skip_same_pe=(eng == "pe"))
        k = self._semkey_engine(eng)
        self._newsem(k)
        self.cnt[k] += 1
        sv = (k, self.cnt[k])
        self._commit(r, w, sv)
        self.ops[eng].append((waits, fn, k, 1))

    def DMA(self, eng, fn, r=(), w=(), ch=None):
        waits = self._deps(eng, r, w, skip_same_pe=False)
        k = ("D", ch, 0)
        n = 0
        while self.cnt.get(("D", ch, n), 0) >= 30000:
            n += 1
        k = ("D", ch, n)
        self._newsem(k)
        self.cnt[k] += 16
        sv = (k, self.cnt[k])
        self._commit(r, w, sv)
        self.ops[eng].append((waits, fn, k, 16))

    def CC(self, eng, fn, r=(), w=()):
        waits = self._deps(eng, r, w, skip_same_pe=False)
        k = ("C", "cc", 0)
        self._newsem(k)
        self.cnt[k] += 1
        sv = (k, self.cnt[k])
        self._commit(r, w, sv)
        self.ops[eng].append((waits, fn, k, 1))

    def barrier(self):
        for e in ENG:
            waits = []
            for k, v in self.cnt.items():
                if v > 0 and self.waited.get((e, k), 0) < v:
                    self.waited[(e, k)] = v
                    waits.append((k, v))
            if waits:
                self.ops[e].append((waits, None, None, 0))

    def emit(self):
        nc = self.nc
        with contextlib.ExitStack() as st:
            for i, k in enumerate(self.semkeys):
                self.sems[k] = st.enter_context(nc.semaphore("s%d" % i))
            block = st.enter_context(nc.Block())
            engobj = {"pe": "tensor", "act": "scalar", "dve": "vector", "pool": "gpsimd", "sp": "sync"}

            def run(e, ename):
                for waits, fn, k, inc in self.ops[ename]:
                    for wk, wv in waits:
                        e.wait_ge(self.sems[wk], wv)
                    if fn is not None:
                        fn(e).then_inc(self.sems[k], inc)

            @block.tensor
            def _(e):
                run(e, "pe")

            @block.scalar
            def _(e):
                run(e, "act")

            @block.vector
            def _(e):
                run(e, "dve")

            @block.gpsimd
            def _(e):
                run(e, "pool")
            @block.sync
            def _(e):
                run(e, "sp")


class Arena:
    def __init__(self, t, n):
        self.t = t
        self.n = n
        self.off = 0
        self.mark = 0

    def f32(self, cols):
        o = self.off
        self.off += cols
        assert self.off <= self.n, ("arena overflow", self.off, self.n)
        return self.t[:, o:o + cols]

    def bf16(self, cols):
        c = (cols + 1) // 2
        return self.f32(c).bitcast(BF16)

    def u32(self, cols):
        return self.f32(cols).bitcast(U32)


def build(debug=False, p2=True, nblk=16, cc=True, stop=0, ntile=16):
    nc = bass.Bass("TRN2", target_bir_lowering=False)
    P = Prog(nc)

    def din(name, shape, dt=F32):
        return nc.dram_tensor(name, list(shape), dt, kind="ExternalInput").ap()

    x_b = din("x_b", [SEQ, D])
    w1 = din("w1", [D, 1024])
    cw = din("cw", [128, 16])
    pv = din("pv", [128, 40])
    lw = din("lw", [2, 2, 64, 64])
    if not p2:
        din = lambda *a, **k: None
    mem_b = din("mem_b", [256, D])
    w_out = din("w_out", [D, D])
    xwq = din("xwq", [D, D])
    xwkv = din("xwkv", [D, 2 * D])
    xwo = din("xwo", [D, D])
    pwq = din("pwq", [D, 2 * D])
    skT = din("skT", [128, 16 * 128])
    peer_u = din("peer_u", [16384, D])
    peer_v = din("peer_v", [16384, D])
    rows = din("rows", [2, D])
    out = nc.dram_tensor("out", [TOK, D], F32, kind="ExternalOutput").ap()
    cin_t = [nc.dram_tensor("cin%d" % q, [256, 2048], BF16) for q in range(4)]
    coutall = nc.dram_tensor("coutall", [4, 1024, 2048], BF16)
    cin = [t.ap().rearrange("r (x t) -> (r x) t", t=128) for t in cin_t]
    coutflat = coutall.ap().rearrange("q r (x t) -> (q r x) t", t=128)
    ub = nc.dram_tensor("ub", [16384, D], BF16) if p2 else None
    vb = nc.dram_tensor("vb", [16384, D], BF16) if p2 else None
    idxy = din("idxy", [128, 8], U32)
    x_tok = din("x_tok", [TOK, D])
    if debug:
        dbg = nc.dram_tensor("dbg", [3, TOK, D], F32, kind="ExternalOutput").ap()

    st = contextlib.ExitStack()
    NA = 49000
    arena_t = st.enter_context(nc.sbuf_tensor("arena", [128, NA], F32))
    ps_all = st.enter_context(nc.psum_tensor("ps_all", [128, 4096], F32))
    psb = [ps_all[:, i * 512:(i + 1) * 512] for i in range(8)]

    A = Arena(arena_t, NA)
    ident = A.f32(128)
    identb = A.bf16(128)
    ones = A.f32(512)
    onesb = A.bf16(128)
    mU = A.f32(128)
    mUs = A.f32(128)
    cwt = A.f32(16)
    pvt = A.f32(40)
    A.mark = A.off

    P.I("pool", lambda e: e.memset(ones, 1.0), w=["ones"])
    P.I("pool", lambda e: e.memset(onesb, 1.0), w=["onesb"])
    P.I("pool", lambda e: e.affine_select(out=ident, in_=ones[:, 0:128], pattern=[[-1, 128]],
                                          compare_op=OP.is_equal, fill=0.0, base=0, channel_multiplier=1),
        r=["ones"], w=["ident"])
    P.I("pool", lambda e: e.tensor_copy(out=identb, in_=ident), r=["ident"], w=["identb"])
    P.I("pool", lambda e: e.affine_select(out=mU, in_=ones[:, 0:128], pattern=[[1, 128]],
                                          compare_op=OP.is_ge, fill=0.0, base=0, channel_multiplier=-1),
        r=["ones"], w=["mU"])
    P.I("pool", lambda e: e.affine_select(out=mUs, in_=ones[:, 0:128], pattern=[[1, 128]],
                                          compare_op=OP.is_gt, fill=0.0, base=0, channel_multiplier=-1),
        r=["ones"], w=["mUs"])
    P.DMA("sp", lambda e: e.dma_start(out=cwt, in_=cw[:, :]), w=["cwt"], ch="c_cwt")
    P.DMA("sp", lambda e: e.dma_start(out=pvt, in_=pv[:, :]), w=["pvt"], ch="c_pvt")

    PV_CB, PV_BA, PV_BX, PV_LAM, PV_ALOG, PV_DTB, PV_GNW = 0, 1, 2, 3, 4, 5, 6
    PV_NMIX, PV_NXA, PV_NMEM = 8, 16, 24

    def col(i):
        return pvt[:, i:i + 1]

    phase1(nc, P, A, locals())
    P.barrier()
    if cc:
      for q in range(4):
        P.CC("pool", lambda e, q=q: e.collective_compute("AllGather", OP.bypass,
                                                        replica_groups=[[0, 1, 2, 3], [4, 5, 6, 7]],
                                                        ins=[cin_t[q].ap()], outs=[coutall.ap()[q]]),
             r=["cin"], w=["cout"])
    P.barrier()
    A.off = A.mark
    if p2:
        phase2(nc, P, A, locals())
    P.barrier()
    P.emit()
    st.close()
    return nc


def phase1(nc, P, A, G):
    psb = G["psb"]; ident = G["ident"]; identb = G["identb"]; ones = G["ones"]
    mU = G["mU"]; mUs = G["mUs"]; cwt = G["cwt"]; pvt = G["pvt"]; col = G["col"]
    x_b = G["x_b"]; w1 = G["w1"]; lw = G["lw"]; cin = G["cin"]
    PV_CB, PV_BA, PV_BX, PV_LAM, PV_ALOG, PV_DTB, PV_GNW, PV_NMIX = 0, 1, 2, 3, 4, 5, 6, 8

    w1b = A.bf16(8 * 1024).rearrange("p (k c) -> p k c", k=8)
    wabd = A.f32(128)
    wxbd = A.f32(128)
    sc1 = A.f32(16)
    negsp8, negsp16, nA, dtb = sc1[:, 0:1], sc1[:, 1:2], sc1[:, 2:3], sc1[:, 3:4]
    tmpc = sc1[:, 4:6]
    xin = [A.f32(4 * 1024).rearrange("p (s d) -> p s d", s=4) for _ in range(2)]
    junkb = A.bf16(1024)
    ssq = A.f32(4); msq = A.f32(4); rstd = A.f32(4)
    xn = A.bf16(4 * 1024).rearrange("p (s d) -> p s d", s=4)
    hT = A.bf16(8 * 512).rearrange("p (k t) -> p k t", k=8)
    cb = [A.f32(515) for _ in range(4)]
    cy = [A.f32(512) for _ in range(4)]
    qs = A.f32(512); ks = A.f32(512); vs = A.f32(512)
    gg = A.f32(512); zs = A.f32(512); grow = A.f32(512); brow = A.f32(512)
    t512 = [A.f32(512) for _ in range(8)]
    qn = A.f32(512); kn = A.f32(512)
    hprev = A.f32(1)
    ystage = [A.bf16(2 * 512).rearrange("p (g t) -> p g t", g=2) for _ in range(2)]
    S = [A.f32(128) for _ in range(2)]
    NCH = 4
    HB = {nm: [buf, A.f32(512)] for nm, buf in (("qn", qn), ("kn", kn), ("vs", vs), ("grow", grow), ("brow", brow), ("zs", zs))}
    def cbufs():
        d = {}
        for nm in ["gcum", "arg", "DT", "eg", "ekd", "t1", "t2", "t3", "kbgT", "vbT", "qdT", "kdT",
                   "B", "Am", "M", "B2", "A2", "kbg", "vb", "kd", "wT", "u", "attnT", "vnew", "on"]:
            d[nm] = A.f32(128)
        d["small"] = A.f32(8)
        return d
    CB = [cbufs() for _ in range(NCH)]
    pTs = [psb[6][:, :].bitcast(BF16), psb[7][:, :].bitcast(BF16)]

    P.DMA("pool", lambda e: e.dma_start(out=w1b, in_=w1.rearrange("(k p) c -> p k c", p=128)), w=["w1b"], ch="w1")
    P.I("pool", lambda e: e.memset(wabd, 0.0), w=["wabd"])
    P.I("pool", lambda e: e.memset(wxbd, 0.0), w=["wxbd"])
    for i in range(2):
        P.DMA("sp", lambda e, i=i: e.dma_start(out=wabd[i * 64:(i + 1) * 64, i * 64:(i + 1) * 64], in_=lw[0, i]), w=["wabd"], ch="c_wabd")
        P.DMA("sp", lambda e, i=i: e.dma_start(out=wxbd[i * 64:(i + 1) * 64, i * 64:(i + 1) * 64], in_=lw[1, i]), w=["wxbd"], ch="c_wxbd")
    P.I("act", lambda e: e.activation(out=tmpc[:, 0:1], in_=col(PV_LAM), func=AF.Exp, scale=-1.0), r=["pvt"], w=["tmpc"])
    P.I("act", lambda e: e.activation(out=tmpc[:, 1:2], in_=tmpc[:, 0:1], func=AF.Ln, bias=1.0), r=["tmpc"], w=["tmpc2"])
    P.I("dve", lambda e: e.tensor_scalar(out=negsp8, in0=tmpc[:, 1:2], scalar1=-8.0, scalar2=None, op0=OP.mult), r=["tmpc2"], w=["negsp8"])
    P.I("dve", lambda e: e.tensor_scalar(out=negsp16, in0=tmpc[:, 1:2], scalar1=-16.0, scalar2=None, op0=OP.mult), r=["tmpc2"], w=["negsp16"])
    P.I("act", lambda e: e.activation(out=nA, in_=col(PV_ALOG), func=AF.Exp), r=["pvt"], w=["nA0"])
    P.I("dve", lambda e: e.tensor_scalar(out=nA, in0=nA, scalar1=-1.0, scalar2=None, op0=OP.mult), r=["nA0"], w=["nA"])
    for c in range(4):
        P.I("pool", lambda e, c=c: e.memset(cb[c][:, 0:3], 0.0), w=[("cbh", c)])
    P.I("pool", lambda e: e.memset(S[0], 0.0), w=[("S", 0)])
    P.I("pool", lambda e: e.memset(hprev, 0.0), w=["hprev"])

    NBLK = G['nblk']
    stop = G['stop']
    ps_rot = [0]

    def load_x(blk, DMA):
        s = blk % 2
        DMA("sp", lambda e: e.dma_start(out=xin[s], in_=x_b[blk * 512:(blk + 1) * 512, :].rearrange("(s p) d -> p s d", p=128)),
              w=[("xin", s)], ch=("xin", s))

    import collections
    pending = collections.deque()

    def Idef(*a, **k):
        pending.append(("I", a, k))

    def DMAdef(*a, **k):
        pending.append(("DMA", a, k))

    cnt = [0]

    def CI(*a, **k):
        P.I(*a, **k)
        cnt[0] += 1
        if cnt[0] % 3 != 0 and pending:
            kind, a2, k2 = pending.popleft()
            getattr(P, kind)(*a2, **k2)

    def front(blk, I, DMA):
        sl = blk % 2
        par = blk % 2
        qn, kn, vs, grow, brow, zs = (HB[nm][par] for nm in ("qn", "kn", "vs", "grow", "brow", "zs"))
        if G["ub"] is not None:
            for ck in range(blk * 16 // NBLK, (blk + 1) * 16 // NBLK):
                for (src, dst, nm) in ((G["peer_u"], G["ub"], "ub"), (G["peer_v"], G["vb"], "vb")):
                    DMA("pool", lambda e, ck=ck, src=src, dst=dst: e.dma_start(out=dst.ap()[ck * 1024:(ck + 1) * 1024, :], in_=src[ck * 1024:(ck + 1) * 1024, :]),
                          w=[nm], ch="cvt")
        if blk + 1 < NBLK:
            load_x(blk + 1, DMA)
        X = xin[sl]
        for s in range(4):
            I("act", lambda e, s=s, X=X: e.activation(out=junkb, in_=X[:, s, :], func=AF.Square, accum_out=ssq[:, s:s + 1]),
                r=[("xin", sl)], w=["junkb", ("ssq", s)])
        I("dve", lambda e: e.tensor_scalar(out=msq, in0=ssq, scalar1=1.0 / D, scalar2=EPS, op0=OP.mult, op1=OP.add),
            r=[("ssq", s) for s in range(4)], w=["msq"])
        I("act", lambda e: e.activation(out=msq, in_=msq, func=AF.Sqrt), r=["msq"], w=["msq"])
        I("dve", lambda e: e.reciprocal(out=rstd, in_=msq), r=["msq"], w=["rstd"])
        for s in range(4):
            eng = "dve"
            I(eng, lambda e, s=s, X=X: e.tensor_scalar(out=xn[:, s, :], in0=X[:, s, :], scalar1=rstd[:, s:s + 1], scalar2=None, op0=OP.mult),
                r=[("xin", sl), "rstd"], w=[("xn", s)])
        for k in range(8):
            half = k % 2
            for s in range(4):
                I("pe", lambda e, k=k, s=s, half=half: e.transpose(out=pTs[half][:, s * 128:(s + 1) * 128],
                                                                     in_=xn[:, s, k * 128:(k + 1) * 128], identity=identb),
                    r=[("xn", s), "identb"], w=[("bank", 6 + half)])
            if False:
                I("act", lambda e, k=k, half=half: e.activation(out=hT[:, k, :], in_=pTs[half][:, 0:512], func=AF.Copy,
                                                                  scale=col(PV_NMIX + k)),
                    r=[("bank", 6 + half), "pvt"], w=[("hT", k)])
            else:
                I("dve", lambda e, k=k, half=half: e.tensor_scalar(out=hT[:, k, :], in0=pTs[half][:, 0:512],
                                                                     scalar1=col(PV_NMIX + k), scalar2=None, op0=OP.mult),
                    r=[("bank", 6 + half), "pvt"], w=[("hT", k)])
        for c in range(8):
            pb = ps_rot[0] % 2
            ps_rot[0] += 1
            pst = psb[pb]
            for k in range(8):
                I("pe", lambda e, c=c, k=k, pst=pst: e.matmul(pst[:, :], lhsT=w1b[:, k, c * 128:(c + 1) * 128], rhs=hT[:, k, :],
                                                                start=(k == 0), stop=(k == 7)),
                    r=["w1b", ("hT", k)], w=[("bank", pb)])
            if c < 4:
                j = c
                if blk > 0:
                    I("dve", lambda e, j=j: e.tensor_copy(out=cb[j][:, 0:3], in_=cb[j][:, 512:515]), r=[("cb", j)], w=[("cbh", j)])
                I("act", lambda e, j=j, pst=pst: e.activation(out=cb[j][:, 3:515], in_=pst[:, :], func=AF.Copy),
                    r=[("bank", pb), ("cbh", j)], w=[("cb", j)])
            elif c == 4:
                I("act", lambda e, pst=pst: e.activation(out=gg, in_=pst[:, :], func=AF.Gelu_apprx_tanh), r=[("bank", pb)], w=["gg"])
            elif c == 5:
                I("act", lambda e, pst=pst: e.activation(out=zs, in_=pst[:, :], func=AF.Silu), r=[("bank", pb)], w=[("zs", par)])
            elif c == 6:
                I("act", lambda e, pst=pst: e.activation(out=grow, in_=pst[:, :], func=AF.Exp, bias=col(PV_DTB)), r=[("bank", pb), "pvt"], w=[("grow", par)])
                I("act", lambda e: e.activation(out=grow, in_=grow, func=AF.Ln, bias=1.0), r=[("grow", par)], w=[("grow", par)])
                I("dve", lambda e: e.tensor_scalar(out=grow, in0=grow, scalar1=nA, scalar2=None, op0=OP.mult), r=[("grow", par), "nA"], w=[("grow", par)])
            else:
                I("act", lambda e, pst=pst: e.activation(out=brow, in_=pst[:, :], func=AF.Sigmoid), r=[("bank", pb)], w=[("brow", par)])
        for j in range(4):
            eng = "dve" if j % 2 == 0 else "pool"
            if j == 3:
                I("dve", lambda e, j=j: e.tensor_scalar(out=cy[j], in0=cb[j][:, 0:512], scalar1=cwt[:, 4 * j:4 * j + 1], scalar2=col(PV_CB),
                                                          op0=OP.mult, op1=OP.add), r=[("cb", j), ("cbh", j), "cwt", "pvt"], w=[("cy", j)])
            else:
                I("dve", lambda e, j=j: e.tensor_scalar(out=cy[j], in0=cb[j][:, 0:512], scalar1=cwt[:, 4 * j:4 * j + 1], scalar2=None,
                                                          op0=OP.mult), r=[("cb", j), ("cbh", j), "cwt"], w=[("cy", j)])
            for tp in range(1, 4):
                I("dve", lambda e, j=j, tp=tp: e.scalar_tensor_tensor(out=cy[j], in0=cb[j][:, tp:tp + 512], scalar=cwt[:, 4 * j + tp:4 * j + tp + 1],
                                                                         in1=cy[j], op0=OP.mult, op1=OP.add),
                    r=[("cb", j), ("cbh", j), "cwt", ("cy", j)], w=[("cy", j)])
        I("act", lambda e: e.activation(out=qs, in_=cy[0], func=AF.Silu), r=[("cy", 0)], w=["qs"])
        I("act", lambda e: e.activation(out=ks, in_=cy[1], func=AF.Silu), r=[("cy", 1)], w=["ks"])
        I("act", lambda e: e.activation(out=vs, in_=cy[2], func=AF.Silu), r=[("cy", 2)], w=[("vs", par)])
        xrc = cy[3]
        r_, i_, a_, a2_, ix_, hs_ = t512[0], t512[1], t512[2], t512[3], t512[4], t512[5]
        I("pe", lambda e: e.matmul(psb[2][:, :], lhsT=wabd, rhs=xrc, start=True, stop=True), r=["wabd", ("cy", 3)], w=[("bank", 2)])
        I("act", lambda e: e.activation(out=r_, in_=psb[2][:, :], func=AF.Sigmoid, bias=col(PV_BA)), r=[("bank", 2), "pvt"], w=["r_"])
        I("pe", lambda e: e.matmul(psb[2][:, :], lhsT=wxbd, rhs=xrc, start=True, stop=True), r=["wxbd", ("cy", 3)], w=[("bank", 2)])
        I("act", lambda e: e.activation(out=i_, in_=psb[2][:, :], func=AF.Sigmoid, bias=col(PV_BX)), r=[("bank", 2), "pvt"], w=["i_"])
        I("act", lambda e: e.activation(out=a_, in_=r_, func=AF.Exp, scale=negsp8), r=["r_", "negsp8"], w=["a_"])
        I("act", lambda e: e.activation(out=a2_, in_=r_, func=AF.Exp, scale=negsp16), r=["r_", "negsp16"], w=["a2_"])
        I("dve", lambda e: e.tensor_scalar(out=a2_, in0=a2_, scalar1=-1.0, scalar2=1.0, op0=OP.mult, op1=OP.add), r=["a2_"], w=["a2_"])
        I("act", lambda e: e.activation(out=a2_, in_=a2_, func=AF.Sqrt), r=["a2_"], w=["a2_"])
        I("pool", lambda e: e.tensor_tensor(out=ix_, in0=i_, in1=xrc, op=OP.mult), r=["i_", ("cy", 3)], w=["ix_"])
        I("pool", lambda e: e.tensor_tensor(out=ix_, in0=ix_, in1=a2_, op=OP.mult), r=["ix_", "a2_"], w=["ix_"])
        I("dve", lambda e: e.tensor_tensor_scan(out=hs_, data0=a_, data1=ix_, initial=hprev[:, 0:1], op0=OP.mult, op1=OP.add),
            r=["a_", "ix_", "hprev"], w=["hs_"])
        I("dve", lambda e: e.tensor_copy(out=hprev, in_=hs_[:, 511:512]), r=["hs_"], w=["hprev"])
        I("pool", lambda e, sl=sl: e.tensor_tensor(out=ystage[sl][:, 1, :], in0=hs_, in1=gg, op=OP.mult), r=["hs_", "gg"], w=[("ystage", sl)])
        sq_, rq_ = t512[6], t512[7]
        for (src, dst, scl, nm) in ((qs, qn, 128.0 ** -0.5, ("qn", par)), (ks, kn, 1.0, ("kn", par))):
            I("pool", lambda e, src=src: e.tensor_tensor(out=sq_, in0=src, in1=src, op=OP.mult), r=["qs", "ks"], w=["sq_"])
            I("pe", lambda e: e.matmul(psb[2][:, :], lhsT=ones[:, 0:128], rhs=sq_, start=True, stop=True), r=["ones", "sq_"], w=[("bank", 2)])
            I("act", lambda e: e.activation(out=rq_, in_=psb[2][:, :], func=AF.Sqrt, bias=EPS), r=[("bank", 2)], w=["rq_"])
            I("dve", lambda e: e.reciprocal(out=rq_, in_=rq_), r=["rq_"], w=["rq_"])
            I("dve", lambda e, src=src, dst=dst, scl=scl: e.scalar_tensor_tensor(out=dst, in0=src, scalar=scl, in1=rq_, op0=OP.mult, op1=OP.mult),
                r=["qs", "ks", "rq_"], w=[nm])

    def chunks(blk):
        sl = blk % 2
        par = blk % 2
        qn, kn, vs, grow, brow, zs = (HB[nm][par] for nm in ("qn", "kn", "vs", "grow", "brow", "zs"))
        slots = [(3, 0), (4, 0), (3, 1), (4, 1), (3, 2), (4, 2), (3, 3), (4, 3)]
        sr = [0]

        def pslot():
            b, q = slots[sr[0] % len(slots)]
            sr[0] += 1
            return psb[b][:, q * 128:(q + 1) * 128], ("bank", b)

        def K(ch, nm):
            return (nm, ch)

        for ch in range(NCH):
            c = CB[ch]
            cs = slice(ch * 128, (ch + 1) * 128)
            sm = c["small"]
            gcol, ngcol, gl, dcol = sm[:, 0:1], sm[:, 1:2], sm[:, 2:3], sm[:, 3:4]
            CI("dve", lambda e, c=c, cs=cs: e.tensor_tensor_scan(out=c["gcum"], data0=ones[:, 0:128], data1=grow[:, cs], initial=0.0,
                                                                   op0=OP.mult, op1=OP.add), r=[("grow", par), "ones"], w=[K(ch, "gcum")])
            pt, pk = pslot()
            CI("pe", lambda e, c=c, pt=pt: e.matmul(pt, lhsT=c["gcum"], rhs=ident, start=True, stop=True), r=[K(ch, "gcum"), "ident"], w=[pk])
            CI("dve", lambda e, pt=pt, ngcol=ngcol: e.tensor_scalar(out=ngcol, in0=pt[:, 0:1], scalar1=-1.0, scalar2=None, op0=OP.mult),
                r=[pk], w=[K(ch, "ngcol")])
            CI("dve", lambda e, c=c, ngcol=ngcol: e.tensor_scalar(out=c["arg"], in0=c["gcum"], scalar1=ngcol, scalar2=0.0, op0=OP.add, op1=OP.min),
                r=[K(ch, "gcum"), K(ch, "ngcol")], w=[K(ch, "arg")])
            CI("act", lambda e, c=c: e.activation(out=c["DT"], in_=c["arg"], func=AF.Exp), r=[K(ch, "arg")], w=[K(ch, "DT")])
            CI("act", lambda e, c=c: e.activation(out=c["eg"], in_=c["gcum"], func=AF.Exp), r=[K(ch, "gcum")], w=[K(ch, "eg")])
            CI("act", lambda e, c=c: e.activation(out=c["ekd"], in_=c["gcum"], func=AF.Exp, scale=-1.0, bias=c["gcum"][:, 127:128]),
                r=[K(ch, "gcum")], w=[K(ch, "ekd")])
            CI("act", lambda e, c=c, dcol=dcol: e.activation(out=dcol, in_=c["gcum"][:, 127:128], func=AF.Exp), r=[K(ch, "gcum")], w=[K(ch, "dcol")])
            CI("pool", lambda e, c=c: e.tensor_tensor(out=c["t1"], in0=c["DT"], in1=mUs, op=OP.mult), r=[K(ch, "DT"), "mUs"], w=[K(ch, "t1")])
            CI("pool", lambda e, c=c, cs=cs: e.tensor_tensor(out=c["t2"], in0=c["t1"], in1=brow[:, cs], op=OP.mult), r=[K(ch, "t1"), ("brow", par)], w=[K(ch, "t2")])
            CI("pool", lambda e, c=c: e.tensor_tensor(out=c["t3"], in0=c["DT"], in1=mU, op=OP.mult), r=[K(ch, "DT"), "mU"], w=[K(ch, "t3")])
            CI("dve", lambda e, c=c, cs=cs: e.tensor_tensor(out=c["vbT"], in0=vs[:, cs], in1=brow[:, cs], op=OP.mult), r=[("vs", par), ("brow", par)], w=[K(ch, "vbT")])
            CI("dve", lambda e, c=c, cs=cs: e.tensor_tensor(out=c["kbgT"], in0=kn[:, cs], in1=brow[:, cs], op=OP.mult), r=[("kn", par), ("brow", par)], w=[K(ch, "kbgT")])
            CI("dve", lambda e, c=c: e.tensor_tensor(out=c["kbgT"], in0=c["kbgT"], in1=c["eg"], op=OP.mult), r=[K(ch, "kbgT"), K(ch, "eg")], w=[K(ch, "kbgT")])
            CI("pool", lambda e, c=c, cs=cs: e.tensor_tensor(out=c["qdT"], in0=qn[:, cs], in1=c["eg"], op=OP.mult), r=[("qn", par), K(ch, "eg")], w=[K(ch, "qdT")])
            CI("pool", lambda e, c=c, cs=cs: e.tensor_tensor(out=c["kdT"], in0=kn[:, cs], in1=c["ekd"], op=OP.mult), r=[("kn", par), K(ch, "ekd")], w=[K(ch, "kdT")])
            pt, pk = pslot()
            CI("pe", lambda e, cs=cs, pt=pt: e.matmul(pt, lhsT=kn[:, cs], rhs=kn[:, cs], start=True, stop=True), r=[("kn", par)], w=[pk])
            CI("dve", lambda e, c=c, pt=pt: e.tensor_tensor(out=c["B"], in0=pt, in1=c["t2"], op=OP.mult), r=[pk, K(ch, "t2")], w=[K(ch, "B")])
            pt, pk = pslot()
            CI("pe", lambda e, cs=cs, pt=pt: e.matmul(pt, lhsT=kn[:, cs], rhs=qn[:, cs], start=True, stop=True), r=[("kn", par), ("qn", par)], w=[pk])
            CI("dve", lambda e, c=c, pt=pt: e.tensor_tensor(out=c["attnT"], in0=pt, in1=c["t3"], op=OP.mult), r=[pk, K(ch, "t3")], w=[K(ch, "attnT")])
            pt, pk = pslot()
            CI("pe", lambda e, c=c, pt=pt: e.matmul(pt, lhsT=c["B"], rhs=ident, start=True, stop=True), r=[K(ch, "B"), "ident"], w=[pk])
            CI("act", lambda e, c=c, pt=pt: e.activation(out=c["Am"], in_=pt, func=AF.Copy), r=[pk], w=[K(ch, "Am")])
            CI("dve", lambda e, c=c: e.tensor_tensor(out=c["M"], in0=ident, in1=c["B"], op=OP.subtract), r=["ident", K(ch, "B")], w=[K(ch, "M")])
            for (srcn, dstn) in (("kbgT", "kbg"), ("vbT", "vb"), ("kdT", "kd")):
                pt, pk = pslot()
                CI("pe", lambda e, c=c, pt=pt, srcn=srcn: e.matmul(pt, lhsT=c[srcn], rhs=ident, start=True, stop=True), r=[K(ch, srcn), "ident"], w=[pk])
                CI("act", lambda e, c=c, pt=pt, dstn=dstn: e.activation(out=c[dstn], in_=pt, func=AF.Copy), r=[pk], w=[K(ch, dstn)])
        cur = [("Am", "B")] * NCH
        for lev in range(6):
            for ch in range(NCH):
                c = CB[ch]
                an, bn = cur[ch]
                na, nb = ("A2", "B2") if an == "Am" else ("Am", "B")
                pt, pk = pslot()
                CI("pe", lambda e, c=c, pt=pt, an=an, bn=bn: e.matmul(pt, lhsT=c[bn], rhs=c[an], start=True, stop=True),
                    r=[K(ch, an), K(ch, bn)], w=[pk])
                pt2, pk2 = (None, None)
                if lev < 5:
                    pt2, pk2 = pslot()
                    CI("pe", lambda e, c=c, pt2=pt2, an=an, bn=bn: e.matmul(pt2, lhsT=c[an], rhs=c[bn], start=True, stop=True),
                        r=[K(ch, an), K(ch, bn)], w=[pk2])
                CI("act", lambda e, c=c, pt=pt, na=na: e.activation(out=c[na], in_=pt, func=AF.Copy), r=[pk], w=[K(ch, na)])
                if lev < 5:
                    CI("dve", lambda e, c=c, pt2=pt2, nb=nb: e.tensor_copy(out=c[nb], in_=pt2), r=[pk2], w=[K(ch, nb)])
                pt3, pk3 = pslot()
                CI("pe", lambda e, c=c, pt3=pt3, na=na: e.matmul(pt3, lhsT=c[na], rhs=c["M"], start=True, stop=True),
                    r=[K(ch, na), K(ch, "M")], w=[pk3])
                CI("dve", lambda e, c=c, pt3=pt3: e.tensor_tensor(out=c["M"], in0=pt3, in1=c["M"], op=OP.add), r=[pk3, K(ch, "M")], w=[K(ch, "M")])
                cur[ch] = (na, nb)
        for ch in range(NCH):
            c = CB[ch]
            pt, pk = pslot()
            CI("pe", lambda e, c=c, pt=pt: e.matmul(pt, lhsT=c["kbg"], rhs=c["M"], start=True, stop=True), r=[K(ch, "kbg"), K(ch, "M")], w=[pk])
            CI("act", lambda e, c=c, pt=pt: e.activation(out=c["wT"], in_=pt, func=AF.Copy), r=[pk], w=[K(ch, "wT")])
            pt, pk = pslot()
            CI("pe", lambda e, c=c, pt=pt: e.matmul(pt, lhsT=c["M"], rhs=c["vb"], start=True, stop=True), r=[K(ch, "vb"), K(ch, "M")], w=[pk])
            CI("act", lambda e, c=c, pt=pt: e.activation(out=c["u"], in_=pt, func=AF.Copy), r=[pk], w=[K(ch, "u")])
        for ch in range(NCH):
            c = CB[ch]
            cs = slice(ch * 128, (ch + 1) * 128)
            n = blk * NCH + ch
            Sc, Sn = S[n % 2], S[(n + 1) % 2]
            kSc, kSn = ("S", n % 2), ("S", (n + 1) % 2)
            sm = c["small"]
            dcol = sm[:, 3:4]; osq = sm[:, 4:5]; orstd = sm[:, 5:6]
            p_ws, p_o, p_ks, p_t = (psb[5][:, q * 128:(q + 1) * 128] for q in range(4))
            CI("pe", lambda e, c=c, Sc=Sc, p_ws=p_ws: e.matmul(p_ws, lhsT=c["wT"], rhs=Sc, start=True, stop=True), r=[K(ch, "wT"), kSc], w=[("bank", 5)])
            CI("dve", lambda e, c=c, p_ws=p_ws: e.tensor_tensor(out=c["vnew"], in0=c["u"], in1=p_ws, op=OP.subtract), r=[K(ch, "u"), ("bank", 5)], w=[K(ch, "vnew")])
            CI("pe", lambda e, c=c, Sc=Sc, p_o=p_o: e.matmul(p_o, lhsT=c["qdT"], rhs=Sc, start=True, stop=False), r=[K(ch, "qdT"), kSc, K(ch, "vnew"), K(ch, "attnT")], w=[("bank", 5)])
            CI("pe", lambda e, c=c, p_o=p_o: e.matmul(p_o, lhsT=c["attnT"], rhs=c["vnew"], start=False, stop=True), r=[K(ch, "attnT"), K(ch, "vnew")], w=[("bank", 5)])
            CI("pe", lambda e, c=c, p_ks=p_ks: e.matmul(p_ks, lhsT=c["kd"], rhs=c["vnew"], start=True, stop=True), r=[K(ch, "kd"), K(ch, "vnew")], w=[("bank", 5)])
            CI("dve", lambda e, Sc=Sc, Sn=Sn, dcol=dcol, p_ks=p_ks: e.scalar_tensor_tensor(out=Sn, in0=Sc, scalar=dcol, in1=p_ks, op0=OP.mult, op1=OP.add),
                r=[kSc, K(ch, "dcol"), ("bank", 5)], w=[kSn])
            CI("act", lambda e, c=c, p_o=p_o, osq=osq: e.activation(out=c["on"], in_=p_o, func=AF.Copy), r=[("bank", 5)], w=[K(ch, "on")])
            CI("act", lambda e, c=c, osq=osq: e.activation(out=c["t1"], in_=c["on"], func=AF.Square, accum_out=osq), r=[K(ch, "on")], w=[K(ch, "t1"), K(ch, "osq")])
            CI("dve", lambda e, osq=osq, orstd=orstd: e.tensor_scalar(out=orstd, in0=osq, scalar1=1.0 / 128, scalar2=EPS, op0=OP.mult, op1=OP.add), r=[K(ch, "osq")], w=[K(ch, "orstd")])
            CI("act", lambda e, orstd=orstd: e.activation(out=orstd, in_=orstd, func=AF.Sqrt), r=[K(ch, "orstd")], w=[K(ch, "orstd")])
            CI("dve", lambda e, orstd=orstd: e.reciprocal(out=orstd, in_=orstd), r=[K(ch, "orstd")], w=[K(ch, "orstd")])
            CI("dve", lambda e, c=c, orstd=orstd: e.tensor_scalar(out=c["t2"], in0=c["on"], scalar1=orstd, scalar2=None, op0=OP.mult), r=[K(ch, "on"), K(ch, "orstd")], w=[K(ch, "t2")])
            pt, pk = pslot()
            CI("pe", lambda e, c=c, pt=pt: e.matmul(pt, lhsT=c["t2"], rhs=ident, start=True, stop=True), r=[K(ch, "t2"), "ident"], w=[pk])
            CI("dve", lambda e, pt=pt, cs=cs, sl=sl: e.scalar_tensor_tensor(out=ystage[sl][:, 0, cs], in0=pt, scalar=col(PV_GNW), in1=zs[:, cs], op0=OP.mult, op1=OP.mult),
                r=[pk, "pvt", ("zs", par)], w=[("ystage", sl)])
        for g in range(2):
            P.DMA("sp", lambda e, blk=blk, sl=sl, g=g: e.dma_start(
                out=cin[blk // 4].rearrange("(i g p) t -> p i g t", i=16, g=2)[:, (blk % 4) * 4:(blk % 4) * 4 + 4, g, :],
                in_=ystage[sl][:, g, :].rearrange("p (i t) -> p i t", i=4)),
                  r=[("ystage", sl)], w=["cin"], ch=("yst", sl))


    load_x(0, P.DMA)
    front(0, P.I, P.DMA)
    for blk in range(NBLK):
        if blk + 1 < NBLK:
            front(blk + 1, Idef, DMAdef)
        chunks(blk)
        while pending:
            kind, a2, k2 = pending.popleft()
            getattr(P, kind)(*a2, **k2)


def phase2(nc, P, A, G):
    psb = G["psb"]; ident = G["ident"]; identb = G["identb"]; ones = G["ones"]; onesb = G["onesb"]
    pvt = G["pvt"]; col = G["col"]; debug = G["debug"]
    PV_NXA, PV_NMEM = 16, 24
    x_tok = G["x_tok"]; mem_b = G["mem_b"]; coutflat = G["coutflat"]; idxy = G["idxy"]
    NT = G.get("ntile", 16)

    def wload(dram, ncols, key):
        t = A.bf16(8 * ncols).rearrange("p (k c) -> p k c", k=8)
        for k in range(8):
            P.DMA("pool", lambda e, k=k, t=t: e.dma_start(out=t[:, k, :], in_=dram[k * 128:(k + 1) * 128, :]), w=[key], ch=key)
        return t
    woutb = wload(G["w_out"], 1024, "woutb")
    wqb = wload(G["xwq"], 1024, "wqb")
    wob = wload(G["xwo"], 1024, "wob")
    big = A.bf16(8 * 2048).rearrange("p (k c) -> p k c", k=8)
    for k in range(8):
        P.DMA("pool", lambda e, k=k: e.dma_start(out=big[:, k, :], in_=G["xwkv"][k * 128:(k + 1) * 128, :]), w=["big"], ch="big")
    skt = A.f32(2048).rearrange("p (c k) -> p c k", c=16)
    P.DMA("sp", lambda e: e.dma_start(out=skt, in_=G["skT"].rearrange("p (c k) -> p c k", c=16)), w=["skt"], ch="skt")
    iy = A.u32(8)
    P.DMA("sp", lambda e: e.dma_start(out=iy, in_=idxy[:, :]), w=["iy"], ch="iy")
    h3acc = A.f32(2048)
    rowt = h3acc
    P.DMA("sp", lambda e: e.dma_start(out=rowt[0:1, :], in_=G["rows"].rearrange("a d -> (a d)").unsqueeze(0)), w=["rowt"], ch="rowt")
    wbc = A.f32(2048)
    for i in range(4):
        P.I("pe", lambda e, i=i: e.matmul(psb[0][:, :], lhsT=ones[0:1, 0:128], rhs=rowt[0:1, i * 512:(i + 1) * 512], start=True, stop=True),
            r=["ones", "rowt"], w=[("bank", 0)])
        P.I("act", lambda e, i=i: e.activation(out=wbc[:, i * 512:(i + 1) * 512], in_=psb[0][:, :], func=AF.Copy), r=[("bank", 0)], w=["wbc"])
    iota16 = A.f32(256)
    P.I("pool", lambda e: e.iota(iota16, pattern=[[1, 256]], base=0, channel_multiplier=0, allow_small_or_imprecise_dtypes=True), w=["iota"])

    kT = A.bf16(8 * 256).rearrange("p (c m) -> p c m", c=8)
    vv = A.bf16(2 * 1024).rearrange("p (m c) -> p m c", m=2)
    xt = A.f32(1024); junk = A.f32(1024); h3 = h3acc[:, 0:1024]; acc = h3acc[:, 1024:2048]
    xnb = A.bf16(1024)
    hT = A.bf16(8 * 128).rearrange("p (k t) -> p k t", k=8)
    yT = A.bf16(8 * 128).rearrange("p (k t) -> p k t", k=8)
    qT = A.bf16(8 * 128).rearrange("p (k t) -> p k t", k=8)
    oT = A.bf16(8 * 128).rearrange("p (k t) -> p k t", k=8)
    expT = A.bf16(2 * 128).rearrange("p (m t) -> p m t", m=2)
    rden = A.f32(128)
    sm = A.f32(8)
    qsall = A.f32(4096)
    qpT = qsall[:, 0:2048].rearrange("p (c t) -> p c t", c=16)
    scs = qsall[:, 2048:4096].rearrange("p (c k) -> p c k", c=16)
    sct = A.f32(128)
    tv = A.f32(256).rearrange("p (c k) -> p c k", c=16)
    tiu = A.u32(256).rearrange("p (c k) -> p c k", c=16)
    tif = A.f32(256).rearrange("p (c k) -> p c k", c=16)
    cand = A.f32(256); cand2 = A.f32(256); cidx = A.f32(256)
    best = A.f32(16); posu = A.u32(16); posf = A.f32(16)
    oh = qsall.rearrange("p (k a) -> p k a", k=16)
    idxf = A.f32(128); idxu = A.u32(128); gate = A.f32(128); sval = A.f32(128); act = A.f32(128)
    NB = 8
    gb = [A.bf16(1024) for _ in range(NB)]
    junkb2 = A.bf16(1024)
    ps_all = G["ps_all"]
    h3p = ps_all[:, 0:1024]
    accp = [ps_all[:, 1024:2048], ps_all[:, 2048:3072]]
    accb = [[("bank", 2), ("bank", 3)], [("bank", 4), ("bank", 5)]]
    print("phase2 arena used", A.off, "of", A.n)
    pTb = psb[2][:, :].bitcast(BF16)

    def rms_to_hT(src, wcol0, key_src):
        P.I("act", lambda e: e.activation(out=junk, in_=src, func=AF.Square, accum_out=sm[:, 0:1]), r=[key_src], w=["junk", "sm0"])
        P.I("dve", lambda e: e.tensor_scalar(out=sm[:, 1:2], in0=sm[:, 0:1], scalar1=1.0 / D, scalar2=EPS, op0=OP.mult, op1=OP.add), r=["sm0"], w=["sm1"])
        P.I("act", lambda e: e.activation(out=sm[:, 1:2], in_=sm[:, 1:2], func=AF.Sqrt), r=["sm1"], w=["sm1"])
        P.I("dve", lambda e: e.reciprocal(out=sm[:, 2:3], in_=sm[:, 1:2]), r=["sm1"], w=["rstd"])
        if wcol0 is None:
            return
        P.I("dve", lambda e: e.tensor_scalar(out=xnb, in0=src, scalar1=sm[:, 2:3], scalar2=None, op0=OP.mult), r=[key_src, "rstd"], w=["xnb"])
        for k in range(8):
            P.I("pe", lambda e, k=k: e.transpose(out=pTb[:, (k % 4) * 128:(k % 4 + 1) * 128], in_=xnb[:, k * 128:(k + 1) * 128], identity=identb),
                r=["xnb", "identb"], w=[("bank", 2)])
            P.I("dve", lambda e, k=k: e.tensor_scalar(out=hT[:, k, :], in0=pTb[:, (k % 4) * 128:(k % 4 + 1) * 128], scalar1=col(wcol0 + k), scalar2=None, op0=OP.mult),
                r=[("bank", 2), "pvt"], w=["hT"])

    for mt in range(2):
        P.DMA("sp", lambda e, mt=mt: e.dma_start(out=xt, in_=mem_b[mt * 128:(mt + 1) * 128, :]), w=[("xt", 0)], ch="xtmem")
        rms_to_hT(xt, PV_NMEM, ("xt", 0))
        for half in range(2):
            for k in range(8):
                P.I("pe", lambda e, k=k, half=half: e.matmul(psb[half][:, :], lhsT=hT[:, k, :], rhs=big[:, k, 1024 + half * 512:1024 + (half + 1) * 512],
                                                             start=(k == 0), stop=(k == 7)), r=["hT", "big"], w=[("bank", half)])
            P.I("act", lambda e, half=half, mt=mt: e.activation(out=vv[:, mt, half * 512:(half + 1) * 512], in_=psb[half][:, :], func=AF.Copy), r=[("bank", half)], w=["vv"])
        for c in range(8):
            pb = 3 + c % 2
            for k in range(8):
                P.I("pe", lambda e, k=k, c=c, pb=pb: e.matmul(psb[pb][:, 0:128], lhsT=big[:, k, c * 128:(c + 1) * 128], rhs=hT[:, k, :], start=(k == 0), stop=(k == 7)),
                    r=["hT", "big"], w=[("bank", pb)])
            P.I("act", lambda e, c=c, pb=pb, mt=mt: e.activation(out=kT[:, c, mt * 128:(mt + 1) * 128], in_=psb[pb][:, 0:128], func=AF.Copy), r=[("bank", pb)], w=["kT"])
    for k in range(8):
        P.DMA("pool", lambda e, k=k: e.dma_start(out=big[:, k, :], in_=G["pwq"][k * 128:(k + 1) * 128, :]), r=[], w=["big"], ch="big2")

    cflat = coutflat
    import collections
    xts = [xt, A.f32(1024)]
    gates = [gate, A.f32(128)]
    idxus = [idxu, A.u32(128)]
    smA = sm
    smG = A.f32(8)
    idxTs = [A.u32(128), A.u32(128)]
    actT = A.bf16(128)
    accTs = A.f32(1024)
    accT = ps_all[:, 1024:2048]
    accp1 = accp[0]
    accbk = accb[0]
    pT6 = psb[6][:, :].bitcast(BF16)
    print("phase2 arena used (pipelined)", A.off, "of", A.n)
    P.I("pe", lambda e: e.matmul(psb[4][:, 0:8], lhsT=ones[0:1, 0:128], rhs=ones[0:1, 0:8], start=True, stop=True),
        r=["woutb", "wqb", "wob", "big"], w=["wts", ("bank", 4)])

    def rms_stats(I, src, key_src, smx, tag, jout, jkeys):
        I("act", lambda e: e.activation(out=jout, in_=src, func=AF.Square, accum_out=smx[:, 0:1]), r=[key_src], w=list(jkeys) + [tag + "0"])
        I("dve", lambda e: e.tensor_scalar(out=smx[:, 1:2], in0=smx[:, 0:1], scalar1=1.0 / D, scalar2=EPS, op0=OP.mult, op1=OP.add), r=[tag + "0"], w=[tag + "1"])
        I("act", lambda e: e.activation(out=smx[:, 1:2], in_=smx[:, 1:2], func=AF.Sqrt), r=[tag + "1"], w=[tag + "1"])
        I("dve", lambda e: e.reciprocal(out=smx[:, 2:3], in_=smx[:, 1:2]), r=[tag + "1"], w=[tag + "rstd"])

    def to_hT(I, wcol0):
        for k in range(8):
            I("pe", lambda e, k=k: e.transpose(out=pT6[:, (k % 4) * 128:(k % 4 + 1) * 128], in_=xnb[:, k * 128:(k + 1) * 128], identity=identb),
              r=["xnb", "identb"], w=[("bank", 6)])
            if wcol0 is None:
                I("dve", lambda e, k=k: e.tensor_copy(out=hT[:, k, :], in_=pT6[:, (k % 4) * 128:(k % 4 + 1) * 128]), r=[("bank", 6)], w=["hT"])
            else:
                I("dve", lambda e, k=k: e.tensor_scalar(out=hT[:, k, :], in0=pT6[:, (k % 4) * 128:(k % 4 + 1) * 128], scalar1=col(wcol0 + k), scalar2=None, op0=OP.mult),
                  r=[("bank", 6), "pvt"], w=["hT"])

    def resid_add(I, lhs, wmat, key_l, chunk_of, xt_, kx):
        for half in range(2):
            bk = 4 + half
            for k in range(8):
                I("pe", lambda e, k=k, half=half, bk=bk: e.matmul(psb[bk][:, :], lhsT=lhs[:, k, :], rhs=wmat[:, chunk_of(k), half * 512:(half + 1) * 512],
                                                                 start=(k == 0), stop=(k == 7)), r=[key_l, "wts"], w=[("bank", bk)])
            I("dve", lambda e, half=half, bk=bk: e.tensor_tensor(out=xt_[:, half * 512:(half + 1) * 512], in0=psb[bk][:, :], in1=xt_[:, half * 512:(half + 1) * 512], op=OP.add),
              r=[("bank", bk), kx], w=[kx])

    def stageA(it, par, I, DMA):
        xt_ = xts[par]; kx = ("xt", par); gate_ = gates[par]; idxu_ = idxus[par]; kg = ("gate", par); ki = ("idxu", par)
        DMA("sp", lambda e: e.dma_start(out=xt_, in_=x_tok[it * 128:(it + 1) * 128, :]), w=[kx], ch=("xt", par))
        for ag in range(8):
            DMA("pool", lambda e, ag=ag: e.indirect_dma_start(out=yT[:, ag, :], out_offset=None, in_=cflat,
                                                             in_offset=bass.IndirectOffsetOnAxis(ap=iy[:, ag:ag + 1], axis=0),
                                                             element_offset=it * 256 * 128),
                r=["cout", "iy"], w=["yT"], ch="yT")
        resid_add(I, yT, woutb, "yT", lambda k: (k // 2) + 4 * (k % 2), xt_, kx)
        if debug:
            DMA("sp", lambda e: e.dma_start(out=G["dbg"][0, it * 128:(it + 1) * 128, :], in_=xt_), r=[kx], w=["dbg0"], ch="dbg0")
        rms_stats(I, xt_, kx, smA, "smA", junk, ["junk"])
        I("dve", lambda e: e.tensor_scalar(out=xnb, in0=xt_, scalar1=smA[:, 2:3], scalar2=None, op0=OP.mult), r=[kx, "smArstd"], w=["xnb"])
        to_hT(I, PV_NXA)
        for c in range(8):
            pb = 6 + c % 2
            for k in range(8):
                I("pe", lambda e, k=k, c=c, pb=pb: e.matmul(psb[pb][:, 0:128], lhsT=wqb[:, k, c * 128:(c + 1) * 128], rhs=hT[:, k, :], start=(k == 0), stop=(k == 7)),
                  r=["hT", "wts"], w=[("bank", pb)])
            I("act", lambda e, c=c, pb=pb: e.activation(out=qT[:, c, :], in_=psb[pb][:, 0:128], func=AF.Copy), r=[("bank", pb)], w=["qT"])
        for h in range(4):
            for mc in range(2):
                for dc in range(2):
                    I("pe", lambda e, h=h, mc=mc, dc=dc: e.matmul(psb[6][:, mc * 128:(mc + 1) * 128], lhsT=kT[:, 2 * h + dc, mc * 128:(mc + 1) * 128], rhs=qT[:, 2 * h + dc, :],
                                                                  start=(dc == 0), stop=(dc == 1)), r=["kT", "qT"], w=[("bank", 6)])
            I("act", lambda e: e.activation(out=expT.rearrange("p m t -> p (m t)"), in_=psb[6][:, 0:256], func=AF.Exp, scale=1.0 / 16.0), r=[("bank", 6)], w=["expT"])
            for mc in range(2):
                I("pe", lambda e, mc=mc: e.matmul(psb[7][:, 0:128], lhsT=onesb, rhs=expT[:, mc, :], start=(mc == 0), stop=(mc == 1)), r=["expT", "onesb"], w=[("bank", 7)])
            I("dve", lambda e: e.reciprocal(out=rden, in_=psb[7][:, 0:128]), r=[("bank", 7)], w=["rden"])
            for dc in range(2):
                for mc in range(2):
                    I("pe", lambda e, h=h, mc=mc, dc=dc: e.matmul(psb[4][:, 0:128], lhsT=vv[:, mc, (2 * h + dc) * 128:(2 * h + dc + 1) * 128], rhs=expT[:, mc, :],
                                                                  start=(mc == 0), stop=(mc == 1)), r=["expT", "vv"], w=[("bank", 4)])
                I("dve", lambda e, h=h, dc=dc: e.tensor_tensor(out=oT[:, 2 * h + dc, :], in0=psb[4][:, 0:128], in1=rden, op=OP.mult), r=[("bank", 4), "rden"], w=["oT"])
        resid_add(I, oT, wob, "oT", lambda k: k, xt_, kx)
        if debug:
            DMA("sp", lambda e: e.dma_start(out=G["dbg"][1, it * 128:(it + 1) * 128, :], in_=xt_), r=[kx], w=["dbg1"], ch="dbg1")
        rms_stats(I, xt_, kx, smA, "smA", junk, ["junk"])
        I("dve", lambda e: e.scalar_tensor_tensor(out=h3, in0=xt_, scalar=smA[:, 2:3], in1=wbc[:, 0:1024], op0=OP.mult, op1=OP.mult), r=[kx, "smArstd", "wbc"], w=["h3"])
        I("act", lambda e: e.activation(out=xnb, in_=h3, func=AF.Copy), r=["h3"], w=["xnb"])
        to_hT(I, None)
        for c in range(16):
            pb = 6 + c % 2
            for k in range(8):
                I("pe", lambda e, k=k, c=c, pb=pb: e.matmul(psb[pb][:, 0:128], lhsT=big[:, k, c * 128:(c + 1) * 128], rhs=hT[:, k, :], start=(k == 0), stop=(k == 7)),
                  r=["hT", "wts"], w=[("bank", pb)])
            I("act", lambda e, c=c, pb=pb: e.activation(out=qpT[:, c, :], in_=psb[pb][:, 0:128], func=AF.Copy), r=[("bank", pb)], w=["qpT"])
        for c in range(16):
            pb = 4 + c % 2
            I("pe", lambda e, c=c, pb=pb: e.matmul(psb[pb][:, 0:128], lhsT=qpT[:, c, :], rhs=skt[:, c, :], start=True, stop=True), r=["qpT", "skt"], w=[("bank", pb)])
            I("act", lambda e, c=c, pb=pb: e.activation(out=scs[:, c, :], in_=psb[pb][:, 0:128], func=AF.Copy), r=[("bank", pb)], w=[("scs", c)])
            I("dve", lambda e, c=c: e.max(out=tv[:, c, 0:8], in_=scs[:, c, :]), r=[("scs", c)], w=[("tv", c)])
            I("dve", lambda e, c=c: e.max_index(out=tiu[:, c, 0:8], in_max=tv[:, c, 0:8], in_values=scs[:, c, :]), r=[("scs", c), ("tv", c)], w=[("tiu", c)])
            I("dve", lambda e, c=c: e.match_replace(out=sct, in_to_replace=tv[:, c, 0:8], in_values=scs[:, c, :], imm_value=-1e30), r=[("scs", c), ("tv", c)], w=["sct"])
            I("dve", lambda e, c=c: e.max(out=tv[:, c, 8:16], in_=sct), r=["sct"], w=[("tv", c)])
            I("dve", lambda e, c=c: e.max_index(out=tiu[:, c, 8:16], in_max=tv[:, c, 8:16], in_values=sct), r=["sct", ("tv", c)], w=[("tiu", c)])
            I("dve", lambda e, c=c: e.tensor_copy(out=tif[:, c, :], in_=tiu[:, c, :]), r=[("tiu", c)], w=[("tif", c)])
        for h in range(8):
            c1, c2 = 2 * h, 2 * h + 1
            c3 = cand.rearrange("p (a b) -> p a b", a=16)
            I("dve", lambda e, c1=c1, c2=c2, c3=c3: e.tensor_tensor(out=c3, in0=tv[:, c1, :].unsqueeze(2).broadcast_to([128, 16, 16]),
                                                                    in1=tv[:, c2, :].unsqueeze(1).broadcast_to([128, 16, 16]), op=OP.add),
              r=[("tv", c1), ("tv", c2)] + [("scs", c) for c in range(16)], w=["cand"])
            I("dve", lambda e, c1=c1, c2=c2: e.scalar_tensor_tensor(out=cidx.rearrange("p (a b) -> p a b", a=16), in0=tif[:, c1, :].unsqueeze(2).broadcast_to([128, 16, 16]), scalar=128.0,
                                                                    in1=tif[:, c2, :].unsqueeze(1).broadcast_to([128, 16, 16]), op0=OP.mult, op1=OP.add),
              r=[("tif", c1), ("tif", c2)], w=["cidx"])
            I("dve", lambda e: e.max(out=best[:, 0:8], in_=cand), r=["cand"], w=["best"])
            I("dve", lambda e: e.max_index(out=posu[:, 0:8], in_max=best[:, 0:8], in_values=cand), r=["cand", "best"], w=["posu"])
            I("dve", lambda e: e.match_replace(out=cand2, in_to_replace=best[:, 0:8], in_values=cand, imm_value=-1e30), r=["cand", "best"], w=["cand2"])
            I("dve", lambda e: e.max(out=best[:, 8:16], in_=cand2), r=["cand2"], w=["best"])
            I("dve", lambda e: e.max_index(out=posu[:, 8:16], in_max=best[:, 8:16], in_values=cand2), r=["cand2", "best"], w=["posu"])
            I("dve", lambda e: e.tensor_copy(out=posf, in_=posu), r=["posu"], w=["posf"])
            I("dve", lambda e: e.tensor_tensor(out=oh, in0=iota16.unsqueeze(1).broadcast_to([128, 16, 256]), in1=posf.unsqueeze(2).broadcast_to([128, 16, 256]), op=OP.is_equal),
              r=["iota", "posf", "qpT"] + [("scs", c) for c in range(16)], w=["oh"])
            I("dve", lambda e: e.tensor_tensor(out=oh, in0=oh, in1=cidx.unsqueeze(1).broadcast_to([128, 16, 256]), op=OP.mult), r=["oh", "cidx"], w=["oh"])
            I("dve", lambda e, h=h: e.tensor_reduce(out=idxf[:, h * 16:(h + 1) * 16], in_=oh, axis=AX.X, op=OP.add), r=["oh"], w=["idxf"])
            I("dve", lambda e: e.tensor_scalar(out=smA[:, 3:4], in0=best[:, 0:1], scalar1=-1.0, scalar2=None, op0=OP.mult), r=["best"], w=["nmax"])
            I("act", lambda e, h=h: e.activation(out=gate_[:, h * 16:(h + 1) * 16], in_=best, func=AF.Exp, bias=smA[:, 3:4], accum_out=smA[:, 4:5]), r=["best", "nmax"], w=[kg, "gsum"])
            I("dve", lambda e: e.reciprocal(out=smA[:, 5:6], in_=smA[:, 4:5]), r=["gsum"], w=["grs"])
            I("dve", lambda e, h=h: e.tensor_scalar(out=gate_[:, h * 16:(h + 1) * 16], in0=gate_[:, h * 16:(h + 1) * 16], scalar1=smA[:, 5:6], scalar2=None, op0=OP.mult), r=[kg, "grs"], w=[kg])
        I("dve", lambda e: e.tensor_copy(out=idxu_, in_=idxf), r=["idxf"], w=[ki])
        idxT_ = idxTs[par]
        I("pe", lambda e: e.matmul(psb[7][:, 0:128], lhsT=idxf, rhs=ident, start=True, stop=True), r=["idxf", "ident"], w=[("bank", 7)])
        I("dve", lambda e: e.tensor_copy(out=idxT_, in_=psb[7][:, 0:128]), r=[("bank", 7)], w=[("idxT", par)])

    def stageG(it, par, pump):
        xt_ = xts[par]; kx = ("xt", par); gate_ = gates[par]; idxu_ = idxus[par]; kg = ("gate", par); ki = ("idxu", par)
        P.I("dve", lambda e: e.tensor_copy(out=h3p, in_=h3), r=["h3"], w=[("bank", 0), ("bank", 1)])
        for j in range(128):
            sb = j % NB
            b = gb[sb]
            P.DMA("pool", lambda e, j=j, b=b: e.indirect_dma_start(out=b, out_offset=None, in_=G["ub"].ap()[:, :],
                                                                   in_offset=bass.IndirectOffsetOnAxis(ap=idxu_[:, j:j + 1], axis=0)),
                  r=[ki, "ub"], w=[("gb", sb)], ch=("gb", sb))
            P.I("dve", lambda e, j=j, b=b: e.scalar_tensor_tensor(out=junkb2, in0=b, scalar=1.0, in1=h3p, op0=OP.mult, op1=OP.mult, accum_out=sval[:, j:j + 1]),
                r=[("gb", sb)], w=[("sval", j)], ro=[("bank", 0), ("bank", 1)])
            pump()
        P.I("act", lambda e: e.activation(out=act, in_=sval, func=AF.Gelu_apprx_tanh), r=[("sval", j) for j in range(128)], w=["act"])
        P.I("dve", lambda e: e.tensor_tensor(out=act, in0=act, in1=gate_, op=OP.mult), r=["act", kg], w=["act"])
        idxT_ = idxTs[par]; kiT = ("idxT", par)
        P.I("pe", lambda e: e.matmul(psb[2][:, 0:128], lhsT=act, rhs=ident, start=True, stop=True), r=["act", "ident"], w=[("bank", 2)])
        P.I("act", lambda e: e.activation(out=actT, in_=psb[2][:, 0:128], func=AF.Copy), r=[("bank", 2)], w=["actT"])
        for t in range(128):
            sb = t % NB
            b = gb[sb]
            P.DMA("pool", lambda e, t=t, b=b: e.indirect_dma_start(out=b, out_offset=None, in_=G["vb"].ap()[:, :],
                                                                   in_offset=bass.IndirectOffsetOnAxis(ap=idxT_[:, t:t + 1], axis=0)),
                  r=[kiT, "vb"], w=[("gb", sb)], ch=("gb", sb))
            for c in range(8):
                P.I("pe", lambda e, t=t, b=b, c=c: e.matmul(accT[:, c * 128 + t:c * 128 + t + 1], lhsT=b[:, c * 128:(c + 1) * 128], rhs=actT[:, t:t + 1], start=True, stop=True),
                    r=[("gb", sb), "actT"], w=[("bank", 2 + c // 4)])
            pump()
        P.I("act", lambda e: e.activation(out=accTs, in_=accT, func=AF.Copy), r=[("bank", 2), ("bank", 3)], w=["accTs"])
        for c in range(8):
            P.I("pe", lambda e, c=c: e.matmul(psb[c // 4][:, (c % 4) * 128:(c % 4 + 1) * 128], lhsT=accTs[:, c * 128:(c + 1) * 128], rhs=ident, start=True, stop=True),
                r=["accTs", "ident"], w=[("bank", c // 4)])
        P.I("dve", lambda e: e.tensor_tensor(out=xt_, in0=h3p, in1=xt_, op=OP.add), r=[("bank", 0), ("bank", 1), kx], w=[kx])
        if debug:
            P.DMA("sp", lambda e: e.dma_start(out=G["dbg"][2, it * 128:(it + 1) * 128, :], in_=xt_), r=[kx], w=["dbg2"], ch="dbg2")
        rms_stats(P.I, xt_, kx, smG, "smG", h3p, [("bank", 0), ("bank", 1)])
        P.I("dve", lambda e: e.scalar_tensor_tensor(out=acc, in0=xt_, scalar=smG[:, 2:3], in1=wbc[:, 1024:2048], op0=OP.mult, op1=OP.mult), r=[kx, "smGrstd", "wbc"], w=["acc"])
        P.DMA("sp", lambda e: e.dma_start(out=G["out"][it * 128:(it + 1) * 128, :], in_=acc), r=["acc"], w=["outd"], ch="outd")

    pending = collections.deque()

    def Idef(*a, **k):
        pending.append(("I", a, k))

    def DMAdef(*a, **k):
        pending.append(("DMA", a, k))

    npump = [3]

    def pump(n=None):
        for _ in range(npump[0] if n is None else n):
            if not pending:
                return
            kind, a, k = pending.popleft()
            getattr(P, kind)(*a, **k)

    stageA(0, 0, P.I, P.DMA)
    for it in range(NT):
        if it + 1 < NT:
            stageA(it + 1, (it + 1) % 2, Idef, DMAdef)
            npump[0] = len(pending) // 250 + 1
        stageG(it, it % 2, pump)
        while pending:
            pump(1000)


def prep_inputs(inp):
    f = lambda a: np.ascontiguousarray(np.asarray(a, dtype=np.float32))
    x = f(inp["x"]); mem = f(inp["mem"])
    w_in = f(inp["w_in"])[0]
    cq = f(inp["conv_qkv_w"])[0]; lcw = f(inp["lru_conv_w"])[0]
    maps = []
    for c in range(NCORES):
        b, j = c // 4, c % 4
        cols = []
        for base in (0, 512, 1024, 2056, 2568, 1536):
            cols.append(w_in[:, base + j * 128: base + (j + 1) * 128])
        cols.append(np.repeat(w_in[:, 2048 + j:2049 + j], 128, axis=1))
        cols.append(np.repeat(w_in[:, 2052 + j:2053 + j], 128, axis=1))
        w1 = np.ascontiguousarray(np.concatenate(cols, axis=1))
        cwm = np.zeros((128, 16), np.float32)
        for s_, base in enumerate((0, 512, 1024)):
            cwm[:, 4 * s_:4 * s_ + 4] = cq[:, base + j * 128: base + (j + 1) * 128].T
        cwm[:, 12:16] = lcw[:, j * 128:(j + 1) * 128].T
        pvm = np.zeros((128, 40), np.float32)
        sl = slice(j * 128, (j + 1) * 128)
        pvm[:, 0] = f(inp["lru_conv_b"])[0, sl]
        pvm[:, 1] = f(inp["lru_ba"])[0, sl]
        pvm[:, 2] = f(inp["lru_bx"])[0, sl]
        pvm[:, 3] = f(inp["lru_lambda"])[0, sl]
        pvm[:, 4] = f(inp["gdn_a_log"])[0, j]
        pvm[:, 5] = f(inp["gdn_dt_bias"])[0, j]
        pvm[:, 6] = f(inp["gdn_norm_w"])[0]
        pvm[:, 8:16] = f(inp["norm_mix_w"])[0].reshape(8, 128).T
        pvm[:, 16:24] = f(inp["norm_xattn_w"])[0].reshape(8, 128).T
        pvm[:, 24:32] = f(inp["norm_mem_w"])[0].reshape(8, 128).T
        lwm = np.stack([f(inp["lru_wa"])[0, 2 * j:2 * j + 2], f(inp["lru_wx"])[0, 2 * j:2 * j + 2]])
        skT = np.ascontiguousarray(f(inp["peer_subkeys"])[0].reshape(16, 128, 128).transpose(2, 0, 1).reshape(128, 16 * 128))
        maps.append({
            "x_b": x[b], "w1": w1, "cw": cwm, "pv": pvm, "lw": np.ascontiguousarray(lwm),
            "mem_b": mem[b], "w_out": f(inp["w_out"])[0], "xwq": f(inp["xattn_wq"])[0],
            "xwkv": f(inp["xattn_wkv"])[0], "xwo": f(inp["xattn_wo"])[0], "pwq": f(inp["peer_wq"])[0],
            "skT": skT, "peer_u": f(inp["peer_u"])[0], "peer_v": f(inp["peer_v"])[0],
            "rows": np.stack([f(inp["norm_ffn_w"])[0], f(inp["norm_final_w"])]),
            "x_tok": np.ascontiguousarray(x[b, j * TOK:(j + 1) * TOK]),
            "idxy": np.ascontiguousarray((j * 16384 + np.arange(4)[None, :, None] * 4096 + np.arange(2)[None, None, :] * 128
                                          + np.arange(128)[:, None, None]).reshape(128, 8).astype(np.uint32)),
        })
    return maps


_NC = {}


def kernel(**inputs):
    if "nc" not in _NC:
        _NC["nc"] = build(False)
    maps = prep_inputs(inputs)
    res = run_bass_kernel_spmd(_NC["nc"], maps, core_ids=list(range(NCORES)))
    o = np.concatenate([res.results[c]["out"] for c in range(NCORES)], axis=0)
    return o.reshape(2, SEQ, D).astype(np.float32)
```

```python
import contextlib
import numpy as np
import concourse.bass as bass
import concourse.mybir as mybir
from concourse.bass_utils import run_bass_kernel_spmd

F32 = mybir.dt.float32
BF16 = mybir.dt.bfloat16
U32 = mybir.dt.uint32
AF = mybir.ActivationFunctionType
OP = mybir.AluOpType
AX = mybir.AxisListType

NCORES = 8
D = 1024
SEQ = 8192
TOK = 2048
EPS = 1e-6
ENG = ("pe", "act", "dve", "pool", "sp")


class Prog:
    def __init__(self, nc):
        self.nc = nc
        self.ops = {e: [] for e in ENG}
        self.cnt = {}
        self.epoch = {e: 0 for e in ENG}
        self.waited = {}
        self.lastw = {}
        self.reads = {}
        self.semkeys = []
        self.sems = {}

    def _semkey_engine(self, e):
        k = ("E", e, self.epoch[e])
        if self.cnt.get(k, 0) >= 30000:
            self.epoch[e] += 1
            k = ("E", e, self.epoch[e])
        return k

    def _deps(self, eng, r, w, skip_same_pe):
        deps = {}
        def add(sv):
            if sv is None:
                return
            k, v = sv
            if skip_same_pe and k[0] == "E" and k[1] == "pe":
                return
            if deps.get(k, 0) < v:
                deps[k] = v
        for b in r:
            add(self.lastw.get(b))
        for b in w:
            add(self.lastw.get(b))
            for sv in self.reads.get(b, ()):
                add(sv)
        out = []
        for k, v in deps.items():
            if self.waited.get((eng, k), 0) < v:
                self.waited[(eng, k)] = v
                out.append((k, v))
        return out

    def _commit(self, r, w, sv):
        for b in r:
            self.reads.setdefault(b, []).append(sv)
        for b in w:
            self.lastw[b] = sv
            self.reads[b] = []

    def _newsem(self, k):
        if k not in self.cnt:
            self.cnt[k] = 0
            self.semkeys.append(k)

    def I(self, eng, fn, r=(), w=(), ro=()):
        bk = [b for b in r if isinstance(b, tuple) and b[0] == "bank"]
        r = list(r) + list(ro)
        if bk:
            r = [b for b in r if b not in bk]
            w = list(w) + bk
        waits = self._deps(eng, r, w, skip_same_pe=(eng == "pe"))
        k = self._semkey_engine(eng)
        self._newsem(k)
        self.cnt[k] += 1
        sv = (k, self.cnt[k])
        self._commit(r, w, sv)
        self.ops[eng].append((waits, fn, k, 1))

    def DMA(self, eng, fn, r=(), w=(), ch=None):
        waits = self._deps(eng, r, w, skip_same_pe=False)
        k = ("D", ch, 0)
        n = 0
        while self.cnt.get(("D", ch, n), 0) >= 30000:
            n += 1
        k = ("D", ch, n)
        self._newsem(k)
        self.cnt[k] += 16
        sv = (k, self.cnt[k])
        self._commit(r, w, sv)
        self.ops[eng].append((waits, fn, k, 16))

    def CC(self, eng, fn, r=(), w=()):
        waits = self._deps(eng, r, w, skip_same_pe=False)
        k = ("C", "cc", 0)
        self._newsem(k)
        self.cnt[k] += 1
        sv = (k, self.cnt[k])
        self._commit(r, w, sv)
        self.ops[eng].append((waits, fn, k, 1))

    def barrier(self):
        for e in ENG:
            waits = []
            for k, v in self.cnt.items():
                if v > 0 and self.waited.get((e, k), 0) < v:
                    self.waited[(e, k)] = v
                    waits.append((k, v))
            if waits:
                self.ops[e].append((waits, None, None, 0))

    def emit(self):
        nc = self.nc
        with contextlib.ExitStack() as st:
            for i, k in enumerate(self.semkeys):
                self.sems[k] = st.enter_context(nc.semaphore("s%d" % i))
            block = st.enter_context(nc.Block())
            engobj = {"pe": "tensor", "act": "scalar", "dve": "vector", "pool": "gpsimd", "sp": "sync"}

            def run(e, ename):
                for waits, fn, k, inc in self.ops[ename]:
                    for wk, wv in waits:
                        e.wait_ge(self.sems[wk], wv)
                    if fn is not None:
                        fn(e).then_inc(self.sems[k], inc)

            @block.tensor
            def _(e):
                run(e, "pe")

            @block.scalar
            def _(e):
                run(e, "act")

            @block.vector
            def _(e):
                run(e, "dve")

            @block.gpsimd
            def _(e):
                run(e, "pool")
            @block.sync
            def _(e):
                run(e, "sp")


class Arena:
    def __init__(self, t, n):
        self.t = t
        self.n = n
        self.off = 0
        self.mark = 0

    def f32(self, cols):
        o = self.off
        self.off += cols
        assert self.off <= self.n, ("arena overflow", self.off, self.n)
        return self.t[:, o:o + cols]

    def bf16(self, cols):
        c = (cols + 1) // 2
        return self.f32(c).bitcast(BF16)

    def u32(self, cols):
        return self.f32(cols).bitcast(U32)


def build(debug=False, p2=True, nblk=16, cc=True, stop=0, ntile=16):
    nc = bass.Bass("TRN2", target_bir_lowering=False)
    P = Prog(nc)

    def din(name, shape, dt=F32):
        return nc.dram_tensor(name, list(shape), dt, kind="ExternalInput").ap()

    x_b = din("x_b", [SEQ, D])
    w1 = din("w1", [D, 1024])
    cw = din("cw", [128, 16])
    pv = din("pv", [128, 40])
    lw = din("lw", [2, 2, 64, 64])
    if not p2:
        din = lambda *a, **k: None
    mem_b = din("mem_b", [256, D])
    w_out = din("w_out", [D, D])
    xwq = din("xwq", [D, D])
    xwkv = din("xwkv", [D, 2 * D])
    xwo = din("xwo", [D, D])
    pwq = din("pwq", [D, 2 * D])
    skT = din("skT", [128, 16 * 128])
    peer_u = din("peer_u", [16384, D])
    peer_v = din("peer_v", [16384, D])
    rows = din("rows", [2, D])
    out = nc.dram_tensor("out", [TOK, D], F32, kind="ExternalOutput").ap()
    cin_t = [nc.dram_tensor("cin%d" % q, [256, 2048], BF16) for q in range(4)]
    coutall = nc.dram_tensor("coutall", [4, 1024, 2048], BF16)
    cin = [t.ap().rearrange("r (x t) -> (r x) t", t=128) for t in cin_t]
    coutflat = coutall.ap().rearrange("q r (x t) -> (q r x) t", t=128)
    ub = nc.dram_tensor("ub", [16384, D], BF16) if p2 else None
    vb = nc.dram_tensor("vb", [16384, D], BF16) if p2 else None
    idxy = din("idxy", [128, 8], U32)
    x_tok = din("x_tok", [TOK, D])
    if debug:
        dbg = nc.dram_tensor("dbg", [3, TOK, D], F32, kind="ExternalOutput").ap()

    st = contextlib.ExitStack()
    NA = 49000
    arena_t = st.enter_context(nc.sbuf_tensor("arena", [128, NA], F32))
    ps_all = st.enter_context(nc.psum_tensor("ps_all", [128, 4096], F32))
    psb = [ps_all[:, i * 512:(i + 1) * 512] for i in range(8)]

    A = Arena(arena_t, NA)
    ident = A.f32(128)
    identb = A.bf16(128)
    ones = A.f32(512)
    onesb = A.bf16(128)
    mU = A.f32(128)
    mUs = A.f32(128)
    cwt = A.f32(16)
    pvt = A.f32(40)
    A.mark = A.off

    P.I("pool", lambda e: e.memset(ones, 1.0), w=["ones"])
    P.I("pool", lambda e: e.memset(onesb, 1.0), w=["onesb"])
    P.I("pool", lambda e: e.affine_select(out=ident, in_=ones[:, 0:128], pattern=[[-1, 128]],
                                          compare_op=OP.is_equal, fill=0.0, base=0, channel_multiplier=1),
        r=["ones"], w=["ident"])
    P.I("pool", lambda e: e.tensor_copy(out=identb, in_=ident), r=["ident"], w=["identb"])
    P.I("pool", lambda e: e.affine_select(out=mU, in_=ones[:, 0:128], pattern=[[1, 128]],
                                          compare_op=OP.is_ge, fill=0.0, base=0, channel_multiplier=-1),
        r=["ones"], w=["mU"])
    P.I("pool", lambda e: e.affine_select(out=mUs, in_=ones[:, 0:128], pattern=[[1, 128]],
                                          compare_op=OP.is_gt, fill=0.0, base=0, channel_multiplier=-1),
        r=["ones"], w=["mUs"])
    P.DMA("sp", lambda e: e.dma_start(out=cwt, in_=cw[:, :]), w=["cwt"], ch="c_cwt")
    P.DMA("sp", lambda e: e.dma_start(out=pvt, in_=pv[:, :]), w=["pvt"], ch="c_pvt")

    PV_CB, PV_BA, PV_BX, PV_LAM, PV_ALOG, PV_DTB, PV_GNW = 0, 1, 2, 3, 4, 5, 6
    PV_NMIX, PV_NXA, PV_NMEM = 8, 16, 24

    def col(i):
        return pvt[:, i:i + 1]

    phase1(nc, P, A, locals())
    P.barrier()
    if cc:
      for q in range(4):
        P.CC("pool", lambda e, q=q: e.collective_compute("AllGather", OP.bypass,
                                                        replica_groups=[[0, 1, 2, 3], [4, 5, 6, 7]],
                                                        ins=[cin_t[q].ap()], outs=[coutall.ap()[q]]),
             r=["cin"], w=["cout"])
    A.off = A.mark
    if p2:
        phase2(nc, P, A, locals())
    P.barrier()
    P.emit()
    st.close()
    return nc


def phase1(nc, P, A, G):
    psb = G["psb"]; ident = G["ident"]; identb = G["identb"]; ones = G["ones"]
    mU = G["mU"]; mUs = G["mUs"]; cwt = G["cwt"]; pvt = G["pvt"]; col = G["col"]
    x_b = G["x_b"]; w1 = G["w1"]; lw = G["lw"]; cin = G["cin"]
    PV_CB, PV_BA, PV_BX, PV_LAM, PV_ALOG, PV_DTB, PV_GNW, PV_NMIX = 0, 1, 2, 3, 4, 5, 6, 8

    w1b = A.bf16(8 * 1024).rearrange("p (k c) -> p k c", k=8)
    wabd = A.f32(128)
    wxbd = A.f32(128)
    sc1 = A.f32(16)
    negsp8, negsp16, nA, dtb = sc1[:, 0:1], sc1[:, 1:2], sc1[:, 2:3], sc1[:, 3:4]
    tmpc = sc1[:, 4:6]
    xin = [A.f32(4 * 1024).rearrange("p (s d) -> p s d", s=4) for _ in range(2)]
    junkb = A.bf16(1024)
    ssq = A.f32(4); msq = A.f32(4); rstd = A.f32(4)
    xn = A.bf16(4 * 1024).rearrange("p (s d) -> p s d", s=4)
    hT = A.bf16(8 * 512).rearrange("p (k t) -> p k t", k=8)
    cb = [A.f32(515) for _ in range(4)]
    cy = [A.f32(512) for _ in range(4)]
    qs = A.f32(512); ks = A.f32(512); vs = A.f32(512)
    gg = A.f32(512); zs = A.f32(512); grow = A.f32(512); brow = A.f32(512)
    t512 = [A.f32(512) for _ in range(8)]
    qn = A.f32(512); kn = A.f32(512)
    hprev = A.f32(1)
    ystage = [A.bf16(2 * 512).rearrange("p (g t) -> p g t", g=2) for _ in range(2)]
    S = [A.f32(128) for _ in range(2)]
    NCH = 4
    HB = {nm: [buf, A.f32(512)] for nm, buf in (("qn", qn), ("kn", kn), ("vs", vs), ("grow", grow), ("brow", brow), ("zs", zs))}
    def cbufs():
        d = {}
        for nm in ["gcum", "arg", "DT", "eg", "ekd", "t1", "t2", "t3", "kbgT", "vbT", "qdT", "kdT",
                   "B", "Am", "M", "B2", "A2", "kbg", "vb", "kd", "wT", "u", "attnT", "vnew", "on"]:
            d[nm] = A.f32(128)
        d["small"] = A.f32(8)
        return d
    CB = [cbufs() for _ in range(NCH)]
    pTs = [psb[6][:, :].bitcast(BF16), psb[7][:, :].bitcast(BF16)]

    P.DMA("pool", lambda e: e.dma_start(out=w1b, in_=w1.rearrange("(k p) c -> p k c", p=128)), w=["w1b"], ch="w1")
    P.I("pool", lambda e: e.memset(wabd, 0.0), w=["wabd"])
    P.I("pool", lambda e: e.memset(wxbd, 0.0), w=["wxbd"])
    for i in range(2):
        P.DMA("sp", lambda e, i=i: e.dma_start(out=wabd[i * 64:(i + 1) * 64, i * 64:(i + 1) * 64], in_=lw[0, i]), w=["wabd"], ch="c_wabd")
        P.DMA("sp", lambda e, i=i: e.dma_start(out=wxbd[i * 64:(i + 1) * 64, i * 64:(i + 1) * 64], in_=lw[1, i]), w=["wxbd"], ch="c_wxbd")
    P.I("act", lambda e: e.activation(out=tmpc[:, 0:1], in_=col(PV_LAM), func=AF.Exp, scale=-1.0), r=["pvt"], w=["tmpc"])
    P.I("act", lambda e: e.activation(out=tmpc[:, 1:2], in_=tmpc[:, 0:1], func=AF.Ln, bias=1.0), r=["tmpc"], w=["tmpc2"])
    P.I("dve", lambda e: e.tensor_scalar(out=negsp8, in0=tmpc[:, 1:2], scalar1=-8.0, scalar2=None, op0=OP.mult), r=["tmpc2"], w=["negsp8"])
    P.I("dve", lambda e: e.tensor_scalar(out=negsp16, in0=tmpc[:, 1:2], scalar1=-16.0, scalar2=None, op0=OP.mult), r=["tmpc2"], w=["negsp16"])
    P.I("act", lambda e: e.activation(out=nA, in_=col(PV_ALOG), func=AF.Exp), r=["pvt"], w=["nA0"])
    P.I("dve", lambda e: e.tensor_scalar(out=nA, in0=nA, scalar1=-1.0, scalar2=None, op0=OP.mult), r=["nA0"], w=["nA"])
    for c in range(4):
        P.I("pool", lambda e, c=c: e.memset(cb[c][:, 0:3], 0.0), w=[("cbh", c)])
    P.I("pool", lambda e: e.memset(S[0], 0.0), w=[("S", 0)])
    P.I("pool", lambda e: e.memset(hprev, 0.0), w=["hprev"])

    NBLK = G['nblk']
    stop = G['stop']
    ps_rot = [0]

    def load_x(blk, DMA):
        s = blk % 2
        DMA("sp", lambda e: e.dma_start(out=xin[s], in_=x_b[blk * 512:(blk + 1) * 512, :].rearrange("(s p) d -> p s d", p=128)),
              w=[("xin", s)], ch=("xin", s))

    import collections
    pending = collections.deque()

    def Idef(*a, **k):
        pending.append(("I", a, k))

    def DMAdef(*a, **k):
        pending.append(("DMA", a, k))

    cnt = [0]

    def CI(*a, **k):
        P.I(*a, **k)
        cnt[0] += 1
        if cnt[0] % 3 != 0 and pending:
            kind, a2, k2 = pending.popleft()
            getattr(P, kind)(*a2, **k2)

    def front(blk, I, DMA):
        sl = blk % 2
        par = blk % 2
        qn, kn, vs, grow, brow, zs = (HB[nm][par] for nm in ("qn", "kn", "vs", "grow", "brow", "zs"))
        if G["ub"] is not None:
            for ck in range(blk * 16 // NBLK, (blk + 1) * 16 // NBLK):
                for (src, dst, nm) in ((G["peer_u"], G["ub"], "ub"), (G["peer_v"], G["vb"], "vb")):
                    DMA("pool", lambda e, ck=ck, src=src, dst=dst: e.dma_start(out=dst.ap()[ck * 1024:(ck + 1) * 1024, :], in_=src[ck * 1024:(ck + 1) * 1024, :]),
                          w=[nm], ch="cvt")
        if blk + 1 < NBLK:
            load_x(blk + 1, DMA)
        X = xin[sl]
        for s in range(4):
            I("act", lambda e, s=s, X=X: e.activation(out=junkb, in_=X[:, s, :], func=AF.Square, accum_out=ssq[:, s:s + 1]),
                r=[("xin", sl)], w=["junkb", ("ssq", s)])
        I("dve", lambda e: e.tensor_scalar(out=msq, in0=ssq, scalar1=1.0 / D, scalar2=EPS, op0=OP.mult, op1=OP.add),
            r=[("ssq", s) for s in range(4)], w=["msq"])
        I("act", lambda e: e.activation(out=msq, in_=msq, func=AF.Sqrt), r=["msq"], w=["msq"])
        I("dve", lambda e: e.reciprocal(out=rstd, in_=msq), r=["msq"], w=["rstd"])
        for s in range(4):
            eng = "dve"
            I(eng, lambda e, s=s, X=X: e.tensor_scalar(out=xn[:, s, :], in0=X[:, s, :], scalar1=rstd[:, s:s + 1], scalar2=None, op0=OP.mult),
                r=[("xin", sl), "rstd"], w=[("xn", s)])
        for k in range(8):
            half = k % 2
            for s in range(4):
                I("pe", lambda e, k=k, s=s, half=half: e.transpose(out=pTs[half][:, s * 128:(s + 1) * 128],
                                                                     in_=xn[:, s, k * 128:(k + 1) * 128], identity=identb),
                    r=[("xn", s), "identb"], w=[("bank", 6 + half)])
            if False:
                I("act", lambda e, k=k, half=half: e.activation(out=hT[:, k, :], in_=pTs[half][:, 0:512], func=AF.Copy,
                                                                  scale=col(PV_NMIX + k)),
                    r=[("bank", 6 + half), "pvt"], w=[("hT", k)])
            else:
                I("dve", lambda e, k=k, half=half: e.tensor_scalar(out=hT[:, k, :], in0=pTs[half][:, 0:512],
                                                                     scalar1=col(PV_NMIX + k), scalar2=None, op0=OP.mult),
                    r=[("bank", 6 + half), "pvt"], w=[("hT", k)])
        for c in range(8):
            pb = ps_rot[0] % 2
            ps_rot[0] += 1
            pst = psb[pb]
            for k in range(8):
                I("pe", lambda e, c=c, k=k, pst=pst: e.matmul(pst[:, :], lhsT=w1b[:, k, c * 128:(c + 1) * 128], rhs=hT[:, k, :],
                                                                start=(k == 0), stop=(k == 7)),
                    r=["w1b", ("hT", k)], w=[("bank", pb)])
            if c < 4:
                j = c
                if blk > 0:
                    I("dve", lambda e, j=j: e.tensor_copy(out=cb[j][:, 0:3], in_=cb[j][:, 512:515]), r=[("cb", j)], w=[("cbh", j)])
                I("act", lambda e, j=j, pst=pst: e.activation(out=cb[j][:, 3:515], in_=pst[:, :], func=AF.Copy),
                    r=[("bank", pb), ("cbh", j)], w=[("cb", j)])
            elif c == 4:
                I("act", lambda e, pst=pst: e.activation(out=gg, in_=pst[:, :], func=AF.Gelu_apprx_tanh), r=[("bank", pb)], w=["gg"])
            elif c == 5:
                I("act", lambda e, pst=pst: e.activation(out=zs, in_=pst[:, :], func=AF.Silu), r=[("bank", pb)], w=[("zs", par)])
            elif c == 6:
                I("act", lambda e, pst=pst: e.activation(out=grow, in_=pst[:, :], func=AF.Exp, bias=col(PV_DTB)), r=[("bank", pb), "pvt"], w=[("grow", par)])
                I("act", lambda e: e.activation(out=grow, in_=grow, func=AF.Ln, bias=1.0), r=[("grow", par)], w=[("grow", par)])
                I("dve", lambda e: e.tensor_scalar(out=grow, in0=grow, scalar1=nA, scalar2=None, op0=OP.mult), r=[("grow", par), "nA"], w=[("grow", par)])
            else:
                I("act", lambda e, pst=pst: e.activation(out=brow, in_=pst[:, :], func=AF.Sigmoid), r=[("bank", pb)], w=[("brow", par)])
        for j in range(4):
            eng = "dve" if j % 2 == 0 else "pool"
            if j == 3:
                I("dve", lambda e, j=j: e.tensor_scalar(out=cy[j], in0=cb[j][:, 0:512], scalar1=cwt[:, 4 * j:4 * j + 1], scalar2=col(PV_CB),
                                                          op0=OP.mult, op1=OP.add), r=[("cb", j), ("cbh", j), "cwt", "pvt"], w=[("cy", j)])
            else:
                I("dve", lambda e, j=j: e.tensor_scalar(out=cy[j], in0=cb[j][:, 0:512], scalar1=cwt[:, 4 * j:4 * j + 1], scalar2=None,
                                                          op0=OP.mult), r=[("cb", j), ("cbh", j), "cwt"], w=[("cy", j)])
            for tp in range(1, 4):
                I("dve", lambda e, j=j, tp=tp: e.scalar_tensor_tensor(out=cy[j], in0=cb[j][:, tp:tp + 512], scalar=cwt[:, 4 * j + tp:4 * j + tp + 1],
                                                                         in1=cy[j], op0=OP.mult, op1=OP.add),
                    r=[("cb", j), ("cbh", j), "cwt", ("cy", j)], w=[("cy", j)])
        I("act", lambda e: e.activation(out=qs, in_=cy[0], func=AF.Silu), r=[("cy", 0)], w=["qs"])
        I("act", lambda e: e.activation(out=ks, in_=cy[1], func=AF.Silu), r=[("cy", 1)], w=["ks"])
        I("act", lambda e: e.activation(out=vs, in_=cy[2], func=AF.Silu), r=[("cy", 2)], w=[("vs", par)])
        xrc = cy[3]
        r_, i_, a_, a2_, ix_, hs_ = t512[0], t512[1], t512[2], t512[3], t512[4], t512[5]
        I("pe", lambda e: e.matmul(psb[2][:, :], lhsT=wabd, rhs=xrc, start=True, stop=True), r=["wabd", ("cy", 3)], w=[("bank", 2)])
        I("act", lambda e: e.activation(out=r_, in_=psb[2][:, :], func=AF.Sigmoid, bias=col(PV_BA)), r=[("bank", 2), "pvt"], w=["r_"])
        I("pe", lambda e: e.matmul(psb[2][:, :], lhsT=wxbd, rhs=xrc, start=True, stop=True), r=["wxbd", ("cy", 3)], w=[("bank", 2)])
        I("act", lambda e: e.activation(out=i_, in_=psb[2][:, :], func=AF.Sigmoid, bias=col(PV_BX)), r=[("bank", 2), "pvt"], w=["i_"])
        I("act", lambda e: e.activation(out=a_, in_=r_, func=AF.Exp, scale=negsp8), r=["r_", "negsp8"], w=["a_"])
        I("act", lambda e: e.activation(out=a2_, in_=r_, func=AF.Exp, scale=negsp16), r=["r_", "negsp16"], w=["a2_"])
        I("dve", lambda e: e.tensor_scalar(out=a2_, in0=a2_, scalar1=-1.0, scalar2=1.0, op0=OP.mult, op1=OP.add), r=["a2_"], w=["a2_"])
        I("act", lambda e: e.activation(out=a2_, in_=a2_, func=AF.Sqrt), r=["a2_"], w=["a2_"])
        I("pool", lambda e: e.tensor_tensor(out=ix_, in0=i_, in1=xrc, op=OP.mult), r=["i_", ("cy", 3)], w=["ix_"])
        I("pool", lambda e: e.tensor_tensor(out=ix_, in0=ix_, in1=a2_, op=OP.mult), r=["ix_", "a2_"], w=["ix_"])
        I("dve", lambda e: e.tensor_tensor_scan(out=hs_, data0=a_, data1=ix_, initial=hprev[:, 0:1], op0=OP.mult, op1=OP.add),
            r=["a_", "ix_", "hprev"], w=["hs_"])
        I("dve", lambda e: e.tensor_copy(out=hprev, in_=hs_[:, 511:512]), r=["hs_"], w=["hprev"])
        I("pool", lambda e, sl=sl: e.tensor_tensor(out=ystage[sl][:, 1, :], in0=hs_, in1=gg, op=OP.mult), r=["hs_", "gg"], w=[("ystage", sl)])
        sq_, rq_ = t512[6], t512[7]
        for (src, dst, scl, nm) in ((qs, qn, 128.0 ** -0.5, ("qn", par)), (ks, kn, 1.0, ("kn", par))):
            I("pool", lambda e, src=src: e.tensor_tensor(out=sq_, in0=src, in1=src, op=OP.mult), r=["qs", "ks"], w=["sq_"])
            I("pe", lambda e: e.matmul(psb[2][:, :], lhsT=ones[:, 0:128], rhs=sq_, start=True, stop=True), r=["ones", "sq_"], w=[("bank", 2)])
            I("act", lambda e: e.activation(out=rq_, in_=psb[2][:, :], func=AF.Sqrt, bias=EPS), r=[("bank", 2)], w=["rq_"])
            I("dve", lambda e: e.reciprocal(out=rq_, in_=rq_), r=["rq_"], w=["rq_"])
            I("dve", lambda e, src=src, dst=dst, scl=scl: e.scalar_tensor_tensor(out=dst, in0=src, scalar=scl, in1=rq_, op0=OP.mult, op1=OP.mult),
                r=["qs", "ks", "rq_"], w=[nm])

    def chunks(blk):
        sl = blk % 2
        par = blk % 2
        qn, kn, vs, grow, brow, zs = (HB[nm][par] for nm in ("qn", "kn", "vs", "grow", "brow", "zs"))
        slots = [(3, 0), (4, 0), (3, 1), (4, 1), (3, 2), (4, 2), (3, 3), (4, 3)]
        sr = [0]

        def pslot():
            b, q = slots[sr[0] % len(slots)]
            sr[0] += 1
            return psb[b][:, q * 128:(q + 1) * 128], ("bank", b)

        def K(ch, nm):
            return (nm, ch)

        for ch in range(NCH):
            c = CB[ch]
            cs = slice(ch * 128, (ch + 1) * 128)
            sm = c["small"]
            gcol, ngcol, gl, dcol = sm[:, 0:1], sm[:, 1:2], sm[:, 2:3], sm[:, 3:4]
            CI("dve", lambda e, c=c, cs=cs: e.tensor_tensor_scan(out=c["gcum"], data0=ones[:, 0:128], data1=grow[:, cs], initial=0.0,
                                                                   op0=OP.mult, op1=OP.add), r=[("grow", par), "ones"], w=[K(ch, "gcum")])
            pt, pk = pslot()
            CI("pe", lambda e, c=c, pt=pt: e.matmul(pt, lhsT=c["gcum"], rhs=ident, start=True, stop=True), r=[K(ch, "gcum"), "ident"], w=[pk])
            CI("dve", lambda e, pt=pt, ngcol=ngcol: e.tensor_scalar(out=ngcol, in0=pt[:, 0:1], scalar1=-1.0, scalar2=None, op0=OP.mult),
                r=[pk], w=[K(ch, "ngcol")])
            CI("dve", lambda e, c=c, ngcol=ngcol: e.tensor_scalar(out=c["arg"], in0=c["gcum"], scalar1=ngcol, scalar2=0.0, op0=OP.add, op1=OP.min),
                r=[K(ch, "gcum"), K(ch, "ngcol")], w=[K(ch, "arg")])
            CI("act", lambda e, c=c: e.activation(out=c["DT"], in_=c["arg"], func=AF.Exp), r=[K(ch, "arg")], w=[K(ch, "DT")])
            CI("act", lambda e, c=c: e.activation(out=c["eg"], in_=c["gcum"], func=AF.Exp), r=[K(ch, "gcum")], w=[K(ch, "eg")])
            CI("act", lambda e, c=c: e.activation(out=c["ekd"], in_=c["gcum"], func=AF.Exp, scale=-1.0, bias=c["gcum"][:, 127:128]),
                r=[K(ch, "gcum")], w=[K(ch, "ekd")])
            CI("act", lambda e, c=c, dcol=dcol: e.activation(out=dcol, in_=c["gcum"][:, 127:128], func=AF.Exp), r=[K(ch, "gcum")], w=[K(ch, "dcol")])
            CI("pool", lambda e, c=c: e.tensor_tensor(out=c["t1"], in0=c["DT"], in1=mUs, op=OP.mult), r=[K(ch, "DT"), "mUs"], w=[K(ch, "t1")])
            CI("pool", lambda e, c=c, cs=cs: e.tensor_tensor(out=c["t2"], in0=c["t1"], in1=brow[:, cs], op=OP.mult), r=[K(ch, "t1"), ("brow", par)], w=[K(ch, "t2")])
            CI("pool", lambda e, c=c: e.tensor_tensor(out=c["t3"], in0=c["DT"], in1=mU, op=OP.mult), r=[K(ch, "DT"), "mU"], w=[K(ch, "t3")])
            CI("dve", lambda e, c=c, cs=cs: e.tensor_tensor(out=c["vbT"], in0=vs[:, cs], in1=brow[:, cs], op=OP.mult), r=[("vs", par), ("brow", par)], w=[K(ch, "vbT")])
            CI("dve", lambda e, c=c, cs=cs: e.tensor_tensor(out=c["kbgT"], in0=kn[:, cs], in1=brow[:, cs], op=OP.mult), r=[("kn", par), ("brow", par)], w=[K(ch, "kbgT")])
            CI("dve", lambda e, c=c: e.tensor_tensor(out=c["kbgT"], in0=c["kbgT"], in1=c["eg"], op=OP.mult), r=[K(ch, "kbgT"), K(ch, "eg")], w=[K(ch, "kbgT")])
            CI("pool", lambda e, c=c, cs=cs: e.tensor_tensor(out=c["qdT"], in0=qn[:, cs], in1=c["eg"], op=OP.mult), r=[("qn", par), K(ch, "eg")], w=[K(ch, "qdT")])
            CI("pool", lambda e, c=c, cs=cs: e.tensor_tensor(out=c["kdT"], in0=kn[:, cs], in1=c["ekd"], op=OP.mult), r=[("kn", par), K(ch, "ekd")], w=[K(ch, "kdT")])
            pt, pk = pslot()
            CI("pe", lambda e, cs=cs, pt=pt: e.matmul(pt, lhsT=kn[:, cs], rhs=kn[:, cs], start=True, stop=True), r=[("kn", par)], w=[pk])
            CI("dve", lambda e, c=c, pt=pt: e.tensor_tensor(out=c["B"], in0=pt, in1=c["t2"], op=OP.mult), r=[pk, K(ch, "t2")], w=[K(ch, "B")])
            pt, pk = pslot()
            CI("pe", lambda e, cs=cs, pt=pt: e.matmul(pt, lhsT=kn[:, cs], rhs=qn[:, cs], start=True, stop=True), r=[("kn", par), ("qn", par)], w=[pk])
            CI("dve", lambda e, c=c, pt=pt: e.tensor_tensor(out=c["attnT"], in0=pt, in1=c["t3"], op=OP.mult), r=[pk, K(ch, "t3")], w=[K(ch, "attnT")])
            pt, pk = pslot()
            CI("pe", lambda e, c=c, pt=pt: e.matmul(pt, lhsT=c["B"], rhs=ident, start=True, stop=True), r=[K(ch, "B"), "ident"], w=[pk])
            CI("act", lambda e, c=c, pt=pt: e.activation(out=c["Am"], in_=pt, func=AF.Copy), r=[pk], w=[K(ch, "Am")])
            CI("dve", lambda e, c=c: e.tensor_tensor(out=c["M"], in0=ident, in1=c["B"], op=OP.subtract), r=["ident", K(ch, "B")], w=[K(ch, "M")])
            for (srcn, dstn) in (("kbgT", "kbg"), ("vbT", "vb"), ("kdT", "kd")):
                pt, pk = pslot()
                CI("pe", lambda e, c=c, pt=pt, srcn=srcn: e.matmul(pt, lhsT=c[srcn], rhs=ident, start=True, stop=True), r=[K(ch, srcn), "ident"], w=[pk])
                CI("act", lambda e, c=c, pt=pt, dstn=dstn: e.activation(out=c[dstn], in_=pt, func=AF.Copy), r=[pk], w=[K(ch, dstn)])
        cur = [("Am", "B")] * NCH
        for lev in range(6):
            for ch in range(NCH):
                c = CB[ch]
                an, bn = cur[ch]
                na, nb = ("A2", "B2") if an == "Am" else ("Am", "B")
                pt, pk = pslot()
                CI("pe", lambda e, c=c, pt=pt, an=an, bn=bn: e.matmul(pt, lhsT=c[bn], rhs=c[an], start=True, stop=True),
                    r=[K(ch, an), K(ch, bn)], w=[pk])
                pt2, pk2 = (None, None)
                if lev < 5:
                    pt2, pk2 = pslot()
                    CI("pe", lambda e, c=c, pt2=pt2, an=an, bn=bn: e.matmul(pt2, lhsT=c[an], rhs=c[bn], start=True, stop=True),
                        r=[K(ch, an), K(ch, bn)], w=[pk2])
                CI("act", lambda e, c=c, pt=pt, na=na: e.activation(out=c[na], in_=pt, func=AF.Copy), r=[pk], w=[K(ch, na)])
                if lev < 5:
                    CI("dve", lambda e, c=c, pt2=pt2, nb=nb: e.tensor_copy(out=c[nb], in_=pt2), r=[pk2], w=[K(ch, nb)])
                pt3, pk3 = pslot()
                CI("pe", lambda e, c=c, pt3=pt3, na=na: e.matmul(pt3, lhsT=c[na], rhs=c["M"], start=True, stop=True),
                    r=[K(ch, na), K(ch, "M")], w=[pk3])
                CI("dve", lambda e, c=c, pt3=pt3: e.tensor_tensor(out=c["M"], in0=pt3, in1=c["M"], op=OP.add), r=[pk3, K(ch, "M")], w=[K(ch, "M")])
                cur[ch] = (na, nb)
        for ch in range(NCH):
            c = CB[ch]
            pt, pk = pslot()
            CI("pe", lambda e, c=c, pt=pt: e.matmul(pt, lhsT=c["kbg"], rhs=c["M"], start=True, stop=True), r=[K(ch, "kbg"), K(ch, "M")], w=[pk])
            CI("act", lambda e, c=c, pt=pt: e.activation(out=c["wT"], in_=pt, func=AF.Copy), r=[pk], w=[K(ch, "wT")])
            pt, pk = pslot()
            CI("pe", lambda e, c=c, pt=pt: e.matmul(pt, lhsT=c["M"], rhs=c["vb"], start=True, stop=True), r=[K(ch, "vb"), K(ch, "M")], w=[pk])
            CI("act", lambda e, c=c, pt=pt: e.activation(out=c["u"], in_=pt, func=AF.Copy), r=[pk], w=[K(ch, "u")])
        for ch in range(NCH):
            c = CB[ch]
            cs = slice(ch * 128, (ch + 1) * 128)
            n = blk * NCH + ch
            Sc, Sn = S[n % 2], S[(n + 1) % 2]
            kSc, kSn = ("S", n % 2), ("S", (n + 1) % 2)
            sm = c["small"]
            dcol = sm[:, 3:4]; osq = sm[:, 4:5]; orstd = sm[:, 5:6]
            p_ws, p_o, p_ks, p_t = (psb[5][:, q * 128:(q + 1) * 128] for q in range(4))
            CI("pe", lambda e, c=c, Sc=Sc, p_ws=p_ws: e.matmul(p_ws, lhsT=c["wT"], rhs=Sc, start=True, stop=True), r=[K(ch, "wT"), kSc], w=[("bank", 5)])
            CI("dve", lambda e, c=c, p_ws=p_ws: e.tensor_tensor(out=c["vnew"], in0=c["u"], in1=p_ws, op=OP.subtract), r=[K(ch, "u"), ("bank", 5)], w=[K(ch, "vnew")])
            CI("pe", lambda e, c=c, Sc=Sc, p_o=p_o: e.matmul(p_o, lhsT=c["qdT"], rhs=Sc, start=True, stop=False), r=[K(ch, "qdT"), kSc, K(ch, "vnew"), K(ch, "attnT")], w=[("bank", 5)])
            CI("pe", lambda e, c=c, p_o=p_o: e.matmul(p_o, lhsT=c["attnT"], rhs=c["vnew"], start=False, stop=True), r=[K(ch, "attnT"), K(ch, "vnew")], w=[("bank", 5)])
            CI("pe", lambda e, c=c, p_ks=p_ks: e.matmul(p_ks, lhsT=c["kd"], rhs=c["vnew"], start=True, stop=True), r=[K(ch, "kd"), K(ch, "vnew")], w=[("bank", 5)])
            CI("dve", lambda e, Sc=Sc, Sn=Sn, dcol=dcol, p_ks=p_ks: e.scalar_tensor_tensor(out=Sn, in0=Sc, scalar=dcol, in1=p_ks, op0=OP.mult, op1=OP.add),
                r=[kSc, K(ch, "dcol"), ("bank", 5)], w=[kSn])
            CI("act", lambda e, c=c, p_o=p_o, osq=osq: e.activation(out=c["on"], in_=p_o, func=AF.Copy), r=[("bank", 5)], w=[K(ch, "on")])
            CI("act", lambda e, c=c, osq=osq: e.activation(out=c["t1"], in_=c["on"], func=AF.Square, accum_out=osq), r=[K(ch, "on")], w=[K(ch, "t1"), K(ch, "osq")])
            CI("dve", lambda e, osq=osq, orstd=orstd: e.tensor_scalar(out=orstd, in0=osq, scalar1=1.0 / 128, scalar2=EPS, op0=OP.mult, op1=OP.add), r=[K(ch, "osq")], w=[K(ch, "orstd")])
            CI("act", lambda e, orstd=orstd: e.activation(out=orstd, in_=orstd, func=AF.Sqrt), r=[K(ch, "orstd")], w=[K(ch, "orstd")])
            CI("dve", lambda e, orstd=orstd: e.reciprocal(out=orstd, in_=orstd), r=[K(ch, "orstd")], w=[K(ch, "orstd")])
            CI("dve", lambda e, c=c, orstd=orstd: e.tensor_scalar(out=c["t2"], in0=c["on"], scalar1=orstd, scalar2=None, op0=OP.mult), r=[K(ch, "on"), K(ch, "orstd")], w=[K(ch, "t2")])
            pt, pk = pslot()
            CI("pe", lambda e, c=c, pt=pt: e.matmul(pt, lhsT=c["t2"], rhs=ident, start=True, stop=True), r=[K(ch, "t2"), "ident"], w=[pk])
            CI("dve", lambda e, pt=pt, cs=cs, sl=sl: e.scalar_tensor_tensor(out=ystage[sl][:, 0, cs], in0=pt, scalar=col(PV_GNW), in1=zs[:, cs], op0=OP.mult, op1=OP.mult),
                r=[pk, "pvt", ("zs", par)], w=[("ystage", sl)])
        for g in range(2):
            P.DMA("sp", lambda e, blk=blk, sl=sl, g=g: e.dma_start(
                out=cin[blk // 4].rearrange("(i g p) t -> p i g t", i=16, g=2)[:, (blk % 4) * 4:(blk % 4) * 4 + 4, g, :],
                in_=ystage[sl][:, g, :].rearrange("p (i t) -> p i t", i=4)),
                  r=[("ystage", sl)], w=["cin"], ch=("yst", sl))


    load_x(0, P.DMA)
    front(0, P.I, P.DMA)
    for blk in range(NBLK):
        if blk + 1 < NBLK:
            front(blk + 1, Idef, DMAdef)
        chunks(blk)
        while pending:
            kind, a2, k2 = pending.popleft()
            getattr(P, kind)(*a2, **k2)


def phase2(nc, P, A, G):
    psb = G["psb"]; ident = G["ident"]; identb = G["identb"]; ones = G["ones"]; onesb = G["onesb"]
    pvt = G["pvt"]; col = G["col"]; debug = G["debug"]
    PV_NXA, PV_NMEM = 16, 24
    x_tok = G["x_tok"]; mem_b = G["mem_b"]; coutflat = G["coutflat"]; idxy = G["idxy"]
    NT = G.get("ntile", 16)

    def wload(dram, ncols, key):
        t = A.bf16(8 * ncols).rearrange("p (k c) -> p k c", k=8)
        for k in range(8):
            P.DMA("pool", lambda e, k=k, t=t: e.dma_start(out=t[:, k, :], in_=dram[k * 128:(k + 1) * 128, :]), w=[key], ch=key)
        return t
    woutb = wload(G["w_out"], 1024, "woutb")
    wqb = wload(G["xwq"], 1024, "wqb")
    wob = wload(G["xwo"], 1024, "wob")
    big = A.bf16(8 * 2048).rearrange("p (k c) -> p k c", k=8)
    for k in range(8):
        P.DMA("pool", lambda e, k=k: e.dma_start(out=big[:, k, :], in_=G["xwkv"][k * 128:(k + 1) * 128, :]), w=["big"], ch="big")
    skt = A.f32(2048).rearrange("p (c k) -> p c k", c=16)
    P.DMA("sp", lambda e: e.dma_start(out=skt, in_=G["skT"].rearrange("p (c k) -> p c k", c=16)), w=["skt"], ch="skt")
    iy = A.u32(8)
    P.DMA("sp", lambda e: e.dma_start(out=iy, in_=idxy[:, :]), w=["iy"], ch="iy")
    h3acc = A.f32(2048)
    rowt = h3acc
    P.DMA("sp", lambda e: e.dma_start(out=rowt[0:1, :], in_=G["rows"].rearrange("a d -> (a d)").unsqueeze(0)), w=["rowt"], ch="rowt")
    wbc = A.f32(2048)
    for i in range(4):
        P.I("pe", lambda e, i=i: e.matmul(psb[0][:, :], lhsT=ones[0:1, 0:128], rhs=rowt[0:1, i * 512:(i + 1) * 512], start=True, stop=True),
            r=["ones", "rowt"], w=[("bank", 0)])
        P.I("act", lambda e, i=i: e.activation(out=wbc[:, i * 512:(i + 1) * 512], in_=psb[0][:, :], func=AF.Copy), r=[("bank", 0)], w=["wbc"])
    iota16 = A.f32(256)
    P.I("pool", lambda e: e.iota(iota16, pattern=[[1, 256]], base=0, channel_multiplier=0, allow_small_or_imprecise_dtypes=True), w=["iota"])

    kT = A.bf16(8 * 256).rearrange("p (c m) -> p c m", c=8)
    vv = A.bf16(2 * 1024).rearrange("p (m c) -> p m c", m=2)
    xt = A.f32(1024); junk = A.f32(1024); h3 = h3acc[:, 0:1024]; acc = h3acc[:, 1024:2048]
    xnb = A.bf16(1024)
    hT = A.bf16(8 * 128).rearrange("p (k t) -> p k t", k=8)
    yT = A.bf16(8 * 128).rearrange("p (k t) -> p k t", k=8)
    qT = A.bf16(8 * 128).rearrange("p (k t) -> p k t", k=8)
    oT = A.bf16(8 * 128).rearrange("p (k t) -> p k t", k=8)
    expT = A.bf16(2 * 128).rearrange("p (m t) -> p m t", m=2)
    rden = A.f32(128)
    sm = A.f32(8)
    qsall = A.f32(4096)
    qpT = qsall[:, 0:2048].rearrange("p (c t) -> p c t", c=16)
    scs = qsall[:, 2048:4096].rearrange("p (c k) -> p c k", c=16)
    sct = A.f32(128)
    tv = A.f32(256).rearrange("p (c k) -> p c k", c=16)
    tiu = A.u32(256).rearrange("p (c k) -> p c k", c=16)
    tif = A.f32(256).rearrange("p (c k) -> p c k", c=16)
    cand = A.f32(256); cand2 = A.f32(256); cidx = A.f32(256)
    best = A.f32(16); posu = A.u32(16); posf = A.f32(16)
    oh = qsall.rearrange("p (k a) -> p k a", k=16)
    idxf = A.f32(128); idxu = A.u32(128); gate = A.f32(128); sval = A.f32(128); act = A.f32(128)
    NB = 8
    gb = [A.bf16(1024) for _ in range(NB)]
    junkb2 = A.bf16(1024)
    ps_all = G["ps_all"]
    h3p = ps_all[:, 0:1024]
    accp = [ps_all[:, 1024:2048], ps_all[:, 2048:3072]]
    accb = [[("bank", 2), ("bank", 3)], [("bank", 4), ("bank", 5)]]
    print("phase2 arena used", A.off, "of", A.n)
    pTb = psb[2][:, :].bitcast(BF16)

    def rms_to_hT(src, wcol0, key_src):
        P.I("act", lambda e: e.activation(out=junk, in_=src, func=AF.Square, accum_out=sm[:, 0:1]), r=[key_src], w=["junk", "sm0"])
        P.I("dve", lambda e: e.tensor_scalar(out=sm[:, 1:2], in0=sm[:, 0:1], scalar1=1.0 / D, scalar2=EPS, op0=OP.mult, op1=OP.add), r=["sm0"], w=["sm1"])
        P.I("act", lambda e: e.activation(out=sm[:, 1:2], in_=sm[:, 1:2], func=AF.Sqrt), r=["sm1"], w=["sm1"])
        P.I("dve", lambda e: e.reciprocal(out=sm[:, 2:3], in_=sm[:, 1:2]), r=["sm1"], w=["rstd"])
        if wcol0 is None:
            return
        P.I("dve", lambda e: e.tensor_scalar(out=xnb, in0=src, scalar1=sm[:, 2:3], scalar2=None, op0=OP.mult), r=[key_src, "rstd"], w=["xnb"])
        for k in range(8):
            P.I("pe", lambda e, k=k: e.transpose(out=pTb[:, (k % 4) * 128:(k % 4 + 1) * 128], in_=xnb[:, k * 128:(k + 1) * 128], identity=identb),
                r=["xnb", "identb"], w=[("bank", 2)])
            P.I("dve", lambda e, k=k: e.tensor_scalar(out=hT[:, k, :], in0=pTb[:, (k % 4) * 128:(k % 4 + 1) * 128], scalar1=col(wcol0 + k), scalar2=None, op0=OP.mult),
                r=[("bank", 2), "pvt"], w=["hT"])

    for mt in range(2):
        P.DMA("sp", lambda e, mt=mt: e.dma_start(out=xt, in_=mem_b[mt * 128:(mt + 1) * 128, :]), w=[("xt", 0)], ch="xtmem")
        rms_to_hT(xt, PV_NMEM, ("xt", 0))
        for half in range(2):
            for k in range(8):
                P.I("pe", lambda e, k=k, half=half: e.matmul(psb[half][:, :], lhsT=hT[:, k, :], rhs=big[:, k, 1024 + half * 512:1024 + (half + 1) * 512],
                                                             start=(k == 0), stop=(k == 7)), r=["hT", "big"], w=[("bank", half)])
            P.I("act", lambda e, half=half, mt=mt: e.activation(out=vv[:, mt, half * 512:(half + 1) * 512], in_=psb[half][:, :], func=AF.Copy), r=[("bank", half)], w=["vv"])
        for c in range(8):
            pb = 3 + c % 2
            for k in range(8):
                P.I("pe", lambda e, k=k, c=c, pb=pb: e.matmul(psb[pb][:, 0:128], lhsT=big[:, k, c * 128:(c + 1) * 128], rhs=hT[:, k, :], start=(k == 0), stop=(k == 7)),
                    r=["hT", "big"], w=[("bank", pb)])
            P.I("act", lambda e, c=c, pb=pb, mt=mt: e.activation(out=kT[:, c, mt * 128:(mt + 1) * 128], in_=psb[pb][:, 0:128], func=AF.Copy), r=[("bank", pb)], w=["kT"])
    for k in range(8):
        P.DMA("pool", lambda e, k=k: e.dma_start(out=big[:, k, :], in_=G["pwq"][k * 128:(k + 1) * 128, :]), r=[], w=["big"], ch="big2")

    cflat = coutflat
    import collections
    xts = [xt, A.f32(1024)]
    gates = [gate, A.f32(128)]
    idxus = [idxu, A.u32(128)]
    smA = sm
    smG = A.f32(8)
    idxTs = [A.u32(128), A.u32(128)]
    actT = A.bf16(128)
    accTs = A.f32(1024)
    accT = ps_all[:, 1024:2048]
    accp1 = accp[0]
    accbk = accb[0]
    pT6 = psb[6][:, :].bitcast(BF16)
    print("phase2 arena used (pipelined)", A.off, "of", A.n)
    P.I("pe", lambda e: e.matmul(psb[4][:, 0:8], lhsT=ones[0:1, 0:128], rhs=ones[0:1, 0:8], start=True, stop=True),
        r=["woutb", "wqb", "wob", "big"], w=["wts", ("bank", 4)])

    def rms_stats(I, src, key_src, smx, tag, jout, jkeys):
        I("act", lambda e: e.activation(out=jout, in_=src, func=AF.Square, accum_out=smx[:, 0:1]), r=[key_src], w=list(jkeys) + [tag + "0"])
        I("dve", lambda e: e.tensor_scalar(out=smx[:, 1:2], in0=smx[:, 0:1], scalar1=1.0 / D, scalar2=EPS, op0=OP.mult, op1=OP.add), r=[tag + "0"], w=[tag + "1"])
        I("act", lambda e: e.activation(out=smx[:, 1:2], in_=smx[:, 1:2], func=AF.Sqrt), r=[tag + "1"], w=[tag + "1"])
        I("dve", lambda e: e.reciprocal(out=smx[:, 2:3], in_=smx[:, 1:2]), r=[tag + "1"], w=[tag + "rstd"])

    def to_hT(I, wcol0):
        for k in range(8):
            I("pe", lambda e, k=k: e.transpose(out=pT6[:, (k % 4) * 128:(k % 4 + 1) * 128], in_=xnb[:, k * 128:(k + 1) * 128], identity=identb),
              r=["xnb", "identb"], w=[("bank", 6)])
            if wcol0 is None:
                I("dve", lambda e, k=k: e.tensor_copy(out=hT[:, k, :], in_=pT6[:, (k % 4) * 128:(k % 4 + 1) * 128]), r=[("bank", 6)], w=["hT"])
            else:
                I("dve", lambda e, k=k: e.tensor_scalar(out=hT[:, k, :], in0=pT6[:, (k % 4) * 128:(k % 4 + 1) * 128], scalar1=col(wcol0 + k), scalar2=None, op0=OP.mult),
                  r=[("bank", 6), "pvt"], w=["hT"])

    def resid_add(I, lhs, wmat, key_l, chunk_of, xt_, kx):
        for half in range(2):
            bk = 4 + half
            for k in range(8):
                I("pe", lambda e, k=k, half=half, bk=bk: e.matmul(psb[bk][:, :], lhsT=lhs[:, k, :], rhs=wmat[:, chunk_of(k), half * 512:(half + 1) * 512],
                                                                 start=(k == 0), stop=(k == 7)), r=[key_l, "wts"], w=[("bank", bk)])
            I("dve", lambda e, half=half, bk=bk: e.tensor_tensor(out=xt_[:, half * 512:(half + 1) * 512], in0=psb[bk][:, :], in1=xt_[:, half * 512:(half + 1) * 512], op=OP.add),
              r=[("bank", bk), kx], w=[kx])

    def stageA(it, par, I, DMA):
        xt_ = xts[par]; kx = ("xt", par); gate_ = gates[par]; idxu_ = idxus[par]; kg = ("gate", par); ki = ("idxu", par)
        DMA("sp", lambda e: e.dma_start(out=xt_, in_=x_tok[it * 128:(it + 1) * 128, :]), w=[kx], ch=("xt", par))
        for ag in range(8):
            DMA("pool", lambda e, ag=ag: e.indirect_dma_start(out=yT[:, ag, :], out_offset=None, in_=cflat,
                                                             in_offset=bass.IndirectOffsetOnAxis(ap=iy[:, ag:ag + 1], axis=0),
                                                             element_offset=it * 256 * 128),
                r=["cout", "iy"], w=["yT"], ch="yT")
        resid_add(I, yT, woutb, "yT", lambda k: (k // 2) + 4 * (k % 2), xt_, kx)
        if debug:
            DMA("sp", lambda e: e.dma_start(out=G["dbg"][0, it * 128:(it + 1) * 128, :], in_=xt_), r=[kx], w=["dbg0"], ch="dbg0")
        rms_stats(I, xt_, kx, smA, "smA", junk, ["junk"])
        I("dve", lambda e: e.tensor_scalar(out=xnb, in0=xt_, scalar1=smA[:, 2:3], scalar2=None, op0=OP.mult), r=[kx, "smArstd"], w=["xnb"])
        to_hT(I, PV_NXA)
        for c in range(8):
            pb = 6 + c % 2
            for k in range(8):
                I("pe", lambda e, k=k, c=c, pb=pb: e.matmul(psb[pb][:, 0:128], lhsT=wqb[:, k, c * 128:(c + 1) * 128], rhs=hT[:, k, :], start=(k == 0), stop=(k == 7)),
                  r=["hT", "wts"], w=[("bank", pb)])
            I("act", lambda e, c=c, pb=pb: e.activation(out=qT[:, c, :], in_=psb[pb][:, 0:128], func=AF.Copy), r=[("bank", pb)], w=["qT"])
        for h in range(4):
            for mc in range(2):
                for dc in range(2):
                    I("pe", lambda e, h=h, mc=mc, dc=dc: e.matmul(psb[6][:, mc * 128:(mc + 1) * 128], lhsT=kT[:, 2 * h + dc, mc * 128:(mc + 1) * 128], rhs=qT[:, 2 * h + dc, :],
                                                                  start=(dc == 0), stop=(dc == 1)), r=["kT", "qT"], w=[("bank", 6)])
            I("act", lambda e: e.activation(out=expT.rearrange("p m t -> p (m t)"), in_=psb[6][:, 0:256], func=AF.Exp, scale=1.0 / 16.0), r=[("bank", 6)], w=["expT"])
            for mc in range(2):
                I("pe", lambda e, mc=mc: e.matmul(psb[7][:, 0:128], lhsT=onesb, rhs=expT[:, mc, :], start=(mc == 0), stop=(mc == 1)), r=["expT", "onesb"], w=[("bank", 7)])
            I("dve", lambda e: e.reciprocal(out=rden, in_=psb[7][:, 0:128]), r=[("bank", 7)], w=["rden"])
            for dc in range(2):
                for mc in range(2):
                    I("pe", lambda e, h=h, mc=mc, dc=dc: e.matmul(psb[4][:, 0:128], lhsT=vv[:, mc, (2 * h + dc) * 128:(2 * h + dc + 1) * 128], rhs=expT[:, mc, :],
                                                                  start=(mc == 0), stop=(mc == 1)), r=["expT", "vv"], w=[("bank", 4)])
                I("dve", lambda e, h=h, dc=dc: e.tensor_tensor(out=oT[:, 2 * h + dc, :], in0=psb[4][:, 0:128], in1=rden, op=OP.mult), r=[("bank", 4), "rden"], w=["oT"])
        resid_add(I, oT, wob, "oT", lambda k: k, xt_, kx)
        if debug:
            DMA("sp", lambda e: e.dma_start(out=G["dbg"][1, it * 128:(it + 1) * 128, :], in_=xt_), r=[kx], w=["dbg1"], ch="dbg1")
        rms_stats(I, xt_, kx, smA, "smA", junk, ["junk"])
        I("dve", lambda e: e.scalar_tensor_tensor(out=h3, in0=xt_, scalar=smA[:, 2:3], in1=wbc[:, 0:1024], op0=OP.mult, op1=OP.mult), r=[kx, "smArstd", "wbc"], w=["h3"])
        I("act", lambda e: e.activation(out=xnb, in_=h3, func=AF.Copy), r=["h3"], w=["xnb"])
        to_hT(I, None)
        for c in range(16):
            pb = 6 + c % 2
            for k in range(8):
                I("pe", lambda e, k=k, c=c, pb=pb: e.matmul(psb[pb][:, 0:128], lhsT=big[:, k, c * 128:(c + 1) * 128], rhs=hT[:, k, :], start=(k == 0), stop=(k == 7)),
                  r=["hT", "wts"], w=[("bank", pb)])
            I("act", lambda e, c=c, pb=pb: e.activation(out=qpT[:, c, :], in_=psb[pb][:, 0:128], func=AF.Copy), r=[("bank", pb)], w=["qpT"])
        for c in range(16):
            pb = 4 + c % 2
            I("pe", lambda e, c=c, pb=pb: e.matmul(psb[pb][:, 0:128], lhsT=qpT[:, c, :], rhs=skt[:, c, :], start=True, stop=True), r=["qpT", "skt"], w=[("bank", pb)])
            I("act", lambda e, c=c, pb=pb: e.activation(out=scs[:, c, :], in_=psb[pb][:, 0:128], func=AF.Copy), r=[("bank", pb)], w=[("scs", c)])
            I("dve", lambda e, c=c: e.max(out=tv[:, c, 0:8], in_=scs[:, c, :]), r=[("scs", c)], w=[("tv", c)])
            I("dve", lambda e, c=c: e.max_index(out=tiu[:, c, 0:8], in_max=tv[:, c, 0:8], in_values=scs[:, c, :]), r=[("scs", c), ("tv", c)], w=[("tiu", c)])
            I("dve", lambda e, c=c: e.match_replace(out=sct, in_to_replace=tv[:, c, 0:8], in_values=scs[:, c, :], imm_value=-1e30), r=[("scs", c), ("tv", c)], w=["sct"])
            I("dve", lambda e, c=c: e.max(out=tv[:, c, 8:16], in_=sct), r=["sct"], w=[("tv", c)])
            I("dve", lambda e, c=c: e.max_index(out=tiu[:, c, 8:16], in_max=tv[:, c, 8:16], in_values=sct), r=["sct", ("tv", c)], w=[("tiu", c)])
            I("dve", lambda e, c=c: e.tensor_copy(out=tif[:, c, :], in_=tiu[:, c, :]), r=[("tiu", c)], w=[("tif", c)])
        for h in range(8):
            c1, c2 = 2 * h, 2 * h + 1
            c3 = cand.rearrange("p (a b) -> p a b", a=16)
            I("dve", lambda e, c1=c1, c2=c2, c3=c3: e.tensor_tensor(out=c3, in0=tv[:, c1, :].unsqueeze(2).broadcast_to([128, 16, 16]),
                                                                    in1=tv[:, c2, :].unsqueeze(1).broadcast_to([128, 16, 16]), op=OP.add),
              r=[("tv", c1), ("tv", c2)] + [("scs", c) for c in range(16)], w=["cand"])
            I("dve", lambda e, c1=c1, c2=c2: e.scalar_tensor_tensor(out=cidx.rearrange("p (a b) -> p a b", a=16), in0=tif[:, c1, :].unsqueeze(2).broadcast_to([128, 16, 16]), scalar=128.0,
                                                                    in1=tif[:, c2, :].unsqueeze(1).broadcast_to([128, 16, 16]), op0=OP.mult, op1=OP.add),
              r=[("tif", c1), ("tif", c2)], w=["cidx"])
            I("dve", lambda e: e.max(out=best[:, 0:8], in_=cand), r=["cand"], w=["best"])
            I("dve", lambda e: e.max_index(out=posu[:, 0:8], in_max=best[:, 0:8], in_values=cand), r=["cand", "best"], w=["posu"])
            I("dve", lambda e: e.match_replace(out=cand2, in_to_replace=best[:, 0:8], in_values=cand, imm_value=-1e30), r=["cand", "best"], w=["cand2"])
            I("dve", lambda e: e.max(out=best[:, 8:16], in_=cand2), r=["cand2"], w=["best"])
            I("dve", lambda e: e.max_index(out=posu[:, 8:16], in_max=best[:, 8:16], in_values=cand2), r=["cand2", "best"], w=["posu"])
            I("dve", lambda e: e.tensor_copy(out=posf, in_=posu), r=["posu"], w=["posf"])
            I("dve", lambda e: e.tensor_tensor(out=oh, in0=iota16.unsqueeze(1).broadcast_to([128, 16, 256]), in1=posf.unsqueeze(2).broadcast_to([128, 16, 256]), op=OP.is_equal),
              r=["iota", "posf", "qpT"] + [("scs", c) for c in range(16)], w=["oh"])
            I("dve", lambda e: e.tensor_tensor(out=oh, in0=oh, in1=cidx.unsqueeze(1).broadcast_to([128, 16, 256]), op=OP.mult), r=["oh", "cidx"], w=["oh"])
            I("dve", lambda e, h=h: e.tensor_reduce(out=idxf[:, h * 16:(h + 1) * 16], in_=oh, axis=AX.X, op=OP.add), r=["oh"], w=["idxf"])
            I("dve", lambda e: e.tensor_scalar(out=smA[:, 3:4], in0=best[:, 0:1], scalar1=-1.0, scalar2=None, op0=OP.mult), r=["best"], w=["nmax"])
            I("act", lambda e, h=h: e.activation(out=gate_[:, h * 16:(h + 1) * 16], in_=best, func=AF.Exp, bias=smA[:, 3:4], accum_out=smA[:, 4:5]), r=["best", "nmax"], w=[kg, "gsum"])
            I("dve", lambda e: e.reciprocal(out=smA[:, 5:6], in_=smA[:, 4:5]), r=["gsum"], w=["grs"])
            I("dve", lambda e, h=h: e.tensor_scalar(out=gate_[:, h * 16:(h + 1) * 16], in0=gate_[:, h * 16:(h + 1) * 16], scalar1=smA[:, 5:6], scalar2=None, op0=OP.mult), r=[kg, "grs"], w=[kg])
        I("dve", lambda e: e.tensor_copy(out=idxu_, in_=idxf), r=["idxf"], w=[ki])
        idxT_ = idxTs[par]
        I("pe", lambda e: e.matmul(psb[7][:, 0:128], lhsT=idxf, rhs=ident, start=True, stop=True), r=["idxf", "ident"], w=[("bank", 7)])
        I("dve", lambda e: e.tensor_copy(out=idxT_, in_=psb[7][:, 0:128]), r=[("bank", 7)], w=[("idxT", par)])

    def stageG(it, par, pump):
        xt_ = xts[par]; kx = ("xt", par); gate_ = gates[par]; idxu_ = idxus[par]; kg = ("gate", par); ki = ("idxu", par)
        P.I("dve", lambda e: e.tensor_copy(out=h3p, in_=h3), r=["h3"], w=[("bank", 0), ("bank", 1)])
        for j in range(128):
            sb = j % NB
            b = gb[sb]
            P.DMA("pool", lambda e, j=j, b=b: e.indirect_dma_start(out=b, out_offset=None, in_=G["ub"].ap()[:, :],
                                                                   in_offset=bass.IndirectOffsetOnAxis(ap=idxu_[:, j:j + 1], axis=0)),
                  r=[ki, "ub"], w=[("gb", sb)], ch=("gb", sb))
            P.I("dve", lambda e, j=j, b=b: e.scalar_tensor_tensor(out=junkb2, in0=b, scalar=1.0, in1=h3p, op0=OP.mult, op1=OP.mult, accum_out=sval[:, j:j + 1]),
                r=[("gb", sb)], w=[("sval", j)], ro=[("bank", 0), ("bank", 1)])
            pump()
        P.I("act", lambda e: e.activation(out=act, in_=sval, func=AF.Gelu_apprx_tanh), r=[("sval", j) for j in range(128)], w=["act"])
        P.I("dve", lambda e: e.tensor_tensor(out=act, in0=act, in1=gate_, op=OP.mult), r=["act", kg], w=["act"])
        idxT_ = idxTs[par]; kiT = ("idxT", par)
        P.I("pe", lambda e: e.matmul(psb[2][:, 0:128], lhsT=act, rhs=ident, start=True, stop=True), r=["act", "ident"], w=[("bank", 2)])
        P.I("act", lambda e: e.activation(out=actT, in_=psb[2][:, 0:128], func=AF.Copy), r=[("bank", 2)], w=["actT"])
        for t in range(128):
            sb = t % NB
            b = gb[sb]
            P.DMA("pool", lambda e, t=t, b=b: e.indirect_dma_start(out=b, out_offset=None, in_=G["vb"].ap()[:, :],
                                                                   in_offset=bass.IndirectOffsetOnAxis(ap=idxT_[:, t:t + 1], axis=0)),
                  r=[kiT, "vb"], w=[("gb", sb)], ch=("gb", sb))
            for c in range(8):
                P.I("pe", lambda e, t=t, b=b, c=c: e.matmul(accT[:, c * 128 + t:c * 128 + t + 1], lhsT=b[:, c * 128:(c + 1) * 128], rhs=actT[:, t:t + 1], start=True, stop=True),
                    r=[("gb", sb), "actT"], w=[("bank", 2 + c // 4)])
            pump()
        P.I("act", lambda e: e.activation(out=accTs, in_=accT, func=AF.Copy), r=[("bank", 2), ("bank", 3)], w=["accTs"])
        for c in range(8):
            P.I("pe", lambda e, c=c: e.matmul(psb[c // 4][:, (c % 4) * 128:(c % 4 + 1) * 128], lhsT=accTs[:, c * 128:(c + 1) * 128], rhs=ident, start=True, stop=True),
                r=["accTs", "ident"], w=[("bank", c // 4)])
        P.I("dve", lambda e: e.tensor_tensor(out=xt_, in0=h3p, in1=xt_, op=OP.add), r=[("bank", 0), ("bank", 1), kx], w=[kx])
        if debug:
            P.DMA("sp", lambda e: e.dma_start(out=G["dbg"][2, it * 128:(it + 1) * 128, :], in_=xt_), r=[kx], w=["dbg2"], ch="dbg2")
        rms_stats(P.I, xt_, kx, smG, "smG", h3p, [("bank", 0), ("bank", 1)])
        P.I("dve", lambda e: e.scalar_tensor_tensor(out=acc, in0=xt_, scalar=smG[:, 2:3], in1=wbc[:, 1024:2048], op0=OP.mult, op1=OP.mult), r=[kx, "smGrstd", "wbc"], w=["acc"])
        P.DMA("sp", lambda e: e.dma_start(out=G["out"][it * 128:(it + 1) * 128, :], in_=acc), r=["acc"], w=["outd"], ch="outd")

    pending = collections.deque()

    def Idef(*a, **k):
        pending.append(("I", a, k))

    def DMAdef(*a, **k):
        pending.append(("DMA", a, k))

    npump = [3]

    def pump(n=None):
        for _ in range(npump[0] if n is None else n):
            if not pending:
                return
            kind, a, k = pending.popleft()
            getattr(P, kind)(*a, **k)

    stageA(0, 0, P.I, P.DMA)
    for it in range(NT):
        if it + 1 < NT:
            stageA(it + 1, (it + 1) % 2, Idef, DMAdef)
            npump[0] = len(pending) // 250 + 1
        stageG(it, it % 2, pump)
        while pending:
            pump(1000)


def prep_inputs(inp):
    f = lambda a: np.ascontiguousarray(np.asarray(a, dtype=np.float32))
    x = f(inp["x"]); mem = f(inp["mem"])
    w_in = f(inp["w_in"])[0]
    cq = f(inp["conv_qkv_w"])[0]; lcw = f(inp["lru_conv_w"])[0]
    maps = []
    for c in range(NCORES):
        b, j = c // 4, c % 4
        cols = []
        for base in (0, 512, 1024, 2056, 2568, 1536):
            cols.append(w_in[:, base + j * 128: base + (j + 1) * 128])
        cols.append(np.repeat(w_in[:, 2048 + j:2049 + j], 128, axis=1))
        cols.append(np.repeat(w_in[:, 2052 + j:2053 + j], 128, axis=1))
        w1 = np.ascontiguousarray(np.concatenate(cols, axis=1))
        cwm = np.zeros((128, 16), np.float32)
        for s_, base in enumerate((0, 512, 1024)):
            cwm[:, 4 * s_:4 * s_ + 4] = cq[:, base + j * 128: base + (j + 1) * 128].T
        cwm[:, 12:16] = lcw[:, j * 128:(j + 1) * 128].T
        pvm = np.zeros((128, 40), np.float32)
        sl = slice(j * 128, (j + 1) * 128)
        pvm[:, 0] = f(inp["lru_conv_b"])[0, sl]
        pvm[:, 1] = f(inp["lru_ba"])[0, sl]
        pvm[:, 2] = f(inp["lru_bx"])[0, sl]
        pvm[:, 3] = f(inp["lru_lambda"])[0, sl]
        pvm[:, 4] = f(inp["gdn_a_log"])[0, j]
        pvm[:, 5] = f(inp["gdn_dt_bias"])[0, j]
        pvm[:, 6] = f(inp["gdn_norm_w"])[0]
        pvm[:, 8:16] = f(inp["norm_mix_w"])[0].reshape(8, 128).T
        pvm[:, 16:24] = f(inp["norm_xattn_w"])[0].reshape(8, 128).T
        pvm[:, 24:32] = f(inp["norm_mem_w"])[0].reshape(8, 128).T
        lwm = np.stack([f(inp["lru_wa"])[0, 2 * j:2 * j + 2], f(inp["lru_wx"])[0, 2 * j:2 * j + 2]])
        skT = np.ascontiguousarray(f(inp["peer_subkeys"])[0].reshape(16, 128, 128).transpose(2, 0, 1).reshape(128, 16 * 128))
        maps.append({
            "x_b": x[b], "w1": w1, "cw": cwm, "pv": pvm, "lw": np.ascontiguousarray(lwm),
            "mem_b": mem[b], "w_out": f(inp["w_out"])[0], "xwq": f(inp["xattn_wq"])[0],
            "xwkv": f(inp["xattn_wkv"])[0], "xwo": f(inp["xattn_wo"])[0], "pwq": f(inp["peer_wq"])[0],
            "skT": skT, "peer_u": f(inp["peer_u"])[0], "peer_v": f(inp["peer_v"])[0],
            "rows": np.stack([f(inp["norm_ffn_w"])[0], f(inp["norm_final_w"])]),
            "x_tok": np.ascontiguousarray(x[b, j * TOK:(j + 1) * TOK]),
            "idxy": np.ascontiguousarray((j * 16384 + np.arange(4)[None, :, None] * 4096 + np.arange(2)[None, None, :] * 128
                                          + np.arange(128)[:, None, None]).reshape(128, 8).astype(np.uint32)),
        })
    return maps


_NC = {}


def kernel(**inputs):
    if "nc" not in _NC:
        _NC["nc"] = build(False)
    maps = prep_inputs(inputs)
    res = run_bass_kernel_spmd(_NC["nc"], maps, core_ids=list(range(NCORES)))
    o = np.concatenate([res.results[c]["out"] for c in range(NCORES)], axis=0)
    return o.reshape(2, SEQ, D).astype(np.float32)
```
